# Optimizing a Trainium2 kernel written in Bass

```python
import jax
import jax.numpy as jnp
from jax import lax
import numpy as np

D_MODEL = 1024
BATCH = 8
SEQ = 2048
DEPTH = 2

HEAD_DIM = 64
NORM_EPS = 1e-6
NEG = -1e30
BIG = 1e30
ATTN_BLOCK = 128

SSD_HEADS = 8
SSD_D_INNER = SSD_HEADS * HEAD_DIM
SSD_STATE = 128
SSD_GROUPS = 2
SSD_CONV = 4
SSD_CHUNK = 128
SSD_CONV_DIM = SSD_D_INNER + 2 * SSD_GROUPS * SSD_STATE

NSA_HEADS = 8
NSA_KV_HEADS = 2
NSA_CMP_BLOCK = 32
NSA_CMP_STRIDE = 16
NSA_CMP_HIDDEN = 64
NSA_SEL_BLOCK = 64
NSA_TOPK = 8
NSA_WINDOW = 512
NSA_SEL_QBLOCK = 64

RWKV_HEADS = 8
RWKV_DIM = RWKV_HEADS * HEAD_DIM
RWKV_W_LORA = 64
RWKV_A_LORA = 64
RWKV_G_LORA = 128
RWKV_IN = 3 * RWKV_DIM + RWKV_W_LORA + RWKV_A_LORA + RWKV_G_LORA
RWKV_GN_EPS = 64e-5

SWA_HEADS = 8
SWA_KV_HEADS = 2
SWA_WINDOW = 128
ROPE_THETA = 150000.0

N_BRANCHES = 4
D_FF = 2816
N_EXPERTS = 8
TOP_K = 2
MOE_BLOCK = 128
N_DENSE = (DEPTH + 1) // 2
N_MOE = DEPTH // 2
PLE_DIM = 256

IN_SPLITS = (SSD_D_INNER, SSD_CONV_DIM, SSD_HEADS,
             NSA_HEADS * HEAD_DIM, 6 * NSA_KV_HEADS * HEAD_DIM, 3 * NSA_HEADS,
             RWKV_IN,
             SWA_HEADS * HEAD_DIM, 2 * SWA_KV_HEADS * HEAD_DIM,
             N_BRANCHES * D_MODEL)
D_IN = sum(IN_SPLITS)

kernel_name = "hybrid_ssd_nsa_rwkv7_swa_moe"


def rmsnorm(x, g):
    xf = x.astype(jnp.float32)
    y = xf * lax.rsqrt(jnp.mean(xf * xf, -1, keepdims=True) + NORM_EPS)
    return (y * g.astype(jnp.float32)).astype(x.dtype)


def split_cols(u, widths):
    offsets = [int(o) for o in np.cumsum(widths)[:-1]]
    return jnp.split(u, offsets, axis=-1)


def swiglu(h, w_gate, w_up, w_down):
    return (jax.nn.silu(h @ w_gate) * (h @ w_up)) @ w_down


def causal_dwconv(x, w, b):
    K, C = w.shape
    y = lax.conv_general_dilated(x, w[:, None, :].astype(x.dtype), window_strides=(1,),
                                 padding=[(K - 1, 0)], dimension_numbers=('NWC', 'WIO', 'NWC'),
                                 feature_group_count=C)
    return y + b.astype(x.dtype)


def rope(x, positions):
    half = HEAD_DIM // 2
    inv_freq = ROPE_THETA ** (-jnp.arange(half, dtype=jnp.float32) / half)
    ang = positions.astype(jnp.float32)[..., None] * inv_freq
    ang = ang.reshape(ang.shape[:2] + (1,) * (x.ndim - 3) + (half,))
    cos, sin = jnp.cos(ang), jnp.sin(ang)
    xf = x.astype(jnp.float32)
    x1, x2 = xf[..., :half], xf[..., half:]
    return jnp.concatenate([x1 * cos - x2 * sin, x2 * cos + x1 * sin], -1).astype(x.dtype)


def banded_attention(q, k, v, window, sinks=None):
    Bsz, S, G, R, dh = q.shape
    nb = S // ATTN_BLOCK
    span = window + ATTN_BLOCK
    kp = jnp.pad(k, ((0, 0), (window, 0), (0, 0), (0, 0)))
    vp = jnp.pad(v, ((0, 0), (window, 0), (0, 0), (0, 0)))
    kidx = jnp.arange(nb)[:, None] * ATTN_BLOCK + jnp.arange(span)[None, :]
    kb = kp[:, kidx]
    vb = vp[:, kidx]
    qb = q.reshape(Bsz, nb, ATTN_BLOCK, G, R, dh)
    s = jnp.einsum('bnqgrd,bnkgd->bngrqk', qb, kb).astype(jnp.float32) * (dh ** -0.5)
    qpos = jnp.arange(S).reshape(nb, ATTN_BLOCK)
    kpos = kidx - window
    rel = qpos[:, :, None] - kpos[:, None, :]
    mask = (rel >= 0) & (rel < window) & (kpos[:, None, :] >= 0)
    mask = mask[None, :, None, None]
    s = jnp.where(mask, s, NEG)
    if sinks is None:
        pr = jax.nn.softmax(s, -1)
    else:
        sk = sinks.astype(jnp.float32).reshape(1, 1, G, R, 1, 1)
        m = jnp.maximum(s.max(-1, keepdims=True), sk)
        e = jnp.exp(s - m)
        pr = e / (e.sum(-1, keepdims=True) + jnp.exp(sk - m))
    o = jnp.einsum('bngrqk,bnkgd->bnqgrd', pr.astype(q.dtype), vb)
    return o.reshape(Bsz, S, G, R, dh)


def ssd_mixer(z, xbc, dt_raw, conv_w, conv_b, dt_bias, a_log, d_skip, norm_w):
    f32 = jnp.float32
    Bsz, S, _ = z.shape
    G, R, P, N, L = SSD_GROUPS, SSD_HEADS // SSD_GROUPS, HEAD_DIM, SSD_STATE, SSD_CHUNK
    nc = S // L
    xbc = jax.nn.silu(causal_dwconv(xbc, conv_w, conv_b))
    xs, Bm, Cm = split_cols(xbc, (SSD_D_INNER, G * N, G * N))
    X = xs.reshape(Bsz, nc, L, G, R, P).astype(f32)
    Bc = Bm.reshape(Bsz, nc, L, G, N).astype(f32)
    Cc = Cm.reshape(Bsz, nc, L, G, N).astype(f32)
    dt = jax.nn.softplus(dt_raw.astype(f32) + dt_bias.astype(f32)).reshape(Bsz, nc, L, G, R)
    A = -jnp.exp(a_log.astype(f32)).reshape(G, R)
    a_cum = jnp.cumsum(dt * A, axis=2)
    tril = jnp.tril(jnp.ones((L, L), bool))[None, None, :, :, None, None]
    seg = a_cum[:, :, :, None] - a_cum[:, :, None, :]
    decay = jnp.exp(jnp.where(tril, seg, -jnp.inf))
    cb = jnp.einsum('bclgn,bcsgn->bclsg', Cc, Bc)
    w_ls = cb[..., None] * decay * dt[:, :, None]
    y_diag = jnp.einsum('bclsgr,bcsgrp->bclgrp', w_ls, X)
    decay_to_end = jnp.exp(a_cum[:, :, -1:] - a_cum)
    states = jnp.einsum('bclgn,bclgr,bclgrp->bcgrpn', Bc, decay_to_end * dt, X)
    chunk_decay = jnp.exp(a_cum[:, :, -1])

    def step(hs, inp):
        st, dec = inp
        return hs * dec[..., None, None] + st, hs

    h0 = jnp.zeros((Bsz, G, R, P, N), f32)
    _, prev = lax.scan(step, h0, (jnp.moveaxis(states, 1, 0), jnp.moveaxis(chunk_decay, 1, 0)))
    prev = jnp.moveaxis(prev, 0, 1)
    y_off = jnp.einsum('bclgn,bcgrpn,bclgr->bclgrp', Cc, prev, jnp.exp(a_cum))
    y = y_diag + y_off + d_skip.astype(f32).reshape(G, R)[..., None] * X
    y = y.reshape(Bsz, S, SSD_D_INNER)
    yg = (y * jax.nn.silu(z.astype(f32))).reshape(Bsz, S, G, SSD_D_INNER // G)
    yg = yg * lax.rsqrt(jnp.mean(yg * yg, -1, keepdims=True) + NORM_EPS)
    return (yg.reshape(Bsz, S, SSD_D_INNER) * norm_w.astype(f32)).astype(z.dtype)


def compress_blocks(kv, pe, w1, w2):
    S = kv.shape[1]
    nc = (S - NSA_CMP_BLOCK) // NSA_CMP_STRIDE + 1
    idx = jnp.arange(nc)[:, None] * NSA_CMP_STRIDE + jnp.arange(NSA_CMP_BLOCK)[None, :]
    blk = kv[:, idx] + pe[None, None, :, None, :].astype(kv.dtype)
    hid = jax.nn.silu(jnp.einsum('bnlgd,ldf->bngf', blk, w1))
    return jnp.einsum('bngf,fd->bngd', hid, w2)


def nsa_mixer(q, kv, gates, cmp_pe, cmp_w1, cmp_w2):
    f32 = jnp.float32
    Bsz, S, _ = q.shape
    G, R, dh = NSA_KV_HEADS, NSA_HEADS // NSA_KV_HEADS, HEAD_DIM
    scale = dh ** -0.5
    q = q.reshape(Bsz, S, G, R, dh)
    k_c, v_c, k_s, v_s, k_w, v_w = [t.reshape(Bsz, S, G, dh) for t in jnp.split(kv, 6, axis=-1)]
    t_pos = jnp.arange(S)
    kc = compress_blocks(k_c, cmp_pe[0], cmp_w1[0], cmp_w2[0])
    vc = compress_blocks(v_c, cmp_pe[1], cmp_w1[1], cmp_w2[1])
    nc = kc.shape[1]
    cmp_end = jnp.arange(nc) * NSA_CMP_STRIDE + NSA_CMP_BLOCK - 1
    cmask = (cmp_end[None, :] <= t_pos[:, None])[None, :, None, None, :]
    s = jnp.einsum('bsgrd,bngd->bsgrn', q, kc).astype(f32) * scale
    s = jnp.where(cmask, s, NEG)
    p_cmp = jnp.where(cmask, jax.nn.softmax(s, -1), 0.0)
    o_cmp = jnp.einsum('bsgrn,bngd->bsgrd', p_cmp.astype(q.dtype), vc)
    n_sel = S // NSA_SEL_BLOCK
    c_start = jnp.arange(nc) * NSA_CMP_STRIDE
    s_start = jnp.arange(n_sel) * NSA_SEL_BLOCK
    overlap = ((c_start[:, None] < s_start[None, :] + NSA_SEL_BLOCK)
               & (c_start[:, None] + NSA_CMP_BLOCK > s_start[None, :])).astype(f32)
    imp = jnp.einsum('bsgrn,nj->bsgj', p_cmp, overlap)
    blk_of_t = t_pos // NSA_SEL_BLOCK
    j = jnp.arange(n_sel)
    valid = (j[None, :] <= blk_of_t[:, None])[None, :, None, :]
    forced = ((j[None, :] == 0) | (j[None, :] == blk_of_t[:, None]))[None, :, None, :]
    score = jnp.where(forced, BIG, jnp.where(valid, imp, NEG))
    k_eff = min(NSA_TOPK, n_sel)
    _, sel_idx = lax.top_k(score, k_eff)
    kb = k_s.reshape(Bsz, n_sel, NSA_SEL_BLOCK, G, dh).transpose(0, 3, 1, 2, 4)
    vb = v_s.reshape(Bsz, n_sel, NSA_SEL_BLOCK, G, dh).transpose(0, 3, 1, 2, 4)
    QB = NSA_SEL_QBLOCK
    nq = S // QB
    bi = jnp.arange(Bsz)[:, None, None, None]
    gi = jnp.arange(G)[None, None, :, None]

    def sel_block(args):
        qb, ib, tb = args
        kg = kb[bi, gi, ib]
        vg = vb[bi, gi, ib]
        sc = jnp.einsum('bqgrd,bqgkld->bqgrkl', qb, kg).astype(f32) * scale
        kpos = ib[..., None] * NSA_SEL_BLOCK + jnp.arange(NSA_SEL_BLOCK)
        m = (kpos <= tb[None, :, None, None, None])[:, :, :, None]
        sc = jnp.where(m, sc, NEG)
        shp = sc.shape
        pr = jax.nn.softmax(sc.reshape(shp[:4] + (-1,)), -1).reshape(shp)
        return jnp.einsum('bqgrkl,bqgkld->bqgrd', pr.astype(qb.dtype), vg)

    qs = q.reshape(Bsz, nq, QB, G, R, dh).swapaxes(0, 1)
    iss = sel_idx.reshape(Bsz, nq, QB, G, k_eff).swapaxes(0, 1)
    ts = t_pos.reshape(nq, QB)
    o_sel = lax.map(sel_block, (qs, iss, ts)).swapaxes(0, 1).reshape(Bsz, S, G, R, dh)
    o_win = banded_attention(q, k_w, v_w, NSA_WINDOW)
    g = jax.nn.sigmoid(gates).reshape(Bsz, S, 3, G, R, 1)
    o = g[:, :, 0] * o_cmp + g[:, :, 1] * o_sel + g[:, :, 2] * o_win
    return o.reshape(Bsz, S, NSA_HEADS * dh).astype(q.dtype)


def rwkv7_mixer(u, mu, w0, w_up, a0, a_up, g_up, k_k, k_a, r_k, ln_w, ln_b):
    f32 = jnp.float32
    Bsz, S, _ = u.shape
    H, N = RWKV_HEADS, HEAD_DIM
    prev = jnp.pad(u, ((0, 0), (1, 0), (0, 0)))[:, :-1]
    u = u + (prev - u) * mu.astype(u.dtype)
    r, k, v, wd, ad, gd = split_cols(u, (RWKV_DIM, RWKV_DIM, RWKV_DIM, RWKV_W_LORA, RWKV_A_LORA, RWKV_G_LORA))
    w = -jax.nn.softplus(-(w0 + jnp.tanh(wd) @ w_up).astype(f32)) - 0.5
    decay = jnp.exp(-jnp.exp(w))
    a = jax.nn.sigmoid((a0 + ad @ a_up).astype(f32))
    g = jax.nn.sigmoid(gd) @ g_up
    r = r.astype(f32)
    k = k.astype(f32)
    v = v.astype(f32)
    kk = (k * k_k.astype(f32)).reshape(Bsz, S, H, N)
    kk = kk / jnp.maximum(jnp.sqrt(jnp.sum(kk * kk, -1, keepdims=True)), 1e-12)
    k = k * (1.0 + (a - 1.0) * k_a.astype(f32))
    heads = lambda t: t.reshape(Bsz, S, H, N)
    r, k, v, decay, a = heads(r), heads(k), heads(v), heads(decay), heads(a)

    def step(state, inp):
        r_t, w_t, k_t, v_t, kk_t, a_t = inp
        sa = jnp.einsum('bhvk,bhk->bhv', state, -kk_t)
        state = (state * w_t[:, :, None, :] + sa[..., None] * (kk_t * a_t)[:, :, None, :]
                 + v_t[..., None] * k_t[:, :, None, :])
        return state, jnp.einsum('bhvk,bhk->bhv', state, r_t)

    xs = tuple(jnp.moveaxis(t, 1, 0) for t in (r, decay, k, v, kk, a))
    _, o = lax.scan(step, jnp.zeros((Bsz, H, N, N), f32), xs)
    o = jnp.moveaxis(o, 0, 1)
    mean = o.mean(-1, keepdims=True)
    var = jnp.mean((o - mean) ** 2, -1, keepdims=True)
    o = ((o - mean) * lax.rsqrt(var + RWKV_GN_EPS)).reshape(Bsz, S, RWKV_DIM)
    o = o * ln_w.astype(f32) + ln_b.astype(f32)
    bonus = jnp.sum(r * k * r_k.astype(f32).reshape(H, N), -1, keepdims=True) * v
    o = o + bonus.reshape(Bsz, S, RWKV_DIM)
    return (o * g.astype(f32)).astype(u.dtype)


def swa_mixer(q, kv, positions, sinks):
    Bsz, S, _ = q.shape
    G, R = SWA_KV_HEADS, SWA_HEADS // SWA_KV_HEADS
    q = rope(q.reshape(Bsz, S, G, R, HEAD_DIM), positions)
    k, v = jnp.split(kv, 2, axis=-1)
    k = rope(k.reshape(Bsz, S, G, HEAD_DIM), positions)
    v = v.reshape(Bsz, S, G, HEAD_DIM)
    o = banded_attention(q, k, v, SWA_WINDOW, sinks)
    return o.reshape(Bsz, S, SWA_HEADS * HEAD_DIM)


def moe_swiglu(h, router, w_gate, w_up, w_down):
    f32 = jnp.float32
    Bsz, S, D = h.shape
    T = Bsz * S
    TK = T * TOP_K
    xt = h.reshape(T, D)
    logits = (xt @ router).astype(f32)
    top_v, top_e = lax.top_k(logits, TOP_K)
    wts = jax.nn.softmax(top_v, -1)
    flat_e = top_e.reshape(-1)
    flat_t = jnp.repeat(jnp.arange(T, dtype=jnp.int32), TOP_K)
    flat_w = wts.reshape(-1)
    order = jnp.argsort(flat_e)
    se = flat_e[order]
    counts = jnp.bincount(flat_e, length=N_EXPERTS)
    starts = jnp.cumsum(counts) - counts
    pcounts = (counts + MOE_BLOCK - 1) // MOE_BLOCK * MOE_BLOCK
    pends = jnp.cumsum(pcounts)
    pstarts = pends - pcounts
    dest = pstarts[se] + jnp.arange(TK) - starts[se]
    nblk = -(-(TK + N_EXPERTS * (MOE_BLOCK - 1)) // MOE_BLOCK)
    P = nblk * MOE_BLOCK
    slot_tok = jnp.full((P,), T, jnp.int32).at[dest].set(flat_t[order])
    slot_w = jnp.zeros((P,), f32).at[dest].set(flat_w[order])
    blk_e = jnp.minimum(jnp.searchsorted(pends, jnp.arange(nblk) * MOE_BLOCK, side='right'), N_EXPERTS - 1)
    xpad = jnp.concatenate([xt, jnp.zeros((1, D), xt.dtype)], 0)
    xb = xpad[slot_tok].reshape(nblk, MOE_BLOCK, D)

    def expert_block(args):
        xs, e = args
        return swiglu(xs, w_gate[e], w_up[e], w_down[e])

    yb = lax.map(expert_block, (xb, blk_e)).reshape(P, D)
    yb = yb * slot_w[:, None].astype(yb.dtype)
    out = jnp.zeros((T + 1, D), yb.dtype).at[slot_tok].add(yb)[:T]
    return out.reshape(Bsz, S, D)


def setup_inputs(seed: int = 0) -> dict:
    key = jax.random.key(seed)
    keys = jax.random.split(key, 64)
    cnt = [0]

    def nk():
        cnt[0] += 1
        return keys[cnt[0] - 1]

    def nrm(shape, scale):
        return jax.random.normal(nk(), shape, jnp.float32) * scale

    def unif(shape, lo, hi):
        return jax.random.uniform(nk(), shape, jnp.float32, lo, hi)

    def gain(shape):
        return 1.0 + nrm(shape, 0.02)

    L = DEPTH
    dt = jnp.exp(unif((L, SSD_HEADS), float(np.log(1e-3)), float(np.log(1e-1))))
    return {
        "x": nrm((BATCH, SEQ, D_MODEL), 1.0),
        "p": nrm((DEPTH, BATCH, SEQ, PLE_DIM), 1.0),
        "positions": jax.random.randint(nk(), (BATCH, 1), 0, 4096, dtype=jnp.int32) + jnp.arange(SEQ, dtype=jnp.int32)[None, :],
        "norm_mix": gain((L, D_MODEL)),
        "w_in": nrm((L, D_MODEL, D_IN), D_MODEL ** -0.5),
        "ssd_conv_w": nrm((L, SSD_CONV, SSD_CONV_DIM), SSD_CONV ** -0.5),
        "ssd_conv_b": nrm((L, SSD_CONV_DIM), 0.02),
        "ssd_dt_bias": dt + jnp.log(-jnp.expm1(-dt)),
        "ssd_a_log": jnp.log(unif((L, SSD_HEADS), 1.0, 16.0)),
        "ssd_d": 1.0 + nrm((L, SSD_HEADS), 0.1),
        "ssd_norm": gain((L, SSD_D_INNER)),
        "nsa_cmp_pe": nrm((L, 2, NSA_CMP_BLOCK, HEAD_DIM), 0.1),
        "nsa_cmp_w1": nrm((L, 2, NSA_CMP_BLOCK, HEAD_DIM, NSA_CMP_HIDDEN), (NSA_CMP_BLOCK * HEAD_DIM) ** -0.5),
        "nsa_cmp_w2": nrm((L, 2, NSA_CMP_HIDDEN, HEAD_DIM), NSA_CMP_HIDDEN ** -0.5),
        "rwkv_mu": unif((L, RWKV_IN), 0.0, 1.0),
        "rwkv_w0": unif((L, RWKV_DIM), -6.0, -1.0),
        "rwkv_w_up": nrm((L, RWKV_W_LORA, RWKV_DIM), 0.1 * RWKV_W_LORA ** -0.5),
        "rwkv_a0": nrm((L, RWKV_DIM), 0.1),
        "rwkv_a_up": nrm((L, RWKV_A_LORA, RWKV_DIM), 0.1 * RWKV_A_LORA ** -0.5),
        "rwkv_g_up": nrm((L, RWKV_G_LORA, RWKV_DIM), RWKV_G_LORA ** -0.5),
        "rwkv_k_k": 0.85 + nrm((L, RWKV_DIM), 0.02),
        "rwkv_k_a": 1.0 + nrm((L, RWKV_DIM), 0.02),
        "rwkv_r_k": nrm((L, RWKV_DIM), 0.1),
        "rwkv_ln_w": gain((L, RWKV_DIM)),
        "rwkv_ln_b": nrm((L, RWKV_DIM), 0.02),
        "swa_sinks": nrm((L, SWA_HEADS), 0.5),
        "w_br_ssd": nrm((L, SSD_D_INNER, D_MODEL), SSD_D_INNER ** -0.5),
        "w_br_nsa": nrm((L, NSA_HEADS * HEAD_DIM, D_MODEL), (NSA_HEADS * HEAD_DIM) ** -0.5),
        "w_br_rwkv": nrm((L, RWKV_DIM, D_MODEL), RWKV_DIM ** -0.5),
        "w_br_swa": nrm((L, SWA_HEADS * HEAD_DIM, D_MODEL), (SWA_HEADS * HEAD_DIM) ** -0.5),
        "w_out": nrm((L, D_MODEL, D_MODEL), D_MODEL ** -0.5),
        "norm_ffn": gain((L, D_MODEL)),
        "ffn_w_gate": nrm((N_DENSE, D_MODEL, D_FF), D_MODEL ** -0.5),
        "ffn_w_up": nrm((N_DENSE, D_MODEL, D_FF), D_MODEL ** -0.5),
        "ffn_w_down": nrm((N_DENSE, D_FF, D_MODEL), D_FF ** -0.5),
        "moe_router": nrm((N_MOE, D_MODEL, N_EXPERTS), D_MODEL ** -0.5),
        "moe_w_gate": nrm((N_MOE, N_EXPERTS, D_MODEL, D_FF), D_MODEL ** -0.5),
        "moe_w_up": nrm((N_MOE, N_EXPERTS, D_MODEL, D_FF), D_MODEL ** -0.5),
        "moe_w_down": nrm((N_MOE, N_EXPERTS, D_FF, D_MODEL), D_FF ** -0.5),
        "ple_proj": nrm((L, PLE_DIM, D_MODEL), PLE_DIM ** -0.5),
        "ple_gate": nrm((L, D_MODEL, D_MODEL), D_MODEL ** -0.5),
        "norm_final": gain((D_MODEL,)),
    }


def reference(x, p, positions, norm_mix, w_in, ssd_conv_w, ssd_conv_b, ssd_dt_bias, ssd_a_log, ssd_d,
              ssd_norm, nsa_cmp_pe, nsa_cmp_w1, nsa_cmp_w2, rwkv_mu, rwkv_w0, rwkv_w_up, rwkv_a0, rwkv_a_up,
              rwkv_g_up, rwkv_k_k, rwkv_k_a, rwkv_r_k, rwkv_ln_w, rwkv_ln_b, swa_sinks, w_br_ssd, w_br_nsa,
              w_br_rwkv, w_br_swa, w_out, norm_ffn, ffn_w_gate, ffn_w_up, ffn_w_down, moe_router, moe_w_gate,
              moe_w_up, moe_w_down, ple_proj, ple_gate, norm_final):
    Bsz, S, _ = x.shape
    for i in range(DEPTH):
        h = rmsnorm(x, norm_mix[i])
        u = h @ w_in[i]
        (z, xbc, dt_raw, nsa_q, nsa_kv, nsa_g, rwkv_u, swa_q, swa_kv, gate_logits) = split_cols(u, IN_SPLITS)
        y_ssd = ssd_mixer(z, xbc, dt_raw, ssd_conv_w[i], ssd_conv_b[i], ssd_dt_bias[i], ssd_a_log[i],
                          ssd_d[i], ssd_norm[i])
        y_nsa = nsa_mixer(nsa_q, nsa_kv, nsa_g, nsa_cmp_pe[i], nsa_cmp_w1[i], nsa_cmp_w2[i])
        y_rwkv = rwkv7_mixer(rwkv_u, rwkv_mu[i], rwkv_w0[i], rwkv_w_up[i], rwkv_a0[i], rwkv_a_up[i],
                             rwkv_g_up[i], rwkv_k_k[i], rwkv_k_a[i], rwkv_r_k[i], rwkv_ln_w[i], rwkv_ln_b[i])
        y_swa = swa_mixer(swa_q, swa_kv, positions, swa_sinks[i])
        g = jax.nn.sigmoid(gate_logits).reshape(Bsz, S, N_BRANCHES, D_MODEL)
        merged = (g[:, :, 0] * (y_ssd @ w_br_ssd[i]) + g[:, :, 1] * (y_nsa @ w_br_nsa[i])
                  + g[:, :, 2] * (y_rwkv @ w_br_rwkv[i]) + g[:, :, 3] * (y_swa @ w_br_swa[i]))
        x = x + merged @ w_out[i]
        h = rmsnorm(x, norm_ffn[i])
        j = i // 2
        if i % 2 == 0:
            x = x + swiglu(h, ffn_w_gate[j], ffn_w_up[j], ffn_w_down[j])
        else:
            x = x + moe_swiglu(h, moe_router[j], moe_w_gate[j], moe_w_up[j], moe_w_down[j])
        x = x + jax.nn.sigmoid(x @ ple_gate[i]) * (p[i] @ ple_proj[i])
    return rmsnorm(x, norm_final)
```

```python
import types
import numpy as np
import ml_dtypes
import concourse.bass as bass
import concourse.mybir as mybir
from concourse.bass_utils import run_bass_kernel_spmd

F32 = mybir.dt.float32
BF16 = mybir.dt.bfloat16
I32 = mybir.dt.int32
AF = mybir.ActivationFunctionType
ALU = mybir.AluOpType
AX = mybir.AxisListType

ENGS = ["sync", "scalar", "vector", "gpsimd", "tensor"]
N_DMA_SEMS = 48

T = 2048
D = 1024
NT = 16
DEPTH = 2
D_IN = 9504
D_FF = 2816
NEGB = -30000.0
OFF_Z = 0
OFF_XBC = 512
OFF_DT = 1536
OFF_NQ = 1544
OFF_NKV = 2056
OFF_NG = 2824
OFF_RW = 2848
OFF_SQ = 4640
OFF_SKV = 5152
OFF_GATE = 5408


class Buf:
    def __init__(self, t, name):
        self.t = t
        self.name = name
        self.tr = {}

    def __getitem__(self, idx):
        return self.t[idx]

    def _entries(self, key):
        if key is None:
            if None not in self.tr:
                self.tr[None] = [[], []]
            return list(self.tr.values())
        out = []
        if None in self.tr:
            out.append(self.tr[None])
        if key not in self.tr:
            self.tr[key] = [[], []]
        out.append(self.tr[key])
        return out

    def all_deps(self):
        d = []
        for w, r in self.tr.values():
            d.extend(w)
            d.extend(r)
        return d


def _freeze(fn):
    if getattr(fn, "__closure__", None) is None:
        return fn
    cells = []
    for c in fn.__closure__:
        try:
            cells.append(types.CellType(c.cell_contents))
        except ValueError:
            cells.append(c)
    return types.FunctionType(fn.__code__, fn.__globals__, fn.__name__, fn.__defaults__, tuple(cells))


def _compress(lst):
    best = {}
    for s, v in lst:
        if s not in best or best[s] < v:
            best[s] = v
    return list(best.items())


class Prog:
    def __init__(self):
        self.nc = bass.Bass("TRN2", target_bir_lowering=False)
        nc = self.nc
        self.ops = {e: [] for e in ENGS}
        self.cnt = {e: 0 for e in ENGS}
        self.esem = {e: nc.alloc_semaphore("es_" + e) for e in ENGS}
        self.dsem = [nc.alloc_semaphore("ds%d" % i) for i in range(N_DMA_SEMS)]
        self.dcnt = [0] * N_DMA_SEMS
        self.dnext = 0
        self.waited = {e: {} for e in ENGS}
        self.nbuf = 0
        self.out_deps = []
        self.arena = None
        self.aoff = 0
        self.alive = []
        self.retired = []

    def sb(self, shape, dt=F32, name=None):
        self.nbuf += 1
        name = name or "sb%d" % self.nbuf
        return Buf(self.nc.alloc_sbuf_tensor(name, list(shape), dt), name)

    def ps(self, shape, dt=F32, name=None):
        self.nbuf += 1
        name = name or "ps%d" % self.nbuf
        return Buf(self.nc.alloc_psum_tensor(name, list(shape), dt), name)

    def dram(self, name, shape, dt=F32, kind="Internal"):
        return Buf(self.nc.dram_tensor(name, list(shape), dt, kind=kind), name)

    def make_arena(self, nwords):
        self.arena = self.nc.alloc_sbuf_tensor("arena", [128, nwords], F32)
        self.awords = nwords

    def mark(self):
        return self.aoff

    def release(self, mark):
        keep = []
        for s, e, b in self.alive:
            if s >= mark:
                self.retired.append((s, e, _compress(b.all_deps())))
            else:
                keep.append((s, e, b))
        self.alive = keep
        self.aoff = mark
        if len(self.retired) > 64:
            alld = []
            lo = min(r[0] for r in self.retired)
            hi = max(r[1] for r in self.retired)
            for r in self.retired:
                alld.extend(r[2])
            self.retired = [(lo, hi, _compress(alld))]

    def carve(self, shape, dt=F32, name=None):
        esz = 4 if dt in (F32, I32) else 2
        nfree = int(np.prod(shape[1:]))
        nwords = (nfree * esz + 3) // 4
        nwords = (nwords + 7) // 8 * 8
        s = self.aoff
        e = s + nwords
        assert e <= self.awords, "arena overflow %d > %d (%s)" % (e, self.awords, name)
        self.aoff = e
        v = self.arena[0:shape[0], s:s + (nfree * esz + 3) // 4]
        if esz == 2:
            v = v.bitcast(BF16)
            v = v[:, 0:nfree]
        elif dt == I32:
            v = v.bitcast(I32)
        if len(shape) == 3:
            v = v.rearrange("p (a b) -> p a b", a=shape[1])
        elif len(shape) == 4:
            v = v.rearrange("p (a b c) -> p a b c", a=shape[1], b=shape[2])
        self.nbuf += 1
        b = Buf(v, name or "cv%d" % self.nbuf)
        inh = []
        for rs, re, rd in self.retired:
            if rs < e and re > s:
                inh.extend(rd)
        if inh:
            b.tr[None] = [_compress(inh), []]
        self.alive.append((s, e, b))
        return b

    def fence(self, b):
        d = _compress(b.all_deps())
        b.tr = {None: [d, []]}

    def _collect(self, reads, writes):
        deps = []
        for b, k in reads:
            for ent in b._entries(k):
                deps.extend(ent[0])
        for b, k in writes:
            for ent in b._entries(k):
                deps.extend(ent[0])
                deps.extend(ent[1])
        return deps

    def _record(self, reads, writes, tok):
        for b, k in reads:
            if k not in b.tr:
                b.tr[k] = [[], []]
            b.tr[k][1].append(tok)
            if len(b.tr[k][1]) > 48:
                b.tr[k][1] = _compress(b.tr[k][1])
        for b, k in writes:
            if k is None:
                b.tr = {None: [[tok], []]}
            else:
                b.tr[k] = [[tok], []]

    @staticmethod
    def _norm(lst):
        out = []
        for x in lst:
            if isinstance(x, Buf):
                out.append((x, None))
            else:
                out.append(x)
        return out

    def _waits(self, eng, deps):
        need = {}
        for s, v in deps:
            if eng == "tensor" and s == ("e", "tensor"):
                continue
            if self.waited[eng].get(s, 0) >= v:
                continue
            if need.get(s, 0) < v:
                need[s] = v
        for s, v in need.items():
            self.waited[eng][s] = v
        return list(need.items())

    def _sem(self, s):
        return self.esem[s[1]] if s[0] == "e" else self.dsem[s[1]]

    def op(self, eng, fn, reads=(), writes=()):
        reads = self._norm(reads)
        writes = self._norm(writes)
        deps = self._collect(reads, writes)
        waits = self._waits(eng, deps)
        self.cnt[eng] += 1
        tok = (("e", eng), self.cnt[eng])
        self.ops[eng].append((waits, _freeze(fn), ("e", eng), 1))
        self._record(reads, writes, tok)
        return tok

    def dma(self, out, in_, reads=(), writes=(), eng="sync", is_output=False):
        reads = self._norm(reads)
        writes = self._norm(writes)
        deps = self._collect(reads, writes)
        i = self.dnext
        self.dnext = (self.dnext + 1) % N_DMA_SEMS
        if self.dcnt[i] > 0:
            deps.append((("d", i), 16 * self.dcnt[i]))
        waits = self._waits(eng, deps)
        self.dcnt[i] += 1
        tok = (("d", i), 16 * self.dcnt[i])
        fn = lambda e, o=out, a=in_: e.dma_start(out=o, in_=a)
        self.ops[eng].append((waits, fn, ("d", i), 16))
        self._record(reads, writes, tok)
        if is_output:
            self.out_deps.append(tok)
        return tok

    def finish(self):
        nc = self.nc
        final_deps = list(self.out_deps)
        for e in ENGS:
            if self.cnt[e] > 0 and e != "sync":
                final_deps.append((("e", e), self.cnt[e]))
        for i in range(N_DMA_SEMS):
            if self.dcnt[i] > 0:
                final_deps.append((("d", i), 16 * self.dcnt[i]))
        fwaits = self._waits("sync", final_deps)
        ops = self.ops
        sem = self._sem
        with nc.Block() as block:

            def mk(engname):
                def body(e):
                    for waits, fn, s, inc in ops[engname]:
                        for ws, wv in waits:
                            e.wait_ge(sem(ws), wv)
                        ins = fn(e)
                        ins.then_inc(sem(s), inc)
                    if engname == "sync":
                        for ws, wv in fwaits:
                            e.wait_ge(sem(ws), wv)

                return body

            block.sync(mk("sync"))
            block.scalar(mk("scalar"))
            block.vector(mk("vector"))
            block.gpsimd(mk("gpsimd"))
            block.tensor(mk("tensor"))
        return nc


def _mask_tile(W, d):
    m = np.full((128, 512), NEGB, np.float32)
    s = np.arange(128)[:, None]
    t = np.arange(128)[None, :]
    for c in range(4):
        delta = c - d
        blk = m[:, c * 128:(c + 1) * 128]
        if delta < 0 or delta > W:
            continue
        if delta == 0:
            blk[:] = np.where(s <= t, 0.0, NEGB)
        elif delta < W:
            blk[:] = 0.0
        else:
            blk[:] = np.where(s > t, 0.0, NEGB)
    return m


SWA_MASK0 = 0
NSA_MASK0 = 5
N_MASKS = 13


def build_consts():
    c = {}
    masks = [_mask_tile(1, d) for d in range(-1, 4)] + [_mask_tile(4, d) for d in range(-4, 4)]
    c["c_masks"] = np.stack(masks).transpose(1, 0, 2).astype(ml_dtypes.bfloat16)
    c["c_identb"] = np.eye(128, dtype=np.float32).astype(ml_dtypes.bfloat16)
    c["c_identf"] = np.eye(128, dtype=np.float32)
    half = 32
    inv = (150000.0 ** (-np.arange(half, dtype=np.float32) / half)).astype(np.float32)
    rc = np.zeros((64, 2), np.float32)
    rc[:, 0] = np.concatenate([inv, inv])
    rc[:, 1] = np.concatenate([-np.ones(32), np.ones(32)])
    c["c_rope"] = rc
    i_ = np.arange(128)[:, None]
    j_ = np.arange(128)[None, :]
    ssd = np.zeros((128, 5, 128), np.float32)
    ssd[:, 4] = (i_ < j_)
    ssd[:, 0] = (i_ <= j_)
    ssd[:, 1] = (i_ > j_)
    ssd[:, 2] = np.where(j_ >= i_, 0.0, NEGB)
    ssd[:, 3] = 1.0
    c["c_ssd"] = ssd
    rw = np.zeros((128, 642), np.float32)
    rw[0:64, 0:64] = 1.0
    rw[64:128, 64:128] = 1.0
    rw[0:64, 128] = 1.0
    rw[64:128, 129] = 1.0
    cmk = np.ones((512,), np.float32)
    cmk[::128] = 0.0
    rw[:, 130:642] = cmk[None, :]
    c["c_rw"] = rw
    n_ = np.arange(128)[:, None]
    t_ = np.arange(T)[None, :]
    c["c_cmpmask"] = np.where((16 * n_ + 31 <= t_) & (n_ < 127), 0.0, NEGB).astype(np.float32).astype(ml_dtypes.bfloat16)
    E = np.zeros((32, 16, 128), np.float32)
    for k in range(16):
        for s_ in range(128):
            E[2 * k + s_ // 64, k, s_] = 1.0
    c["c_E"] = E.astype(ml_dtypes.bfloat16)
    sel = np.zeros((24, 24, 128), np.float32)
    for i in range(24):
        sel[i, i, :] = 1.0
    c["c_sel"] = sel.astype(ml_dtypes.bfloat16)
    jj = np.arange(32)[None, :]
    cs = (16 * np.arange(128))[:, None]
    ovl = ((cs < 64 * jj + 64) & (cs + 32 > 64 * jj)).astype(np.float32)
    ovl[127, :] = 0.0
    c["c_ovl"] = ovl
    tt = np.arange(T)[:, None]
    blk = tt // 64
    valid = jj <= blk
    forced = (jj == 0) | (jj == blk)
    vmul = (valid & ~forced).astype(np.float32)
    amask = np.where(forced, 1e30, np.where(valid, 0.0, -1e30)).astype(np.float32)
    c["c_vmul"] = np.ascontiguousarray(vmul.reshape(16, 128, 32).transpose(1, 0, 2))
    c["c_amask"] = np.ascontiguousarray(amask.reshape(16, 128, 32).transpose(1, 0, 2))
    return c


CONST_SPECS = {
    "c_masks": ([128, N_MASKS, 512], BF16),
    "c_identb": ([128, 128], BF16),
    "c_identf": ([128, 128], F32),
    "c_rope": ([64, 2], F32),
    "c_ssd": ([128, 5, 128], F32),
    "c_rw": ([128, 642], F32),
    "c_cmpmask": ([128, T], BF16),
    "c_E": ([32, 16, 128], BF16),
    "c_sel": ([24, 24, 128], BF16),
    "c_ovl": ([128, 32], F32),
    "c_vmul": ([128, 16, 32], F32),
    "c_amask": ([128, 16, 32], F32),
}

ROW_NORM_MIX = 0
ROW_NORM_FFN = 1024
ROW_SINKS = 2048
ROW_DTB = 2056
ROW_ALOG = 2064
ROW_DSK = 2072
ROW_SSDN = 2080
ROW_LNW = 2592
ROW_LNB = 3104
ROW_PER_LAYER = 3616
COL_CONVW = 0
COL_CONVB = 32
COL_MU = 40
COL_W0 = 54
COL_A0 = 58
COL_KK = 62
COL_KA = 66
COL_RK = 70
COL_PER_LAYER = 80
ROW_FINAL = DEPTH * ROW_PER_LAYER
ROW_TOTAL = ROW_FINAL + 1024


def build_rows(inp):
    rows = np.zeros((ROW_TOTAL,), np.float32)
    for l in range(DEPTH):
        o = l * ROW_PER_LAYER
        rows[o + ROW_NORM_MIX:o + ROW_NORM_MIX + 1024] = inp["norm_mix"][l]
        rows[o + ROW_NORM_FFN:o + ROW_NORM_FFN + 1024] = inp["norm_ffn"][l]
        rows[o + ROW_SINKS:o + ROW_SINKS + 8] = inp["swa_sinks"][l]
        rows[o + ROW_DTB:o + ROW_DTB + 8] = inp["ssd_dt_bias"][l]
        rows[o + ROW_ALOG:o + ROW_ALOG + 8] = inp["ssd_a_log"][l]
        rows[o + ROW_DSK:o + ROW_DSK + 8] = inp["ssd_d"][l]
        rows[o + ROW_SSDN:o + ROW_SSDN + 512] = inp["ssd_norm"][l]
        rows[o + ROW_LNW:o + ROW_LNW + 512] = inp["rwkv_ln_w"][l]
        rows[o + ROW_LNB:o + ROW_LNB + 512] = inp["rwkv_ln_b"][l]
    rows[ROW_FINAL:ROW_FINAL + 1024] = inp["norm_final"]
    return np.ascontiguousarray(np.broadcast_to(rows[None, :], (128, ROW_TOTAL)))


def build_cols(inp):
    cols = np.zeros((128, DEPTH * COL_PER_LAYER), np.float32)
    for l in range(DEPTH):
        o = l * COL_PER_LAYER
        cw = np.asarray(inp["ssd_conv_w"][l])
        cols[:, o + COL_CONVW:o + COL_CONVW + 32] = cw.reshape(4, 8, 128).transpose(2, 1, 0).reshape(128, 32)
        cols[:, o + COL_CONVB:o + COL_CONVB + 8] = np.asarray(inp["ssd_conv_b"][l]).reshape(8, 128).T
        cols[:, o + COL_MU:o + COL_MU + 14] = np.asarray(inp["rwkv_mu"][l]).reshape(14, 128).T
        for nm, co in (("rwkv_w0", COL_W0), ("rwkv_a0", COL_A0), ("rwkv_k_k", COL_KK), ("rwkv_k_a", COL_KA), ("rwkv_r_k", COL_RK)):
            cols[:, o + co:o + co + 4] = np.asarray(inp[nm][l]).reshape(4, 128).T
    return np.ascontiguousarray(cols)


WEIGHT_SPECS = {
    "w_in": [DEPTH, D, D_IN],
    "w_br_ssd": [DEPTH, 512, D],
    "w_br_nsa": [DEPTH, 512, D],
    "w_br_rwkv": [DEPTH, 512, D],
    "w_br_swa": [DEPTH, 512, D],
    "w_out": [DEPTH, D, D],
    "ffn_w_gate": [1, D, D_FF],
    "ffn_w_up": [1, D, D_FF],
    "ffn_w_down": [1, D_FF, D],
    "moe_router": [1, D, 8],
    "moe_w_gate": [1, 8, D, D_FF],
    "moe_w_up": [1, 8, D, D_FF],
    "moe_w_down": [1, 8, D_FF, D],
    "ple_proj": [DEPTH, 256, D],
    "ple_gate": [DEPTH, D, D],
    "nsa_cmp_w1": [DEPTH, 2, 32, 64, 64],
    "nsa_cmp_w2": [DEPTH, 2, 64, 64],
    "nsa_peT": [DEPTH, 2, 64, 32],
    "rwkv_w_up": [DEPTH, 64, 512],
    "rwkv_a_up": [DEPTH, 64, 512],
    "rwkv_g_up": [DEPTH, 128, 512],
}


def build(n_layers=DEPTH, mixers=("ssd", "nsa", "rwkv", "swa"), debug=False):
    p = Prog()
    nc = p.nc
    EI = "ExternalInput"
    x_in = p.dram("x", [T, D], F32, EI)
    p_in = p.dram("p", [DEPTH, T, 256], F32, EI)
    pos_in = p.dram("pos", [64, T], I32, EI)
    rows_in = p.dram("rows", [128, ROW_TOTAL], F32, EI)
    cols_in = p.dram("cols", [128, DEPTH * COL_PER_LAYER], F32, EI)
    W = {k: p.dram(k, shp, F32, EI) for k, shp in WEIGHT_SPECS.items()}
    C = {k: p.dram(k, shp, dt, EI) for k, (shp, dt) in CONST_SPECS.items()}
    out_d = p.dram("out", [T, D], F32, "ExternalOutput")
    xs = p.dram("xs", [T, D], F32)
    yT_d = [p.dram("yT%d" % m, [4, 128, T], BF16) for m in range(4)]
    dbg = {}
    if debug:
        for m in range(4):
            dbg["yT%d" % m] = p.dram("dbg_yT%d" % m, [4, 128, T], BF16, "ExternalOutput")
        dbg["x_mix"] = p.dram("dbg_x_mix", [T, D], F32, "ExternalOutput")
        dbg["x_ffn"] = p.dram("dbg_x_ffn", [T, D], F32, "ExternalOutput")
        dbg["x_l0"] = p.dram("dbg_x_l0", [T, D], F32, "ExternalOutput")

    def dump(name, ap, shape, dt, reads):
        if not debug:
            return
        d = p.dram("dbg_" + name, shape, dt, "ExternalOutput")
        p.dma(d.t.ap() if False else d[tuple(slice(None) for _ in shape)], ap, reads=reads, writes=[d], is_output=True)

    masks = p.sb([128, N_MASKS, 512], BF16, "masks")
    identb = p.sb([128, 128], BF16, "identb")
    identf = p.sb([128, 128], F32, "identf")
    ropec = p.sb([64, 2], F32, "ropec")
    rows = p.sb([128, ROW_PER_LAYER], F32, "rows_sb")
    rowsf = p.sb([128, 1024], F32, "rowsf_sb")
    hT = p.sb([128, 8, T], BF16, "hT")
    p.dma(masks[:], C["c_masks"][:], writes=[masks])
    p.dma(identb[:], C["c_identb"][:], writes=[identb])
    p.dma(identf[:], C["c_identf"][:], writes=[identf])
    p.dma(ropec[:], C["c_rope"][:], writes=[ropec])
    p.dma(rowsf[:], rows_in[:, ROW_FINAL:ROW_FINAL + 1024], writes=[rowsf])
    cols = p.sb([128, DEPTH * COL_PER_LAYER], F32, "cols_sb")
    p.dma(cols[:], cols_in[:], writes=[cols])
    cssd = p.sb([128, 5, 128], F32, "cssd")
    p.dma(cssd[:], C["c_ssd"][:], writes=[cssd])
    epsc = p.sb([128, 1], F32, "epsc")
    p.op("vector", lambda e: e.memset(epsc[:], 1e-6), writes=[epsc])

    NSTG = 2
    STGW = 2048
    stg = [p.sb([128, STGW], F32, "stg%d" % i) for i in range(NSTG)]
    stgi = [0]

    p.make_arena((nc.sbuf_bytes_remaining - 2048) // 4)

    psb = [p.ps([128, 512], F32, "psb%d" % i) for i in range(6)]
    pst = [p.ps([128, 1024], BF16, "pst%d" % i) for i in range(2)]
    psi = [0]
    psti = [0]

    pli = [0]

    def nps():
        b = psb[psi[0] % 4]
        psi[0] += 1
        return b

    def npl():
        b = psb[4 + pli[0] % 2]
        pli[0] += 1
        return b

    def npst():
        b = pst[psti[0] % 2]
        psti[0] += 1
        return b

    cast_rr = [0]

    def load_w(dst, dst_ap_fn, src_ap, shape, key=None, parts=128):
        a, b = shape
        assert a * b <= STGW, (a, b)
        s = stg[stgi[0] % NSTG]
        stgi[0] += 1
        sv = s[0:parts, 0:a * b].rearrange("p (a b) -> p a b", a=a)
        p.dma(sv, src_ap, writes=[s])
        for ent in dst_ap_fn(sv):
            if len(ent) == 3:
                dbuf, o_ap, i_ap = ent
            else:
                dbuf = dst
                o_ap, i_ap = ent
            eng = "gpsimd"
            p.op(eng, lambda e, o=o_ap, i=i_ap: e.tensor_copy(o, i), reads=[s], writes=[(dbuf, key)])

    def load_w_plain(dst, dview, src_ap, shape, key=None):
        a, b = shape
        bc = max(1, STGW // a)
        for b0 in range(0, b, bc):
            bw = min(bc, b - b0)
            load_w(dst, lambda sv, b0=b0, bw=bw: [(dview[:, :, b0:b0 + bw], sv)],
                   src_ap[:, :, b0:b0 + bw], (a, bw), key)

    def win_cols(l, c0, n):
        return W["w_in"][l, :, c0:c0 + n].rearrange("(c p) n -> p c n", p=128)

    def rmsnorm_to_hT(src_tile_fn, grow0, also=None):
        mk = p.mark()
        hb = [p.carve([128, D], BF16, "hb%d" % i) for i in range(2)]
        junk = p.carve([128, D], F32, "junk")
        st = [p.carve([128, 4], F32, "st%d" % i) for i in range(2)]
        for i in range(NT):
            xb, xap, xk = src_tile_fn(i)
            s_ = st[i % 2]
            h_ = hb[i % 2]
            p.op("scalar", lambda e, xap=xap, s_=s_: e.activation(junk[:], xap, AF.Square, accum_out=s_[:, 0:1]),
                 reads=[(xb, xk)], writes=[junk, s_])
            p.op("scalar", lambda e, s_=s_: e.activation(s_[:, 1:2], s_[:, 0:1], AF.Sqrt, bias=epsc[:, 0:1], scale=1.0 / D),
                 reads=[s_, epsc], writes=[s_])
            p.op("vector", lambda e, s_=s_: e.reciprocal(s_[:, 2:3], s_[:, 1:2]), reads=[s_], writes=[s_])
            p.op("vector", lambda e, xap=xap, s_=s_, h_=h_: e.scalar_tensor_tensor(
                out=h_[:], in0=xap, scalar=s_[:, 2:3], in1=rows[:, grow0:grow0 + D], op0=ALU.mult, op1=ALU.mult),
                 reads=[(xb, xk), s_, rows], writes=[h_])
            if also is not None:
                also(i, s_, xb, xap, xk)
            pt = npst()
            for c in range(8):
                p.op("tensor", lambda e, pt=pt, c=c, h_=h_: e.transpose(pt[:, c * 128:(c + 1) * 128], h_[:, c * 128:(c + 1) * 128], identb[:]),
                     reads=[h_, identb], writes=[pt])
            eng = "vector" if i % 2 == 0 else "scalar"
            dstv = hT[:, :, i * 128:(i + 1) * 128]
            srcv = pt[:, :].rearrange("p (c t) -> p c t", c=8)
            if eng == "vector":
                p.op("vector", lambda e, dstv=dstv, srcv=srcv: e.tensor_copy(dstv, srcv), reads=[pt], writes=[(hT, i)])
            else:
                p.op("scalar", lambda e, dstv=dstv, srcv=srcv: e.copy(dstv, srcv), reads=[pt], writes=[(hT, i)])
        p.release(mk)

    def hT_reads(t0, tw):
        return [(hT, i) for i in range(t0 // 128, (t0 + tw + 127) // 128)]

    def proj_fm(ps_ap, psbuf, wbuf, wview_fn, t0, tw, wkey=None):
        for c in range(8):
            p.op("tensor", lambda e, c=c: e.matmul(ps_ap, wview_fn(c), hT[:, c, t0:t0 + tw], start=(c == 0), stop=(c == 7)),
                 reads=[(wbuf, wkey)] + hT_reads(t0, tw), writes=[psbuf])

    def proj_tm(ps_ap, psbuf, wbuf, wview_fn, i, wkey=None):
        for c in range(8):
            p.op("tensor", lambda e, c=c: e.matmul(ps_ap, hT[:, c, i * 128:(i + 1) * 128], wview_fn(c), start=(c == 0), stop=(c == 7)),
                 reads=[(wbuf, wkey), (hT, i)], writes=[psbuf])

    def attn_group(qT, qkey, h, b, kT, g, vext, pairs, PT, den_extra_col, out_cb):
        ps2 = npl()
        n = len(pairs)

        def emit_S(idx):
            j, extra = pairs[idx]
            ps1 = nps()
            nx = len(extra)
            p.op("tensor", lambda e: e.matmul(ps1[:, :], kT[0:64, g, j * 128:(j + 1) * 128], qT[0:64, h, b * 512:(b + 1) * 512],
                                              start=True, stop=(nx == 0)),
                 reads=[(kT, None), (qT, qkey)], writes=[ps1])
            for xi, (la, ra, rd) in enumerate(extra):
                p.op("tensor", lambda e: e.matmul(ps1[:, :], la, ra, start=False, stop=(xi == nx - 1)), reads=rd, writes=[ps1])
            pt_ = PT[idx % len(PT)]
            p.op("scalar", lambda e: e.activation(pt_[:, :], ps1[:, :], AF.Exp, scale=0.125), reads=[ps1], writes=[pt_])

        def emit_PV(idx):
            j, extra = pairs[idx]
            pt_ = PT[idx % len(PT)]
            p.op("tensor", lambda e: e.matmul(ps2[:, :], vext[:, j, g, :], pt_[:, :], start=(idx == 0), stop=(idx == n - 1)),
                 reads=[pt_, (vext, None)], writes=[ps2])

        emit_S(0)
        if n > 1:
            emit_S(1)
        for idx in range(n):
            if idx + 2 < n:
                emit_S(idx + 2)
            emit_PV(idx)
        out_cb(ps2)

    def mixer_swa(l):
        mk = p.mark()
        qT = p.carve([64, 8, T], BF16, "swa_qT")
        kT = p.carve([64, 2, T], BF16, "swa_kT")
        vext = p.carve([128, NT, 2, 128], BF16, "swa_v")
        cosT = p.carve([64, T], F32, "cosT")
        sinT = p.carve([64, T], F32, "sinT")
        PT = [p.carve([128, 512], BF16, "PT%d" % i) for i in range(3)]
        sinke = p.carve([128, 8], F32, "sinke")
        mk2 = p.mark()
        posi = p.carve([64, T], I32, "posi")
        ang = p.carve([64, T], F32, "ang")
        tmpf = p.carve([64, T], F32, "tmpf")
        tmpi = p.carve([64, T], I32, "tmpi")
        p.dma(posi[:], pos_in[:], writes=[posi])
        p.op("vector", lambda e: e.tensor_copy(ang[:], posi[:]), reads=[posi], writes=[ang])
        p.op("vector", lambda e: e.tensor_scalar(ang[:], ang[:], ropec[:, 0:1], None, ALU.mult), reads=[ang, ropec], writes=[ang])
        TWO_PI = 2.0 * np.pi
        for shift, dst in ((0.0, sinT), (np.pi / 2, cosT)):
            p.op("vector", lambda e, shift=shift: e.tensor_scalar(tmpf[:], ang[:], float(shift), 1.0 / TWO_PI, ALU.add, ALU.mult),
                 reads=[ang], writes=[tmpf])
            p.op("vector", lambda e: e.tensor_copy(tmpi[:], tmpf[:]), reads=[tmpf], writes=[tmpi])
            p.op("vector", lambda e: e.tensor_copy(tmpf[:], tmpi[:]), reads=[tmpi], writes=[tmpf])
            p.op("vector", lambda e: e.scalar_tensor_tensor(out=tmpf[:], in0=tmpf[:], scalar=-TWO_PI, in1=ang[:], op0=ALU.mult, op1=ALU.add),
                 reads=[tmpf, ang], writes=[tmpf])
            p.op("vector", lambda e, shift=shift: e.tensor_scalar(tmpf[:], tmpf[:], float(shift), 3.14159, ALU.add, ALU.min),
                 reads=[tmpf], writes=[tmpf])
            p.op("vector", lambda e: e.tensor_scalar(tmpf[:], tmpf[:], -3.14159, None, ALU.max), reads=[tmpf], writes=[tmpf])
            p.op("scalar", lambda e, dst=dst: e.activation(dst[:], tmpf[:], AF.Sin), reads=[tmpf], writes=[dst])
        p.op("vector", lambda e: e.tensor_scalar(sinT[:], sinT[:], ropec[:, 1:2], None, ALU.mult), reads=[sinT, ropec], writes=[sinT])
        p.release(mk2)
        ro = 0 + ROW_SINKS
        p.op("scalar", lambda e: e.activation(sinke[:], rows[:, ro:ro + 8], AF.Exp), reads=[rows], writes=[sinke])
        wq = p.carve([128, 8, 512], BF16, "swa_wq")
        wqs = p.carve([128, 8, 512], BF16, "swa_wqs")
        wkv = p.carve([128, 8, 256], BF16, "swa_wkv")
        wks = p.carve([128, 8, 128], BF16, "swa_wks")
        for h0 in range(0, 8, 4):
            def cast_q(sv, h0=h0):
                s4 = sv.rearrange("p c (h d) -> p c h d", d=64)
                ops_ = [(wq[:, :, h0 * 64:(h0 + 4) * 64], sv)]
                d4 = wqs[:, :, h0 * 64:(h0 + 4) * 64].rearrange("p c (h d) -> p c h d", d=64)
                ops_.append((wqs, d4[:, :, :, 0:32], s4[:, :, :, 32:64]))
                ops_.append((wqs, d4[:, :, :, 32:64], s4[:, :, :, 0:32]))
                return ops_
            load_w(wq, cast_q, win_cols(l, OFF_SQ + h0 * 64, 256), (8, 256))
        p.fence(wq)

        def cast_kv(sv):
            ops_ = [(wkv[:, :, :], sv)]
            s4 = sv[:, :, 0:128].rearrange("p c (h d) -> p c h d", d=64)
            d4 = wks[:, :, :].rearrange("p c (h d) -> p c h d", d=64)
            ops_.append((wks, d4[:, :, :, 0:32], s4[:, :, :, 32:64]))
            ops_.append((wks, d4[:, :, :, 32:64], s4[:, :, :, 0:32]))
            return ops_
        load_w(wkv, cast_kv, win_cols(l, OFF_SKV, 256), (8, 256))
        t1 = [p.carve([64, 512], F32, "rt1_%d" % i) for i in range(2)]
        t2 = [p.carve([64, 512], F32, "rt2_%d" % i) for i in range(2)]
        cnt = 0
        for dstT, nh, wa, wb, wbufa, wbufb in ((kT, 2, wkv, wks, wkv, wks), (qT, 8, wq, wqs, wq, wqs)):
            for h in range(nh):
                for b in range(4):
                    pa = nps()
                    pb = nps()
                    proj_fm(pa[0:64, :], pa, wbufa, lambda c, wa=wa, h=h: wa[:, c, h * 64:(h + 1) * 64], b * 512, 512)
                    proj_fm(pb[0:64, :], pb, wbufb, lambda c, wb=wb, h=h: wb[:, c, h * 64:(h + 1) * 64], b * 512, 512)
                    a_ = t1[cnt % 2]
                    b_ = t2[cnt % 2]
                    cnt += 1
                    p.op("vector", lambda e, a_=a_, pa=pa, b=b: e.tensor_tensor(a_[:, :], pa[0:64, :], cosT[:, b * 512:(b + 1) * 512], ALU.mult),
                         reads=[pa, cosT], writes=[a_])
                    p.op("vector", lambda e, b_=b_, pb=pb, b=b: e.tensor_tensor(b_[:, :], pb[0:64, :], sinT[:, b * 512:(b + 1) * 512], ALU.mult),
                         reads=[pb, sinT], writes=[b_])
                    p.op("gpsimd", lambda e, a_=a_, b_=b_, dstT=dstT, h=h, b=b: e.tensor_tensor(dstT[:, h, b * 512:(b + 1) * 512], a_[:, :], b_[:, :], ALU.add),
                         reads=[a_, b_], writes=[(dstT, (h, b))])
        p.fence(kT)
        p.op("gpsimd", lambda e: e.memset(vext[:, :, :, 64:128], 1.0), writes=[vext])
        for i in range(NT):
            pv = nps()
            proj_tm(pv[:, 0:128], pv, wkv, lambda c: wkv[:, c, 128:256], i)
            p.op("vector", lambda e, pv=pv, i=i: e.tensor_copy(vext[:, i, :, 0:64], pv[:, 0:128].rearrange("p (g d) -> p g d", g=2)),
                 reads=[pv], writes=[(vext, i)])
        p.fence(vext)
        rec = [p.carve([128, 512], F32, "rec%d" % i) for i in range(2)]
        yo = [p.carve([64, 512], BF16, "yo%d" % i) for i in range(2)]
        cnt = 0
        for h in range(8):
            g = h // 4
            for b in range(4):
                pairs = []
                for j in range(4 * b - 1, 4 * b + 4):
                    if j < 0:
                        continue
                    d = j - 4 * b
                    pairs.append((j, [(identb[:, :], masks[:, SWA_MASK0 + d + 1, :], [identb, masks])]))
                r_ = rec[cnt % 2]
                y_ = yo[cnt % 2]
                cnt += 1

                def fin(ps2, r_=r_, y_=y_, h=h, b=b):
                    p.op("vector", lambda e: e.tensor_scalar(r_[64:128, :], ps2[64:128, :], sinke[64:128, h:h + 1], None, ALU.add),
                         reads=[ps2, sinke], writes=[r_])
                    p.op("vector", lambda e: e.reciprocal(r_[64:128, :], r_[64:128, :]), reads=[r_], writes=[r_])
                    p.op("vector", lambda e: e.tensor_tensor(y_[:, :], ps2[0:64, :], r_[64:128, :], ALU.mult), reads=[ps2, r_], writes=[y_])
                    p.dma(yT_d[3][h // 2, (h % 2) * 64:(h % 2) * 64 + 64, b * 512:(b + 1) * 512], y_[:, :], reads=[y_], writes=[(yT_d[3], (h, b))])
                attn_group(qT, (h, b), h, b, kT, g, vext, pairs, PT, None, fin)
        p.fence(yT_d[3])
        p.release(mk)


    def mixer_nsa(l):
        mk = p.mark()
        cmpmask = p.carve([128, T], BF16, "cmpmask")
        Emat = p.carve([32, 16, 128], BF16, "Emat")
        Sel = p.carve([24, 24, 128], BF16, "Sel")
        ovl = p.carve([128, 32], F32, "ovl")
        vmul = p.carve([128, 16, 32], F32, "vmul")
        amask = p.carve([128, 16, 32], F32, "amask")
        p.dma(cmpmask[:, :], C["c_cmpmask"][:, :], writes=[cmpmask])
        p.dma(Emat[:, :, :], C["c_E"][:, :, :], writes=[Emat])
        p.dma(Sel[:, :, :], C["c_sel"][:, :, :], writes=[Sel])
        p.dma(ovl[:, :], C["c_ovl"][:, :], writes=[ovl])
        p.dma(vmul[:, :, :], C["c_vmul"][:, :, :], writes=[vmul])
        p.dma(amask[:, :, :], C["c_amask"][:, :, :], writes=[amask])
        gT = p.carve([24, T], BF16, "nsa_gT")
        wg = p.carve([128, 8, 24], BF16, "nsa_wg")
        load_w(wg, lambda sv: [(wg[:, :, :], sv)], win_cols(l, OFF_NG, 24), (8, 24))
        for b in range(4):
            pg = nps()
            proj_fm(pg[0:24, :], pg, wg, lambda c: wg[:, c, :], b * 512, 512)
            p.op("scalar", lambda e, pg=pg, b=b: e.activation(gT[:, b * 512:(b + 1) * 512], pg[0:24, :], AF.Sigmoid), reads=[pg], writes=[(gT, b)])
        p.fence(gT)
        w1 = [p.carve([64, 32, 64], BF16, "cw1_%d" % i) for i in range(2)]
        w2 = [p.carve([64, 64], BF16, "cw2_%d" % i) for i in range(2)]
        peT = [p.carve([64, 32], BF16, "cpe_%d" % i) for i in range(2)]
        for kv in range(2):
            load_w(w1[kv], lambda sv, kv=kv: [(w1[kv][:, :, :], sv[0:64, :, :])],
                   W["nsa_cmp_w1"][l, kv].rearrange("l d f -> d l f"), (32, 64), parts=64)
            load_w(w2[kv], lambda sv, kv=kv: [(w2[kv][:, :], sv[0:64, 0, :])], W["nsa_cmp_w2"][l, kv].unsqueeze(1), (1, 64), parts=64)
            load_w(peT[kv], lambda sv, kv=kv: [(peT[kv][:, :], sv[0:64, 0, :])], W["nsa_peT"][l, kv].unsqueeze(1), (1, 32), parts=64)
        qT = p.carve([64, 4, T], BF16, "nsa_qT")
        ycmp = p.carve([64, 4, T], BF16, "nsa_ycmp")
        ksT = p.carve([64, 1, T], BF16, "nsa_ksT")
        kwT = p.carve([64, 1, T], BF16, "nsa_kwT")
        vs = p.carve([128, NT, 1, 128], BF16, "nsa_vs")
        vw = p.carve([128, NT, 1, 128], BF16, "nsa_vw")
        impT = p.carve([32, T], F32, "nsa_impT")
        selbT = p.carve([32, 1, T], BF16, "nsa_selbT")
        kcmpT = p.carve([64, 1, 128], BF16, "nsa_kcmpT")
        vcmp = p.carve([128, 64], BF16, "nsa_vcmp")
        for g in range(2):
            mk_g = p.mark()
            kcvT = p.carve([64, 2, T], BF16, "nsa_kcvT")
            hidb = [p.carve([64, 128], BF16, "nsa_hid%d" % i) for i in range(2)]
            wq = p.carve([128, 8, 256], BF16, "nsa_wq")
            wkv = p.carve([128, 8, 6, 64], BF16, "nsa_wkv")
            load_w(wq, lambda sv: [(wq[:, :, :], sv)], win_cols(l, OFF_NQ + g * 256, 256), (8, 256))
            for part in range(6):
                load_w(wkv, lambda sv, part=part: [(wkv[:, :, part, :], sv)], win_cols(l, OFF_NKV + part * 128 + g * 64, 64), (8, 64), key=part)
            p.fence(wkv)
            cnt = 0
            for hh in range(4):
                for b in range(4):
                    pa = nps()
                    proj_fm(pa[0:64, :], pa, wq, lambda c, hh=hh: wq[:, c, hh * 64:(hh + 1) * 64], b * 512, 512)
                    eng = "vector" if cnt % 2 == 0 else "scalar"
                    cnt += 1
                    if eng == "vector":
                        p.op("vector", lambda e, pa=pa, hh=hh, b=b: e.tensor_copy(qT[:, hh, b * 512:(b + 1) * 512], pa[0:64, :]), reads=[pa], writes=[(qT, (hh, b))])
                    else:
                        p.op("scalar", lambda e, pa=pa, hh=hh, b=b: e.copy(qT[:, hh, b * 512:(b + 1) * 512], pa[0:64, :]), reads=[pa], writes=[(qT, (hh, b))])
            for part, dstT, di in ((0, kcvT, 0), (1, kcvT, 1), (2, ksT, 0), (4, kwT, 0)):
                for b in range(4):
                    pa = nps()
                    proj_fm(pa[0:64, :], pa, wkv, lambda c, part=part: wkv[:, c, part, :], b * 512, 512)
                    p.op("vector", lambda e, pa=pa, dstT=dstT, di=di, b=b: e.tensor_copy(dstT[:, di, b * 512:(b + 1) * 512], pa[0:64, :]), reads=[pa], writes=[(dstT, (di, b))])
            p.fence(kcvT)
            p.fence(ksT)
            p.fence(kwT)
            for part, vdst in ((3, vs), (5, vw)):
                p.op("gpsimd", lambda e, vdst=vdst: e.memset(vdst[:, :, :, 64:128], 1.0), writes=[vdst])
                for i in range(NT):
                    pv = nps()
                    proj_tm(pv[:, 0:64], pv, wkv, lambda c, part=part: wkv[:, c, part, :], i)
                    p.op("vector", lambda e, pv=pv, i=i, vdst=vdst: e.tensor_copy(vdst[:, i, 0, 0:64], pv[:, 0:64]), reads=[pv], writes=[(vdst, i)])
                p.fence(vdst)
            p.op("gpsimd", lambda e: e.memset(kcmpT[:, :, :], 0.0), writes=[kcmpT])
            p.op("gpsimd", lambda e: e.memset(vcmp[:, :], 0.0), writes=[vcmp])
            for kv in range(2):
                ph = nps()
                src3 = kcvT[:, kv, :].rearrange("p (n s) -> p n s", s=16)
                for ll in range(32):
                    rhs = src3[:, 0:127, ll] if ll < 16 else src3[:, 1:128, ll - 16]
                    p.op("tensor", lambda e, ph=ph, ll=ll, rhs=rhs, kv=kv: e.matmul(ph[0:64, 0:127], w1[kv][:, ll, :], rhs, start=(ll == 0), stop=False),
                         reads=[w1[kv], kcvT], writes=[ph])
                    p.op("tensor", lambda e, ph=ph, ll=ll, kv=kv: e.matmul(ph[0:64, 0:127], w1[kv][:, ll, :], peT[kv][:, ll:ll + 1].to_broadcast([64, 127]), start=False, stop=(ll == 31)),
                         reads=[w1[kv], peT[kv]], writes=[ph])
                hb = hidb[kv]
                p.op("scalar", lambda e, hb=hb, ph=ph: e.activation(hb[:, 0:127], ph[0:64, 0:127], AF.Silu), reads=[ph], writes=[hb])
                pc = nps()
                if kv == 0:
                    p.op("tensor", lambda e, pc=pc, hb=hb: e.matmul(pc[0:64, 0:127], w2[0][:, :], hb[:, 0:127], start=True, stop=True), reads=[w2[0], hb], writes=[pc])
                    p.op("vector", lambda e, pc=pc: e.tensor_copy(kcmpT[:, 0, 0:127], pc[0:64, 0:127]), reads=[pc], writes=[kcmpT])
                else:
                    p.op("tensor", lambda e, pc=pc, hb=hb: e.matmul(pc[0:127, 0:64], hb[:, 0:127], w2[1][:, :], start=True, stop=True), reads=[w2[1], hb], writes=[pc])
                    p.op("vector", lambda e, pc=pc: e.tensor_copy(vcmp[0:127, :], pc[0:127, 0:64]), reads=[pc], writes=[vcmp])
            p.release(mk_g)
            PT = [p.carve([128, 512], BF16, "nPT%d" % i) for i in range(3)]
            PTf = [p.carve([128, 512], F32, "nPTf%d" % i) for i in range(2)]
            recd = p.carve([128, 512], F32, "nrecd")
            pn = p.carve([128, 512], F32, "npn")
            pnb = p.carve([128, 512], BF16, "npnb")
            gs = [p.carve([128, 512], F32, "ngs%d" % i) for i in range(2)]
            rec = [p.carve([128, 512], F32, "nrec%d" % i) for i in range(2)]
            tsel = [p.carve([64, 512], F32, "ntsel%d" % i) for i in range(2)]
            yo = [p.carve([64, 512], BF16, "nyo%d" % i) for i in range(2)]
            sc = [p.carve([128, 32], F32, "nsc%d" % i) for i in range(2)]
            m8 = [p.carve([128, 8], F32, "nm8%d" % i) for i in range(2)]
            psimp = psb[4]
            recd2 = [recd, p.carve([128, 512], F32, "nrecd2")]
            pn2 = [pn, p.carve([128, 512], F32, "npn2")]
            pnb2 = [pnb, p.carve([128, 512], BF16, "npnb2")]
            units = [(b, hh) for b in range(4) for hh in range(4)]

            def cmpA(i):
                b, hh = units[i]
                bs = slice(b * 512, (b + 1) * 512)
                ps1 = nps()
                p.op("tensor", lambda e: e.matmul(ps1[:, :], kcmpT[:, 0, :], qT[:, hh, bs], start=True, stop=False),
                     reads=[kcmpT, (qT, (hh, b))], writes=[ps1])
                p.op("tensor", lambda e: e.matmul(ps1[:, :], identb[:, :], cmpmask[:, bs], start=False, stop=True),
                     reads=[identb, cmpmask], writes=[ps1])
                ptf = PTf[i % 2]
                p.op("scalar", lambda e: e.activation(ptf[:, :], ps1[:, :], AF.Exp, scale=0.125), reads=[ps1], writes=[ptf])
                psd = nps()
                p.op("tensor", lambda e: e.matmul(psd[:, :], cssd[:, 3, :], ptf[:, :], start=True, stop=True), reads=[cssd, ptf], writes=[psd])
                rd, pn_, pnb_ = recd2[i % 2], pn2[i % 2], pnb2[i % 2]
                p.op("vector", lambda e: e.tensor_scalar(rd[:, :], psd[:, :], 1e-30, None, ALU.max), reads=[psd], writes=[rd])
                p.op("vector", lambda e: e.reciprocal(rd[:, :], rd[:, :]), reads=[rd], writes=[rd])
                p.op("vector", lambda e: e.tensor_tensor(pn_[:, :], ptf[:, :], rd[:, :], ALU.mult), reads=[ptf, rd], writes=[pn_])
                p.op("gpsimd", lambda e: e.tensor_copy(pnb_[:, :], pn_[:, :]), reads=[pn_], writes=[pnb_])

            def cmpB(i):
                b, hh = units[i]
                h = g * 4 + hh
                bs = slice(b * 512, (b + 1) * 512)
                pn_, pnb_ = pn2[i % 2], pnb2[i % 2]
                p.op("tensor", lambda e: e.matmul(psimp[0:32, :], ovl[:, :], pn_[:, :], start=(hh == 0), stop=(hh == 3)), reads=[ovl, pn_], writes=[psimp])
                pso = nps()
                p.op("tensor", lambda e: e.matmul(pso[0:64, :], vcmp[:, :], pnb_[:, :], start=True, stop=True), reads=[vcmp, pnb_], writes=[pso])
                pgb = nps()
                p.op("tensor", lambda e: e.matmul(pgb[:, :], Sel[:, 0 * 8 + h, :], gT[:, bs], start=True, stop=True), reads=[Sel, gT], writes=[pgb])
                gs_ = gs[hh % 2]
                p.op("scalar", lambda e: e.copy(gs_[0:64, :], pgb[0:64, :]), reads=[pgb], writes=[gs_])
                p.op("vector", lambda e: e.tensor_tensor(ycmp[:, hh, bs], pso[0:64, :], gs_[0:64, :], ALU.mult),
                     reads=[pso, gs_], writes=[(ycmp, (hh, b))])
                if hh == 3:
                    p.op("scalar", lambda e: e.copy(impT[:, bs], psimp[0:32, :]), reads=[psimp], writes=[(impT, b)])

            cmpA(0)
            for i in range(len(units)):
                if i + 1 < len(units):
                    cmpA(i + 1)
                cmpB(i)
            p.fence(impT)
            for i in range(NT):
                ts_ = slice(i * 128, (i + 1) * 128)
                pt1 = nps()
                p.op("tensor", lambda e, pt1=pt1, ts_=ts_: e.transpose(pt1[:, 0:32], impT[:, ts_], identf[0:32, 0:32]), reads=[impT, identf], writes=[pt1])
                sc_ = sc[i % 2]
                m8_ = m8[i % 2]
                p.op("vector", lambda e, sc_=sc_, pt1=pt1, i=i: e.tensor_tensor(sc_[:, :], pt1[:, 0:32], vmul[:, i, :], ALU.mult), reads=[pt1, vmul], writes=[sc_])
                p.op("vector", lambda e, sc_=sc_, i=i: e.tensor_tensor(sc_[:, :], sc_[:, :], amask[:, i, :], ALU.add), reads=[sc_, amask], writes=[sc_])
                p.op("vector", lambda e, sc_=sc_, m8_=m8_: e.max(m8_[:, :], sc_[:, :]), reads=[sc_], writes=[m8_])
                p.op("vector", lambda e, sc_=sc_, m8_=m8_: e.tensor_scalar(sc_[:, :], sc_[:, :], m8_[:, 7:8], None, ALU.is_ge), reads=[sc_, m8_], writes=[sc_])
                p.op("vector", lambda e, sc_=sc_: e.tensor_scalar(sc_[:, :], sc_[:, :], -1.0, -NEGB, ALU.add, ALU.mult), reads=[sc_], writes=[sc_])
                pt2 = nps()
                p.op("tensor", lambda e, pt2=pt2, sc_=sc_: e.transpose(pt2[0:32, 0:128], sc_[:, :], identf[:, :]), reads=[sc_, identf], writes=[pt2])
                p.op("scalar", lambda e, pt2=pt2, ts_=ts_: e.copy(selbT[:, 0, ts_], pt2[0:32, 0:128]), reads=[pt2], writes=[(selbT, i)])
            p.fence(selbT)
            cnt = 0
            for hh in range(4):
                h = g * 4 + hh
                for b in range(4):
                    bs = slice(b * 512, (b + 1) * 512)
                    res = {}
                    for br in (1, 2):
                        pairs = []
                        if br == 1:
                            for j in range(0, 4 * b + 4):
                                extra = [(Emat[:, j, :], selbT[:, 0, bs], [Emat, selbT])]
                                if j >= 4 * b:
                                    extra.append((identb[:, :], masks[:, NSA_MASK0 + (j - 4 * b) + 4, :], [identb, masks]))
                                pairs.append((j, extra))
                            kT_, v_ = ksT, vs
                        else:
                            for j in range(max(0, 4 * b - 4), 4 * b + 4):
                                pairs.append((j, [(identb[:, :], masks[:, NSA_MASK0 + (j - 4 * b) + 4, :], [identb, masks])]))
                            kT_, v_ = kwT, vw
                        r_ = rec[br - 1]
                        ts2 = tsel[br - 1]

                        def fin(ps2, r_=r_, ts2=ts2, br=br, h=h, bs=bs):
                            pgb = nps()
                            p.op("tensor", lambda e: e.matmul(pgb[:, :], Sel[:, br * 8 + h, :], gT[:, bs], start=True, stop=True), reads=[Sel, gT], writes=[pgb])
                            p.op("vector", lambda e: e.reciprocal(r_[64:128, :], ps2[64:128, :]), reads=[ps2], writes=[r_])
                            p.op("vector", lambda e: e.tensor_tensor(r_[64:128, :], r_[64:128, :], pgb[64:128, :], ALU.mult), reads=[r_, pgb], writes=[r_])
                            p.op("vector", lambda e: e.tensor_tensor(ts2[:, :], ps2[0:64, :], r_[64:128, :], ALU.mult), reads=[ps2, r_], writes=[ts2])
                        attn_group(qT, (hh, b), hh, b, kT_, 0, v_, pairs, PT, None, fin)
                    y_ = yo[cnt % 2]
                    cnt += 1
                    p.op("gpsimd", lambda e, hh=hh, bs=bs: e.tensor_tensor(tsel[0][:, :], tsel[0][:, :], ycmp[:, hh, bs], ALU.add), reads=[tsel[0], (ycmp, (hh, b))], writes=[tsel[0]])
                    p.op("gpsimd", lambda e, y_=y_: e.tensor_tensor(y_[:, :], tsel[0][:, :], tsel[1][:, :], ALU.add), reads=[tsel[0], tsel[1]], writes=[y_])
                    p.dma(yT_d[1][h // 2, (h % 2) * 64:(h % 2) * 64 + 64, bs], y_[:, :], reads=[y_], writes=[(yT_d[1], (h, b))])
            p.fence(qT)
            p.fence(ycmp)
            p.release(mk_g)
        p.fence(yT_d[1])
        p.release(mk)


    def mixer_rwkv(l):
        mk = p.mark()
        ro = 0
        co = l * COL_PER_LAYER
        crw = p.carve([128, 642], F32, "crw")
        p.dma(crw[:, :], C["c_rw"][:, :], writes=[crw])
        bones = crw[:, 0:128]
        hsel = crw[:, 128:130]
        cmask = crw[:, 130:642]
        colv = lambda cidx: cols[:, co + cidx:co + cidx + 1]
        lora_in = p.carve([128, T], BF16, "rw_lora_in")
        sgT = p.carve([128, T], BF16, "rw_sgT")
        wup = p.carve([128, 512], BF16, "rw_wup")
        aup = p.carve([128, 512], BF16, "rw_aup")
        gup = p.carve([128, 512], BF16, "rw_gup")
        p.op("gpsimd", lambda e: e.memset(wup[:, :], 0.0), writes=[wup])
        p.op("gpsimd", lambda e: e.memset(aup[:, :], 0.0), writes=[aup])
        load_w(wup, lambda sv: [(wup[0:64, :], sv[0:64, 0, :])], W["rwkv_w_up"][l].unsqueeze(1), (1, 512), parts=64)
        sA = stg[stgi[0] % NSTG]
        stgi[0] += 1
        p.dma(sA[64:128, 0:512], W["rwkv_a_up"][l], writes=[sA])
        p.op("gpsimd", lambda e: e.tensor_copy(aup[64:128, :], sA[64:128, 0:512]), reads=[sA], writes=[aup])
        load_w(gup, lambda sv: [(gup[:, :], sv[:, 0, :])], W["rwkv_g_up"][l].unsqueeze(1), (1, 512))
        wch = [p.carve([128, 8, 128], BF16, "rw_wch%d" % i) for i in range(3)]
        ur = [[p.carve([128, 520], F32, "rw_ur%d_%d" % (q, i)) for i in range(2)] for q in range(3)]
        dtmp = p.carve([128, 512], F32, "rw_dtmp")

        def proj_lerp(chunk, tb, wbuf, urq, dst_ap, dst_buf, dst_key, act=None):
            un = urq[tb % 2]
            uo = urq[(tb + 1) % 2]
            if tb == 0:
                p.op("gpsimd", lambda e: e.memset(un[:, 0:1], 0.0), writes=[(un, "c")])
            else:
                p.op("gpsimd", lambda e: e.tensor_copy(un[:, 0:1], uo[:, 512:513]), reads=[uo], writes=[(un, "c")])
            pa = nps()
            proj_fm(pa[:, :], pa, wbuf, lambda c: wbuf[:, c, :], tb * 512, 512)
            p.op("scalar", lambda e: e.copy(un[:, 1:513], pa[:, :]), reads=[pa], writes=[(un, "d")])
            p.fence(un)
            p.op("vector", lambda e: e.tensor_tensor(dtmp[:, :], un[:, 0:512], un[:, 1:513], ALU.subtract), reads=[un], writes=[dtmp])
            p.op("vector", lambda e: e.scalar_tensor_tensor(out=dst_ap, in0=dtmp[:, :], scalar=colv(COL_MU + chunk), in1=un[:, 1:513], op0=ALU.mult, op1=ALU.add),
                 reads=[dtmp, un, cols], writes=[(dst_buf, dst_key)])

        lx = p.carve([128, 512], F32, "rw_lx")
        for q, chunk in enumerate((12, 13)):
            load_w(wch[q], lambda sv, q=q: [(wch[q][:, :, :], sv)], win_cols(l, OFF_RW + chunk * 128, 128), (8, 128))
            for tb in range(4):
                bs = slice(tb * 512, (tb + 1) * 512)
                proj_lerp(chunk, tb, wch[q], ur[q], lx[:, :], lx, None)
                if chunk == 12:
                    p.op("scalar", lambda e, bs=bs: e.activation(lora_in[0:64, bs], lx[0:64, :], AF.Tanh), reads=[lx], writes=[(lora_in, ("w", tb))])
                    p.op("vector", lambda e, bs=bs: e.tensor_copy(lora_in[64:128, bs], lx[64:128, :]), reads=[lx], writes=[(lora_in, ("a", tb))])
                else:
                    p.op("scalar", lambda e, bs=bs: e.activation(sgT[:, bs], lx[:, :], AF.Sigmoid), reads=[lx], writes=[(sgT, tb)])
        p.fence(lora_in)
        p.fence(sgT)
        def B_(name):
            return p.carve([128, 512], F32, "rw_" + name)
        rT, kT, vT = B_("rT"), B_("kT"), B_("vT")
        lw, av, kk, kmod, bb, cl, eg, egi, egm, tq, rkr = [B_(n) for n in ("lw", "av", "kk", "kmod", "bb", "cl", "eg", "egi", "egm", "tq", "rkr")]
        KR = p.carve([128, 4, 2, 128], F32, "rw_KR")
        bt, kt = B_("bt"), B_("kt")
        mA = {n: [B_(n + "A"), B_(n + "B")] for n in ("b", "k", "kap", "r")}
        NCI = 4
        M1s = [lw, av, kk, kmod]
        M2s = [bb, cl, egi, egm]
        M3s = [p.carve([128, 256], F32, "rw_M3_%d" % i) for i in range(NCI)]
        PPs = [[tq, tq], [kt, kt], [B_("PP_2")] * 2, [B_("PP_3")] * 2]
        Zbs = [p.carve([128, 2, 128], F32, "rw_Z%d" % i) for i in range(NCI)]
        BKtms = [p.carve([128, 4, 128], F32, "rw_BKtm%d" % i) for i in range(NCI)]
        Vtms = [p.carve([128, 128], F32, "rw_Vtm%d" % i) for i in range(NCI)]
        print("rwkv arena words used", p.aoff, "of", p.awords)
        Ast = p.carve([128, 64], F32, "rw_A")
        Yn = p.carve([128, 128], F32, "rw_Yn")
        Us = p.carve([128, 128], F32, "rw_U")
        t1 = p.carve([128, 64], F32, "rw_t1")
        osb = p.carve([128, 128], F32, "rw_osb")
        oc = p.carve([128, 128], F32, "rw_oc")
        sq = p.carve([128, 128], F32, "rw_sq")
        st4 = p.carve([128, 8], F32, "rw_st4")
        ssb = p.carve([128, 2], F32, "rw_ssb")
        ybf = p.carve([128, 128], BF16, "rw_ybf")
        ysb = [p.carve([128, 128], BF16, "rw_ysb%d" % i) for i in range(2)]
        gne = p.carve([128, 1], F32, "rw_gne")
        p.op("vector", lambda e: e.memset(gne[:, :], 64e-5), writes=[gne])
        msk4 = lambda: None
        v2 = lambda ap: ap.rearrange("p (h d) -> p h d", h=2)
        for hp in range(4):
            for q, chunk in enumerate((hp, 4 + hp, 8 + hp)):
                load_w(wch[q], lambda sv, q=q: [(wch[q][:, :, :], sv)], win_cols(l, OFF_RW + chunk * 128, 128), (8, 128))
            p.op("vector", lambda e: e.memset(Ast[:, :], 0.0), writes=[Ast])
            for tb in range(4):
                bs = slice(tb * 512, (tb + 1) * 512)
                for q, (chunk, dst) in enumerate(((hp, rT), (4 + hp, kT), (8 + hp, vT))):
                    proj_lerp(chunk, tb, wch[q], ur[q], dst[:, :], dst, None)
                pw = nps()
                p.op("tensor", lambda e, pw=pw, bs=bs: e.matmul(pw[:, :], wup[:, hp * 128:(hp + 1) * 128], lora_in[:, bs], start=True, stop=True), reads=[wup, lora_in], writes=[pw])
                p.op("scalar", lambda e, pw=pw: e.activation(lw[:, :], pw[:, :], AF.Sigmoid, bias=colv(COL_W0 + hp)), reads=[pw, cols], writes=[lw])
                p.op("vector", lambda e: e.tensor_scalar(lw[:, :], lw[:, :], -0.6065306597126334, None, ALU.mult), reads=[lw], writes=[lw])
                pa_ = nps()
                p.op("tensor", lambda e, pa_=pa_, bs=bs: e.matmul(pa_[:, :], aup[:, hp * 128:(hp + 1) * 128], lora_in[:, bs], start=True, stop=True), reads=[aup, lora_in], writes=[pa_])
                p.op("scalar", lambda e, pa_=pa_: e.activation(av[:, :], pa_[:, :], AF.Sigmoid, bias=colv(COL_A0 + hp)), reads=[pa_, cols], writes=[av])
                p.op("vector", lambda e: e.tensor_scalar(kk[:, :], kT[:, :], colv(COL_KK + hp), None, ALU.mult), reads=[kT, cols], writes=[kk])
                p.op("gpsimd", lambda e: e.tensor_tensor(tq[:, :], kk[:, :], kk[:, :], ALU.mult), reads=[kk], writes=[tq])
                pss = nps()
                p.op("tensor", lambda e, pss=pss: e.matmul(pss[:, :], bones, tq[:, :], start=True, stop=True), reads=[crw, tq], writes=[pss])
                p.op("scalar", lambda e, pss=pss: e.activation(tq[:, :], pss[:, :], AF.Sqrt), reads=[pss], writes=[tq])
                p.op("vector", lambda e: e.tensor_scalar(tq[:, :], tq[:, :], 1e-12, None, ALU.max), reads=[tq], writes=[tq])
                p.op("vector", lambda e: e.reciprocal(tq[:, :], tq[:, :]), reads=[tq], writes=[tq])
                p.op("vector", lambda e: e.tensor_tensor(kk[:, :], kk[:, :], tq[:, :], ALU.mult), reads=[kk, tq], writes=[kk])
                p.op("gpsimd", lambda e: e.tensor_scalar(tq[:, :], av[:, :], -1.0, colv(COL_KA + hp), ALU.add, ALU.mult), reads=[av, cols, tq], writes=[tq])
                p.op("vector", lambda e: e.scalar_tensor_tensor(out=kmod[:, :], in0=tq[:, :], scalar=1.0, in1=kT[:, :], op0=ALU.add, op1=ALU.mult), reads=[tq, kT], writes=[kmod])
                p.op("gpsimd", lambda e: e.tensor_tensor(bb[:, :], kk[:, :], av[:, :], ALU.mult), reads=[kk, av], writes=[bb])
                p.op("vector", lambda e: e.scalar_tensor_tensor(out=rkr[:, :], in0=rT[:, :], scalar=colv(COL_RK + hp), in1=kmod[:, :], op0=ALU.mult, op1=ALU.mult), reads=[rT, kmod, cols], writes=[rkr])
                p.op("vector", lambda e: e.tensor_tensor_scan(cl[:, :], cmask, lw[:, :], 0.0, ALU.mult, ALU.add), reads=[crw, lw], writes=[cl])
                p.op("scalar", lambda e: e.activation(eg[:, :], cl[:, :], AF.Exp), reads=[cl], writes=[eg])
                p.op("scalar", lambda e: e.activation(egi[:, :], cl[:, :], AF.Exp, scale=-1.0), reads=[cl], writes=[egi])
                p.op("gpsimd", lambda e: e.tensor_tensor(egm[:, :], cl[:, :], lw[:, :], ALU.subtract), reads=[cl, lw], writes=[egm])
                p.op("scalar", lambda e: e.activation(egm[:, :], egm[:, :], AF.Exp), reads=[egm], writes=[egm])
                v4 = lambda ap: ap.rearrange("p (c t) -> p c t", c=4)
                p.op("vector", lambda e: e.tensor_tensor(KR[:, :, 0, :], v4(kk[:, :]), v4(egm[:, :]), ALU.mult), reads=[kk, egm], writes=[(KR, 0)])
                p.op("vector", lambda e: e.tensor_tensor(KR[:, :, 1, :], v4(rT[:, :]), v4(eg[:, :]), ALU.mult), reads=[rT, eg], writes=[(KR, 1)])
                p.fence(KR)
                p.op("gpsimd", lambda e: e.tensor_tensor(bt[:, :], bb[:, :], egi[:, :], ALU.mult), reads=[bb, egi], writes=[bt])
                p.op("gpsimd", lambda e: e.tensor_tensor(kt[:, :], kmod[:, :], egi[:, :], ALU.mult), reads=[kmod, egi], writes=[kt])
                for X in range(2):
                    hcol = crw[:, 128 + X:129 + X]
                    p.op("vector", lambda e, X=X, hcol=hcol: e.tensor_scalar(mA["b"][X][:, :], bt[:, :], hcol, None, ALU.mult), reads=[bt, crw], writes=[mA["b"][X]])
                    p.op("scalar", lambda e, X=X, hcol=hcol: e.activation(mA["k"][X][:, :], kt[:, :], AF.Copy, scale=hcol), reads=[kt, crw], writes=[mA["k"][X]])
                    p.op("vector", lambda e, X=X, hcol=hcol: e.tensor_scalar(v4(mA["kap"][X][:, :]), KR[:, :, 0, :], hcol, None, ALU.mult), reads=[KR, crw], writes=[mA["kap"][X]])
                    p.op("scalar", lambda e, X=X, hcol=hcol: e.activation(v4(mA["r"][X][:, :]), KR[:, :, 1, :], AF.Copy, scale=hcol), reads=[KR, crw], writes=[mA["r"][X]])
                for cc in range(4):
                    c = tb * 4 + cc
                    cs = slice(cc * 128, (cc + 1) * 128)
                    M1, M2, M3, Zb, BKtm, Vtm = M1s[cc], M2s[cc], M3s[cc], Zbs[cc], BKtms[cc], Vtms[cc]
                    ts_ = slice(c * 128, (c + 1) * 128)
                    b1, b2, b3 = nps(), nps(), nps()
                    krv = KR[:, cc, :, :].rearrange("p a t -> p (a t)")
                    for X in range(2):
                        p.op("tensor", lambda e, X=X: e.matmul(b1[:, X * 256:(X + 1) * 256], mA["b"][X][:, cs], krv, start=True, stop=True), reads=[mA["b"][X], KR], writes=[b1])
                        p.op("tensor", lambda e, X=X: e.matmul(b2[:, X * 256:(X + 1) * 256], mA["k"][X][:, cs], krv, start=True, stop=True), reads=[mA["k"][X], KR], writes=[b2])
                        p.op("tensor", lambda e, X=X: e.matmul(b3[:, X * 128:(X + 1) * 128], mA["kap"][X][:, cs], bt[:, cs], start=True, stop=True), reads=[mA["kap"][X], bt], writes=[b3])
                    msi = lambda ap: ap.rearrange("p (x m t) -> p x m t", x=2, m=2)
                    for m_, cidx in ((0, 4), (1, 0)):
                        mk_ = cssd[:, cidx, :].unsqueeze(1).to_broadcast([128, 2, 128])
                        p.op("vector", lambda e, m_=m_, mk_=mk_: e.tensor_tensor(msi(M1[:, :])[:, :, m_, :], msi(b1[:, :])[:, :, m_, :], mk_, ALU.mult), reads=[b1, cssd], writes=[(M1, m_)])
                        p.op("vector", lambda e, m_=m_, mk_=mk_: e.tensor_tensor(msi(M2[:, :])[:, :, m_, :], msi(b2[:, :])[:, :, m_, :], mk_, ALU.mult), reads=[b2, cssd], writes=[(M2, m_)])
                    p.fence(M1)
                    p.fence(M2)
                    p.op("vector", lambda e: e.tensor_tensor(v2(M3[:, :]), v2(b3[:, 0:256]), cssd[:, 1, :].unsqueeze(1).to_broadcast([128, 2, 128]), ALU.mult), reads=[b3, cssd], writes=[M3])
                    lbt = lambda X: M1[:, X * 256:X * 256 + 128]
                    p.op("gpsimd", lambda e: e.tensor_tensor(Zb[:, :, :], identf[:, :].unsqueeze(1).to_broadcast([128, 2, 128]), msi(M1[:, :])[:, :, 0, :], ALU.subtract), reads=[identf, M1], writes=[Zb])
                    ptr = nps()
                    for ti, src in enumerate((mA["b"][0], mA["b"][1], mA["k"][0], mA["k"][1])):
                        p.op("tensor", lambda e, ti=ti, src=src: e.transpose(ptr[:, ti * 128:(ti + 1) * 128], src[:, cs], identf[:, :]), reads=[src, identf], writes=[ptr])
                    p.op("scalar", lambda e, ptr=ptr: e.copy(BKtm[:, :, :], ptr[:, :].rearrange("p (a t) -> p a t", a=4)), reads=[ptr], writes=[BKtm])
                    pv_ = nps()
                    p.op("tensor", lambda e, pv_=pv_: e.transpose(pv_[:, 0:128], vT[:, cs], identf[:, :]), reads=[vT, identf], writes=[pv_])
                    p.op("vector", lambda e, pv_=pv_: e.tensor_copy(Vtm[:, :], pv_[:, 0:128]), reads=[pv_], writes=[Vtm])
                Pn_ = [[M3s[cc][:, 0:128], M3s[cc][:, 128:256]] for cc in range(4)]
                Pt_ = [[M1s[cc][:, 0:128], M1s[cc][:, 256:384]] for cc in range(4)]
                Pb_ = [[M3s[cc], M1s[cc]] for cc in range(4)]
                for lev in range(1, 7):
                    for cc in range(4):
                        Zb = Zbs[cc]
                        Pn, Pt = Pn_[cc], Pt_[cc]
                        pbn, pbt = Pb_[cc]
                        pq = nps()
                        for X in range(2):
                            p.op("tensor", lambda e, X=X: e.matmul(pq[:, X * 256:X * 256 + 128], Pt[X], Pn[X], start=True, stop=True), reads=[pbn, pbt], writes=[pq])
                            p.op("tensor", lambda e, X=X: e.matmul(pq[:, X * 256 + 128:X * 256 + 256], Pn[X], Pt[X], start=True, stop=True), reads=[pbn, pbt], writes=[pq])
                        pp_ = PPs[cc][lev % 2]
                        if cc % 2 == 0:
                            p.op("scalar", lambda e: e.copy(pp_[:, :], pq[:, :]), reads=[pq], writes=[pp_])
                        else:
                            p.op("vector", lambda e: e.tensor_copy(pp_[:, :], pq[:, :]), reads=[pq], writes=[pp_])
                        Pn_[cc] = [pp_[:, 0:128], pp_[:, 256:384]]
                        Pt_[cc] = [pp_[:, 128:256], pp_[:, 384:512]]
                        Pb_[cc] = [pp_, pp_]
                        Pn = Pn_[cc]
                        pz = nps()
                        for X in range(2):
                            p.op("tensor", lambda e, X=X: e.matmul(pz[:, X * 128:(X + 1) * 128], Pn[X], Zb[:, X, :], start=True, stop=True), reads=[pp_, Zb], writes=[pz])
                        p.op("vector", lambda e: e.tensor_tensor(Zb[:, :, :], Zb[:, :, :], v2(pz[:, 0:256]), ALU.add), reads=[pz, Zb], writes=[Zb])
                def seq_part(cc):
                    c = tb * 4 + cc
                    cs = slice(cc * 128, (cc + 1) * 128)
                    ts_ = slice(c * 128, (c + 1) * 128)
                    M1, M2, M3, Zb, BKtm, Vtm = M1s[cc], M2s[cc], M3s[cc], Zbs[cc], BKtms[cc], Vtms[cc]
                    py = nps()
                    for X in range(2):
                        p.op("tensor", lambda e, X=X, py=py: e.matmul(py[:, X * 64:(X + 1) * 64], mA["kap"][X][:, cs], Ast[:, :], start=True, stop=False), reads=[mA["kap"][X], Ast], writes=[py])
                        p.op("tensor", lambda e, X=X, py=py: e.matmul(py[:, X * 64:(X + 1) * 64], M2[:, X * 256:X * 256 + 128], Vtm[:, X * 64:(X + 1) * 64], start=False, stop=True), reads=[M2, Vtm], writes=[py])
                    p.op("vector", lambda e, py=py: e.tensor_scalar(Yn[:, :], py[:, 0:128], -1.0, None, ALU.mult), reads=[py], writes=[Yn])
                    pu = nps()
                    for X in range(2):
                        p.op("tensor", lambda e, X=X, pu=pu: e.matmul(pu[:, X * 64:(X + 1) * 64], Zb[:, X, :], Yn[:, X * 64:(X + 1) * 64], start=True, stop=True), reads=[Zb, Yn], writes=[pu])
                    p.op("vector", lambda e, pu=pu: e.tensor_copy(Us[:, :], pu[:, 0:128]), reads=[pu], writes=[Us])
                    po = npl()
                    for X in range(2):
                        p.op("tensor", lambda e, X=X, po=po: e.matmul(po[:, X * 64:(X + 1) * 64], mA["r"][X][:, cs], Ast[:, :], start=True, stop=False), reads=[mA["r"][X], Ast], writes=[po])
                        p.op("tensor", lambda e, X=X, po=po: e.matmul(po[:, X * 64:(X + 1) * 64], M1[:, X * 256 + 128:X * 256 + 256], Us[:, X * 64:(X + 1) * 64], start=False, stop=False), reads=[M1, Us], writes=[po])
                        p.op("tensor", lambda e, X=X, po=po: e.matmul(po[:, X * 64:(X + 1) * 64], M2[:, X * 256 + 128:X * 256 + 256], Vtm[:, X * 64:(X + 1) * 64], start=False, stop=True), reads=[M2, Vtm], writes=[po])
                    pi_ = nps()
                    seq = [(0, Us, 0), (1, Us, 1), (2, Vtm, 0), (3, Vtm, 1)]
                    for si, (ti, rb, X) in enumerate(seq):
                        p.op("tensor", lambda e, si=si, ti=ti, rb=rb, X=X, pi_=pi_: e.matmul(pi_[:, 0:64], BKtm[:, ti, :], rb[:, X * 64:(X + 1) * 64], start=(si == 0), stop=(si == 3)), reads=[BKtm, rb], writes=[pi_])
                    gC = eg[:, cc * 128 + 127:cc * 128 + 128]
                    p.op("vector", lambda e, pi_=pi_, gC=gC: e.tensor_scalar(t1[:, :], pi_[:, 0:64], gC, None, ALU.mult), reads=[pi_, eg], writes=[t1])
                    p.op("vector", lambda e, gC=gC: e.scalar_tensor_tensor(out=Ast[:, :], in0=Ast[:, :], scalar=gC, in1=t1[:, :], op0=ALU.mult, op1=ALU.add), reads=[t1, eg, Ast], writes=[Ast])
                    return po

                def epi_part(cc, po):
                    c = tb * 4 + cc
                    cs = slice(cc * 128, (cc + 1) * 128)
                    ts_ = slice(c * 128, (c + 1) * 128)
                    M1, M2, M3, Zb, BKtm, Vtm = M1s[cc], M2s[cc], M3s[cc], Zbs[cc], BKtms[cc], Vtms[cc]
                    p.op("scalar", lambda e, po=po: e.copy(osb[:, :], po[:, 0:128]), reads=[po], writes=[osb])
                    p.op("vector", lambda e: e.tensor_reduce(out=st4[:, 0:2], in_=v2(osb[:, :]), axis=AX.X, op=ALU.add), reads=[osb], writes=[(st4, 0)])
                    p.op("vector", lambda e: e.tensor_scalar(st4[:, 2:4], st4[:, 0:2], -1.0 / 64, None, ALU.mult), reads=[(st4, 0)], writes=[(st4, 1)])
                    p.op("vector", lambda e: e.tensor_tensor(v2(oc[:, :]), v2(osb[:, :]), st4[:, 2:4].unsqueeze(2).to_broadcast([128, 2, 64]), ALU.add), reads=[osb, (st4, 1)], writes=[oc])
                    p.op("gpsimd", lambda e: e.tensor_tensor(sq[:, :], oc[:, :], oc[:, :], ALU.mult), reads=[oc], writes=[sq])
                    p.op("vector", lambda e: e.tensor_reduce(out=st4[:, 4:6], in_=v2(sq[:, :]), axis=AX.X, op=ALU.add), reads=[sq], writes=[(st4, 2)])
                    p.op("scalar", lambda e: e.activation(st4[:, 6:8], st4[:, 4:6], AF.Sqrt, bias=gne[:, 0:1], scale=1.0 / 64), reads=[(st4, 2), gne], writes=[(st4, 3)])
                    p.op("vector", lambda e: e.reciprocal(st4[:, 6:8], st4[:, 6:8]), reads=[(st4, 3)], writes=[(st4, 3)])
                    p.op("vector", lambda e: e.tensor_tensor(v2(oc[:, :]), v2(oc[:, :]), st4[:, 6:8].unsqueeze(2).to_broadcast([128, 2, 64]), ALU.mult), reads=[oc, (st4, 3)], writes=[oc])
                    p.op("gpsimd", lambda e: e.tensor_tensor(oc[:, :], oc[:, :], rows[:, ro + ROW_LNW + hp * 128:ro + ROW_LNW + (hp + 1) * 128], ALU.mult), reads=[oc, rows], writes=[oc])
                    p.op("gpsimd", lambda e: e.tensor_tensor(oc[:, :], oc[:, :], rows[:, ro + ROW_LNB + hp * 128:ro + ROW_LNB + (hp + 1) * 128], ALU.add), reads=[oc, rows], writes=[oc])
                    pb_ = nps()
                    p.op("tensor", lambda e, pb_=pb_: e.matmul(pb_[:, 0:2], rkr[:, cs], hsel, start=True, stop=True), reads=[rkr, crw], writes=[pb_])
                    p.op("vector", lambda e, pb_=pb_: e.tensor_copy(ssb[:, :], pb_[:, 0:2]), reads=[pb_], writes=[ssb])
                    p.op("vector", lambda e: e.tensor_tensor(v2(sq[:, :]), v2(Vtm[:, :]), ssb[:, :].unsqueeze(2).to_broadcast([128, 2, 64]), ALU.mult), reads=[Vtm, ssb, sq], writes=[sq])
                    p.op("gpsimd", lambda e: e.tensor_tensor(oc[:, :], oc[:, :], sq[:, :], ALU.add), reads=[oc, sq], writes=[oc])
                    pg_ = nps()
                    p.op("tensor", lambda e, pg_=pg_, ts_=ts_: e.matmul(pg_[:, 0:128], sgT[:, ts_], gup[:, hp * 128:(hp + 1) * 128], start=True, stop=True), reads=[sgT, gup], writes=[pg_])
                    p.op("vector", lambda e, pg_=pg_: e.tensor_tensor(ybf[:, :], oc[:, :], pg_[:, 0:128], ALU.mult), reads=[oc, pg_], writes=[ybf])
                    pt = npst()
                    p.op("tensor", lambda e, pt=pt: e.transpose(pt[:, 0:128], ybf[:, :], identb[:]), reads=[ybf, identb], writes=[pt])
                    ys_ = ysb[c % 2]
                    p.op("scalar", lambda e, pt=pt, ys_=ys_: e.copy(ys_[:, :], pt[:, 0:128]), reads=[pt], writes=[ys_])
                    p.dma(yT_d[2][hp, :, ts_], ys_[:, :], reads=[ys_], writes=[(yT_d[2], (hp, c))])
                pos_ = {}
                for step in range(5):
                    if step < 4:
                        pos_[step] = seq_part(step)
                    if step >= 1:
                        epi_part(step - 1, pos_[step - 1])
        if l == 0:
            for nm, bf in (("rT", rT), ("kT", kT), ("vT", vT), ("lw", lw), ("av", av), ("kk", kk), ("kmod", kmod), ("cl", cl), ("rkr", rkr)):
                dump("rw_" + nm, bf[:, :], [128, 512], F32, [bf])
            dump("rw_osb", osb[:, :], [128, 128], F32, [osb])
            dump("rw_oc", oc[:, :], [128, 128], F32, [oc])
            dump("rw_Us", Us[:, :], [128, 128], F32, [Us])
            dump("rw_A", Ast[:, :], [128, 64], F32, [Ast])
        p.fence(yT_d[2])
        p.release(mk)

    def mixer_ssd(l):
        mk = p.mark()
        ro = 0
        co = l * COL_PER_LAYER
        xbcT = p.carve([128, 8, T], BF16, "xbcT")
        Xtm = p.carve([128, NT, 512], BF16, "Xtm")
        Btm = p.carve([128, NT, 256], BF16, "Btm")
        dt_all = p.carve([128, NT, 8], F32, "dt_all")
        a_all = p.carve([128, NT, 8], F32, "a_all")
        acum = p.carve([128, NT, 8], F32, "acum")
        tot = p.carve([128, NT, 8], F32, "tot")
        ea = p.carve([128, NT, 8], F32, "ea")
        dts = p.carve([128, NT, 8], F32, "dts")
        cd = p.carve([128, NT, 8], F32, "cd")
        Arow = p.carve([128, 8], F32, "Arow")
        mk2 = p.mark()
        xr = [p.carve([128, T + 8], F32, "xr%d" % i) for i in range(2)]
        cacc = [p.carve([128, T], F32, "cacc%d" % i) for i in range(2)]
        wx = [p.carve([128, 8, 128], BF16, "wx%d" % i) for i in range(2)]
        for ch in range(8):
            xr_, ca_, wx_ = xr[ch % 2], cacc[ch % 2], wx[ch % 2]
            load_w(wx_, lambda sv, wx_=wx_: [(wx_[:, :, :], sv)], win_cols(l, OFF_XBC + ch * 128, 128), (8, 128))
            p.op("vector", lambda e, xr_=xr_: e.memset(xr_[:, 0:4], 0.0), writes=[(xr_, "z")])
            for b in range(4):
                pa = nps()
                proj_fm(pa[:, :], pa, wx_, lambda c, wx_=wx_: wx_[:, c, :], b * 512, 512)
                p.op("scalar", lambda e, xr_=xr_, pa=pa, b=b: e.copy(xr_[:, 4 + b * 512:4 + (b + 1) * 512], pa[:, :]), reads=[pa], writes=[(xr_, b)])
            p.fence(xr_)
            for k in range(4):
                wcol = cols[:, co + COL_CONVW + ch * 4 + k:co + COL_CONVW + ch * 4 + k + 1]
                if k == 0:
                    p.op("vector", lambda e, ca_=ca_, xr_=xr_, wcol=wcol: e.tensor_scalar(ca_[:, :], xr_[:, 1:1 + T], wcol, None, ALU.mult),
                         reads=[xr_, cols], writes=[ca_])
                else:
                    p.op("vector", lambda e, ca_=ca_, xr_=xr_, wcol=wcol, k=k: e.scalar_tensor_tensor(out=ca_[:, :], in0=xr_[:, 1 + k:1 + k + T], scalar=wcol, in1=ca_[:, :], op0=ALU.mult, op1=ALU.add),
                         reads=[xr_, cols, ca_], writes=[ca_])
            p.op("scalar", lambda e, ca_=ca_, ch=ch: e.activation(xbcT[:, ch, :], ca_[:, :], AF.Silu, bias=cols[:, co + COL_CONVB + ch:co + COL_CONVB + ch + 1]),
                 reads=[ca_, cols], writes=[(xbcT, ch)])
        p.fence(xbcT)
        p.release(mk2)
        import os as _os
        _stop = int(_os.environ.get("SSD_STOP", "99"))
        if _stop <= 1:
            p.release(mk)
            return
        for i in range(NT):
            pt = npst()
            for c in range(6):
                p.op("tensor", lambda e, pt=pt, c=c, i=i: e.transpose(pt[:, c * 128:(c + 1) * 128], xbcT[:, c, i * 128:(i + 1) * 128], identb[:]),
                     reads=[xbcT, identb], writes=[pt])
            p.op("vector", lambda e, pt=pt, i=i: e.tensor_copy(Xtm[:, i, :], pt[:, 0:512]), reads=[pt], writes=[(Xtm, i)])
            p.op("vector", lambda e, pt=pt, i=i: e.tensor_copy(Btm[:, i, :], pt[:, 512:768]), reads=[pt], writes=[(Btm, i)])
        p.fence(Xtm)
        p.fence(Btm)
        if _stop <= 2:
            p.release(mk)
            return
        wdt = p.carve([128, 8, 8], BF16, "wdt")
        load_w(wdt, lambda sv: [(wdt[:, :, :], sv)], win_cols(l, OFF_DT, 8), (8, 8))
        pd = nps()
        for i in range(NT):
            proj_tm(pd[:, i * 8:(i + 1) * 8], pd, wdt, lambda c: wdt[:, c, :], i)
        b3 = lambda r0: rows[:, ro + r0:ro + r0 + 8].unsqueeze(1).to_broadcast([128, NT, 8])
        p.op("vector", lambda e: e.tensor_tensor(dt_all[:, :, :], pd[:, 0:128].rearrange("p (i h) -> p i h", h=8), b3(ROW_DTB), ALU.add),
             reads=[pd, rows], writes=[dt_all])
        p.op("scalar", lambda e: e.activation(dt_all[:, :, :], dt_all[:, :, :], AF.Exp), reads=[dt_all], writes=[dt_all])
        p.op("scalar", lambda e: e.activation(dt_all[:, :, :], dt_all[:, :, :], AF.Ln, bias=cssd[:, 3, 0:1]), reads=[dt_all, cssd], writes=[dt_all])
        p.op("scalar", lambda e: e.activation(Arow[:, :], rows[:, ro + ROW_ALOG:ro + ROW_ALOG + 8], AF.Exp), reads=[rows], writes=[Arow])
        p.op("vector", lambda e: e.tensor_scalar(Arow[:, :], Arow[:, :], -1.0, None, ALU.mult), reads=[Arow], writes=[Arow])
        p.op("vector", lambda e: e.tensor_tensor(a_all[:, :, :], dt_all[:, :, :], Arow[:, :].unsqueeze(1).to_broadcast([128, NT, 8]), ALU.mult),
             reads=[dt_all, Arow], writes=[a_all])
        pc = nps()
        pt_ = nps()
        for i in range(NT):
            p.op("tensor", lambda e, i=i: e.matmul(pc[:, i * 8:(i + 1) * 8], cssd[:, 0, :], a_all[:, i, :], start=True, stop=True), reads=[cssd, a_all], writes=[pc])
            p.op("tensor", lambda e, i=i: e.matmul(pt_[:, i * 8:(i + 1) * 8], cssd[:, 3, :], a_all[:, i, :], start=True, stop=True), reads=[cssd, a_all], writes=[pt_])
        v3 = lambda ps_: ps_[:, 0:128].rearrange("p (i h) -> p i h", h=8)
        p.op("vector", lambda e: e.tensor_copy(acum[:, :, :], v3(pc)), reads=[pc], writes=[acum])
        p.op("vector", lambda e: e.tensor_copy(tot[:, :, :], v3(pt_)), reads=[pt_], writes=[tot])
        p.op("scalar", lambda e: e.activation(ea[:, :, :], acum[:, :, :], AF.Exp), reads=[acum], writes=[ea])
        p.op("scalar", lambda e: e.activation(cd[:, :, :], tot[:, :, :], AF.Exp), reads=[tot], writes=[cd])
        p.op("vector", lambda e: e.tensor_tensor(dts[:, :, :], tot[:, :, :], acum[:, :, :], ALU.subtract), reads=[tot, acum], writes=[dts])
        p.op("scalar", lambda e: e.activation(dts[:, :, :], dts[:, :, :], AF.Exp), reads=[dts], writes=[dts])
        p.op("vector", lambda e: e.tensor_tensor(dts[:, :, :], dts[:, :, :], dt_all[:, :, :], ALU.mult), reads=[dts, dt_all], writes=[dts])
        if _stop <= 3:
            p.release(mk)
            return
        if l == 0 and _stop == 98:
            dump("ssd_Xtm", Xtm[:, :, :], [128, NT, 512], BF16, [Xtm])
            dump("ssd_Btm", Btm[:, :, :], [128, NT, 256], BF16, [Btm])
            dump("ssd_dt", dt_all[:, :, :], [128, NT, 8], F32, [dt_all])
            dump("ssd_acum", acum[:, :, :], [128, NT, 8], F32, [acum])
            dump("ssd_tot", tot[:, :, :], [128, NT, 8], F32, [tot])
            dump("ssd_xbcT", xbcT[:, :, :], [128, 8, T], BF16, [xbcT])
        wz = p.carve([128, 8, 512], BF16, "wz")
        load_w_plain(wz, wz, win_cols(l, OFF_Z, 512), (8, 512))
        M1 = [p.carve([128, 128], F32, "M1_%d" % i) for i in range(4)]
        Eb = [p.carve([128, 512], F32, "Eb%d" % i) for i in range(2)]
        CBs = [p.carve([128, 128], F32, "CBs%d" % i) for i in range(2)]
        Wt = [p.carve([128, 512], BF16, "Wt%d" % i) for i in range(2)]
        Xdt = [p.carve([128, 512], BF16, "Xdt%d" % i) for i in range(2)]
        Xds = [p.carve([128, 512], BF16, "Xds%d" % i) for i in range(2)]
        prev = p.carve([128, 512], F32, "prev")
        prevb = [p.carve([128, 512], BF16, "prevb%d" % i) for i in range(2)]
        ptmp = p.carve([128, 512], F32, "ptmp")
        y1 = [p.carve([128, 512], F32, "y1_%d" % i) for i in range(2)]
        y2 = [p.carve([128, 512], F32, "y2_%d" % i) for i in range(2)]
        sz = [p.carve([128, 512], F32, "sz%d" % i) for i in range(2)]
        ss = [p.carve([128, 4], F32, "ss%d" % i) for i in range(2)]
        junk = p.carve([128, 256], F32, "sjunk")
        yn = [p.carve([128, 512], BF16, "yn%d" % i) for i in range(2)]
        yst = [p.carve([128, 4, 128], BF16, "yst%d" % i) for i in range(2)]
        p.op("vector", lambda e: e.memset(prev[:, :], 0.0), writes=[prev])
        p.op("vector", lambda e: e.memset(prevb[0][:, :], 0.0), writes=[prevb[0]])
        bc8 = lambda ap8: ap8.unsqueeze(2).to_broadcast([128, 8, 64])
        v8 = lambda ap: ap.rearrange("p (h d) -> p h d", d=64)
        Eb2 = [Eb, [p.carve([128, 512], F32, "Eb2_%d" % i) for i in range(2)]]
        CBs2 = [CBs, [p.carve([128, 128], F32, "CBs2_%d" % i) for i in range(2)]]
        Wt2 = [Wt, [p.carve([128, 512], BF16, "Wt2_%d" % i) for i in range(2)]]

        def stage1(c):
            k = c % 2
            tsl = slice(c * 128, (c + 1) * 128)
            p.op("vector", lambda e: e.tensor_tensor(v8(Xdt[k][:, :]), v8(Xtm[:, c, :]), bc8(dt_all[:, c, :]), ALU.mult),
                 reads=[Xtm, dt_all], writes=[Xdt[k]])
            p.op("gpsimd", lambda e: e.tensor_tensor(v8(Xds[k][:, :]), v8(Xtm[:, c, :]), bc8(dts[:, c, :]), ALU.mult),
                 reads=[Xtm, dts], writes=[Xds[k]])
            for g in range(2):
                pcb = nps()
                p.op("tensor", lambda e: e.matmul(pcb[:, 0:128], xbcT[:, 4 + g, tsl], xbcT[:, 6 + g, tsl], start=True, stop=True),
                     reads=[xbcT], writes=[pcb])
                cb_ = CBs2[k][g]
                p.op("scalar", lambda e: e.copy(cb_[:, :], pcb[:, 0:128]), reads=[pcb], writes=[cb_])
                pseg = nps()
                for hh in range(4):
                    h = g * 4 + hh
                    m1 = M1[hh]
                    p.op("vector", lambda e: e.tensor_scalar(m1[:, :], cssd[:, 1, :], a_all[:, c, h:h + 1], None, ALU.mult),
                         reads=[cssd, a_all], writes=[m1])
                    p.op("tensor", lambda e: e.matmul(pseg[:, hh * 128:(hh + 1) * 128], m1[:, :], cssd[:, 0, :], start=True, stop=False),
                         reads=[m1, cssd], writes=[pseg])
                    p.op("tensor", lambda e: e.matmul(pseg[:, hh * 128:(hh + 1) * 128], identf[:, :], cssd[:, 2, :], start=False, stop=True),
                         reads=[identf, cssd], writes=[pseg])
                eb = Eb2[k][g]
                p.op("scalar", lambda e: e.activation(eb[:, :], pseg[:, :], AF.Exp), reads=[pseg], writes=[eb])
                wt = Wt2[k][g]
                p.op("vector", lambda e: e.tensor_tensor(wt[:, :].rearrange("p (h l) -> p h l", h=4), eb[:, :].rearrange("p (h l) -> p h l", h=4),
                                                         cb_[:, :].unsqueeze(1).to_broadcast([128, 4, 128]), ALU.mult),
                     reads=[eb, cb_], writes=[wt])

        stage1(0)
        for c in range(NT):
            k = c % 2
            pb_c = prevb[c % 2]
            pb_n = prevb[(c + 1) % 2]
            tsl = slice(c * 128, (c + 1) * 128)
            if c + 1 < NT:
                stage1(c + 1)
            pyd = psb[4]
            for g in range(2):
                wt = Wt2[k][g]
                for hh in range(4):
                    h = g * 4 + hh
                    p.op("tensor", lambda e: e.matmul(pyd[:, h * 64:(h + 1) * 64], wt[:, hh * 128:(hh + 1) * 128], Xdt[k][:, h * 64:(h + 1) * 64], start=True, stop=True),
                         reads=[wt, Xdt[k]], writes=[pyd])
            pst_ = nps()
            pyo = psb[5]
            for g in range(2):
                p.op("tensor", lambda e, g=g, c=c, k=k: e.matmul(pst_[:, g * 256:(g + 1) * 256], Btm[:, c, g * 128:(g + 1) * 128], Xds[k][:, g * 256:(g + 1) * 256], start=True, stop=True),
                     reads=[Btm, Xds[k]], writes=[pst_])
                p.op("tensor", lambda e, g=g, tsl=tsl, pb_c=pb_c: e.matmul(pyo[:, g * 256:(g + 1) * 256], xbcT[:, 6 + g, tsl], pb_c[:, g * 256:(g + 1) * 256], start=True, stop=True),
                     reads=[xbcT, pb_c], writes=[pyo])
            y1_ = y1[k]
            y2_ = y2[k]
            p.op("vector", lambda e, y1_=y1_, c=c: e.tensor_tensor(v8(y1_[:, :]), v8(pyo[:, :]), bc8(ea[:, c, :]), ALU.mult), reads=[pyo, ea], writes=[y1_])
            p.op("vector", lambda e, y1_=y1_: e.tensor_tensor(y1_[:, :], y1_[:, :], pyd[:, :], ALU.add), reads=[pyd, y1_], writes=[y1_])
            p.op("gpsimd", lambda e, y2_=y2_, c=c: e.tensor_tensor(v8(y2_[:, :]), v8(Xtm[:, c, :]), bc8(rows[:, ro + ROW_DSK:ro + ROW_DSK + 8]), ALU.mult),
                 reads=[Xtm, rows], writes=[y2_])
            p.op("gpsimd", lambda e, y2_=y2_, y1_=y1_: e.tensor_tensor(y2_[:, :], y2_[:, :], y1_[:, :], ALU.add), reads=[y1_, y2_], writes=[y2_])
            if _stop <= 4 + c:
                break
            if l == 0 and c in (0, 1) and _stop == 98:
                dump("ssd_y2_%d" % c, y2_[:, :], [128, 512], F32, [y2_])
                dump("ssd_y1_%d" % c, y1_[:, :], [128, 512], F32, [y1_])
            if c < NT - 1:
                p.op("vector", lambda e, c=c: e.tensor_tensor(v8(ptmp[:, :]), v8(prev[:, :]), bc8(cd[:, c, :]), ALU.mult), reads=[prev, cd], writes=[ptmp])
                p.op("vector", lambda e: e.tensor_tensor(prev[:, :], ptmp[:, :], pst_[:, :], ALU.add), reads=[ptmp, pst_], writes=[prev])
                p.op("scalar", lambda e, pb_n=pb_n: e.copy(pb_n[:, :], prev[:, :]), reads=[prev], writes=[pb_n])
            pz = nps()
            proj_tm(pz[:, :], pz, wz, lambda cc: wz[:, cc, :], c)
            sz_ = sz[k]
            ss_ = ss[k]
            p.op("scalar", lambda e, sz_=sz_, pz=pz: e.activation(sz_[:, :], pz[:, :], AF.Silu), reads=[pz], writes=[sz_])
            p.op("vector", lambda e, sz_=sz_, y2_=y2_: e.tensor_tensor(sz_[:, :], sz_[:, :], y2_[:, :], ALU.mult), reads=[y2_, sz_], writes=[sz_])
            for g in range(2):
                p.op("scalar", lambda e, sz_=sz_, ss_=ss_, g=g: e.activation(junk[:, :], sz_[:, g * 256:(g + 1) * 256], AF.Square, accum_out=ss_[:, g:g + 1]),
                     reads=[sz_], writes=[junk, (ss_, g)])
            p.fence(ss_)
            p.op("scalar", lambda e, ss_=ss_: e.activation(ss_[:, 2:4], ss_[:, 0:2], AF.Sqrt, bias=epsc[:, 0:1], scale=1.0 / 256), reads=[ss_, epsc], writes=[ss_])
            p.op("vector", lambda e, ss_=ss_: e.reciprocal(ss_[:, 2:4], ss_[:, 2:4]), reads=[ss_], writes=[ss_])
            yn_ = yn[k]
            for g in range(2):
                p.op("vector", lambda e, yn_=yn_, sz_=sz_, ss_=ss_, g=g: e.scalar_tensor_tensor(
                    out=yn_[:, g * 256:(g + 1) * 256], in0=sz_[:, g * 256:(g + 1) * 256], scalar=ss_[:, 2 + g:3 + g],
                    in1=rows[:, ro + ROW_SSDN + g * 256:ro + ROW_SSDN + (g + 1) * 256], op0=ALU.mult, op1=ALU.mult),
                     reads=[sz_, ss_, rows], writes=[(yn_, g)])
            p.fence(yn_)
            pt = npst()
            for cc in range(4):
                p.op("tensor", lambda e, pt=pt, cc=cc, yn_=yn_: e.transpose(pt[:, cc * 128:(cc + 1) * 128], yn_[:, cc * 128:(cc + 1) * 128], identb[:]),
                     reads=[yn_, identb], writes=[pt])
            ys_ = yst[k]
            p.op("scalar", lambda e, ys_=ys_, pt=pt: e.copy(ys_[:, :, :], pt[:, 0:512].rearrange("p (c t) -> p c t", c=4)), reads=[pt], writes=[ys_])
            p.dma(yT_d[0][:, :, c * 128:(c + 1) * 128].rearrange("c p t -> p c t"), ys_[:, :, :], reads=[ys_], writes=[(yT_d[0], c)])
        p.fence(yT_d[0])
        p.release(mk)

    def phase_merge(l, active):
        mk = p.mark()
        mergedT = p.carve([128, 8, T], BF16, "mergedT")
        mk_w = p.mark()
        wg = [p.carve([128, 8, 512], BF16, "mwg%d" % i) for i in range(2)]
        wb = [p.carve([128, 4, 512], BF16, "mwb%d" % i) for i in range(2)]
        acc = [p.carve([128, 512], F32, "macc%d" % i) for i in range(2)]
        sg = [p.carve([128, 512], F32, "msg%d" % i) for i in range(2)]
        tm = [p.carve([128, 512], F32, "mtm%d" % i) for i in range(2)]
        brw = [W["w_br_ssd"], W["w_br_nsa"], W["w_br_rwkv"], W["w_br_swa"]]
        mk2 = p.mark()
        cnt = 0
        wcnt = 0
        for th in range(2):
            p.release(mk2)
            yT = {}
            for m in active:
                yT[m] = p.carve([128, 4, T // 2], BF16, "yTs%d" % m)
                for c in range(4):
                    p.dma(yT[m][:, c, :], yT_d[m][c, :, th * 1024:(th + 1) * 1024], reads=[yT_d[m]], writes=[(yT[m], c)])
                p.fence(yT[m])
            for dc in range(8):
                wg_ = wg[wcnt % 2]
                wb_ = wb[wcnt % 2]
                wcnt += 1
                for m in active:
                    load_w(wg_, lambda sv, m=m, wg_=wg_: [(wg_[:, :, m * 128:(m + 1) * 128], sv)],
                           win_cols(l, OFF_GATE + m * 1024 + dc * 128, 128), (8, 128), key=m)
                    load_w(wb_, lambda sv, m=m, wb_=wb_: [(wb_[:, :, m * 128:(m + 1) * 128], sv)],
                           brw[m][l, :, dc * 128:(dc + 1) * 128].rearrange("(c p) n -> p c n", p=128), (4, 128), key=m)
                for bb in range(2):
                    b = th * 2 + bb
                    acc_ = acc[cnt % 2]
                    cnt += 1
                    for mi, m in enumerate(active):
                        pg = nps()
                        proj_fm(pg[:, :], pg, wg_, lambda c, m=m, wg_=wg_: wg_[:, c, m * 128:(m + 1) * 128], b * 512, 512, wkey=m)
                        sg_ = sg[mi % 2]
                        p.op("scalar", lambda e, sg_=sg_, pg=pg: e.activation(sg_[:, :], pg[:, :], AF.Sigmoid), reads=[pg], writes=[sg_])
                        pb_ = nps()
                        for kc in range(4):
                            p.op("tensor", lambda e, pb_=pb_, kc=kc, m=m, wb_=wb_, bb=bb, yTm=yT[m]: e.matmul(
                                pb_[:, :], wb_[:, kc, m * 128:(m + 1) * 128], yTm[:, kc, bb * 512:(bb + 1) * 512], start=(kc == 0), stop=(kc == 3)),
                                 reads=[(wb_, m), yT[m]], writes=[pb_])
                        last = (mi == len(active) - 1)
                        if mi == 0:
                            dst = mergedT[:, dc, b * 512:(b + 1) * 512] if last else acc_[:, :]
                            p.op("vector", lambda e, dst=dst, pb_=pb_, sg_=sg_: e.tensor_tensor(dst, pb_[:, :], sg_[:, :], ALU.mult),
                                 reads=[pb_, sg_], writes=[(mergedT, (dc, b)) if last else acc_])
                        else:
                            tm_ = tm[mi % 2]
                            p.op("vector", lambda e, tm_=tm_, pb_=pb_, sg_=sg_: e.tensor_tensor(tm_[:, :], pb_[:, :], sg_[:, :], ALU.mult),
                                 reads=[pb_, sg_], writes=[tm_])
                            dst = mergedT[:, dc, b * 512:(b + 1) * 512] if last else acc_[:, :]
                            p.op("vector", lambda e, dst=dst, tm_=tm_, acc_=acc_: e.tensor_tensor(dst, tm_[:, :], acc_[:, :], ALU.add),
                                 reads=[tm_, acc_], writes=[(mergedT, (dc, b)) if last else acc_])
        p.fence(mergedT)
        p.release(mk_w)
        wo = p.carve([128, 8, D], BF16, "wo")
        load_w_plain(wo, wo, W["w_out"][l].rearrange("(c p) n -> p c n", p=128), (8, D))
        xt = [p.carve([128, D], F32, "xt%d" % i) for i in range(2)]
        src = x_in if l == 0 else xs
        for i in range(NT):
            x_ = xt[i % 2]
            p.dma(x_[:, :], src[i * 128:(i + 1) * 128, :], reads=[(src, i)], writes=[x_])
            for hf in range(2):
                po = nps()
                for kc in range(8):
                    p.op("tensor", lambda e, po=po, kc=kc, i=i, hf=hf: e.matmul(po[:, :], mergedT[:, kc, i * 128:(i + 1) * 128], wo[:, kc, hf * 512:(hf + 1) * 512],
                                                                              start=(kc == 0), stop=(kc == 7)),
                         reads=[mergedT, wo], writes=[po])
                p.op("vector", lambda e, x_=x_, po=po, hf=hf: e.tensor_tensor(x_[:, hf * 512:(hf + 1) * 512], x_[:, hf * 512:(hf + 1) * 512], po[:, :], ALU.add),
                     reads=[po, x_], writes=[x_])
            p.dma(xs[i * 128:(i + 1) * 128, :], x_[:, :], reads=[x_], writes=[(xs, i)])
            if debug and l == 0:
                p.dma(dbg["x_mix"][i * 128:(i + 1) * 128, :], x_[:, :], reads=[x_], writes=[(dbg["x_mix"], i)], is_output=True)
        p.release(mk)

    def phase_ffn_ple(l, last):
        mk = p.mark()
        xa = p.carve([128, NT, D], F32, "xacc")
        for i in range(NT):
            p.dma(xa[:, i, :], xs[i * 128:(i + 1) * 128, :], reads=[(xs, i)], writes=[(xa, i)])
        is_moe = (l % 2 == 1)
        j = l // 2
        rw = None
        if is_moe:
            rw = p.carve([128, NT, 8], F32, "rw")
        mk_r = p.mark()
        if is_moe:
            rt = p.carve([128, 8, 8], F32, "router")
            p.dma(rt[:, :, :], W["moe_router"][j].rearrange("(c p) n -> p c n", p=128), writes=[rt])
            hf32 = [p.carve([128, D], F32, "hf32_%d" % i) for i in range(2)]
            hTf = [p.carve([128, 8, 128], F32, "hTf%d" % i) for i in range(2)]
            m8 = [p.carve([128, 8], F32, "m8_%d" % i) for i in range(2)]
            lg = [p.carve([128, 8], F32, "lg_%d" % i) for i in range(2)]
            wv = [p.carve([128, 4], F32, "wv_%d" % i) for i in range(2)]
            e1 = [p.carve([128, 8], F32, "e1_%d" % i) for i in range(2)]
            grow = 0 + ROW_NORM_FFN

            def also(i, s_, xb, xap, xk):
                hf_ = hf32[i % 2]
                p.op("vector", lambda e: e.scalar_tensor_tensor(out=hf_[:], in0=xap, scalar=s_[:, 2:3], in1=rows[:, grow:grow + D], op0=ALU.mult, op1=ALU.mult),
                     reads=[(xb, xk), s_, rows], writes=[hf_])
                hT_ = hTf[i % 2]
                for half in range(2):
                    pp = nps()
                    for c in range(4):
                        cc = half * 4 + c
                        p.op("tensor", lambda e, pp=pp, c=c, cc=cc: e.transpose(pp[:, c * 128:(c + 1) * 128], hf_[:, cc * 128:(cc + 1) * 128], identf[:]),
                             reads=[hf_, identf], writes=[pp])
                    p.op("vector", lambda e, pp=pp, half=half: e.tensor_copy(hT_[:, half * 4:(half + 1) * 4, :], pp[:, :].rearrange("p (c t) -> p c t", c=4)),
                         reads=[pp], writes=[(hT_, half)])
                p.fence(hT_)
                pl = nps()
                for c in range(8):
                    p.op("tensor", lambda e, c=c, pl=pl: e.matmul(pl[:, 0:8], hT_[:, c, :], rt[:, c, :], start=(c == 0), stop=(c == 7)),
                         reads=[hT_, rt], writes=[pl])
                lg_ = lg[i % 2]
                m8_ = m8[i % 2]
                wv_ = wv[i % 2]
                e1_ = e1[i % 2]
                p.op("vector", lambda e: e.tensor_copy(lg_[:, :], pl[:, 0:8]), reads=[pl], writes=[lg_])
                p.op("vector", lambda e: e.max(m8_[:, :], lg_[:, :]), reads=[lg_], writes=[m8_])
                p.op("vector", lambda e: e.tensor_tensor(wv_[:, 0:1], m8_[:, 0:1], m8_[:, 1:2], ALU.subtract), reads=[m8_], writes=[wv_])
                p.op("scalar", lambda e: e.activation(wv_[:, 1:2], wv_[:, 0:1], AF.Sigmoid), reads=[wv_], writes=[wv_])
                p.op("scalar", lambda e: e.activation(wv_[:, 2:3], wv_[:, 0:1], AF.Sigmoid, scale=-1.0), reads=[wv_], writes=[wv_])
                p.op("vector", lambda e: e.tensor_scalar(e1_[:, :], lg_[:, :], m8_[:, 0:1], wv_[:, 1:2], ALU.is_equal, ALU.mult),
                     reads=[lg_, m8_, wv_], writes=[e1_])
                p.op("vector", lambda e: e.tensor_scalar(rw[:, i, :], lg_[:, :], m8_[:, 1:2], wv_[:, 2:3], ALU.is_equal, ALU.mult),
                     reads=[lg_, m8_, wv_], writes=[(rw, i)])
                p.op("vector", lambda e: e.tensor_tensor(rw[:, i, :], rw[:, i, :], e1_[:, :], ALU.add), reads=[e1_, (rw, i)], writes=[(rw, i)])
        else:
            also = None
        rmsnorm_to_hT(lambda i: (xa, xa[:, i, :], i), 0 + ROW_NORM_FFN, also)
        if is_moe:
            p.fence(rw)
        p.release(mk_r)
        mk2 = p.mark()
        FC = 256
        nfc = D_FF // FC
        wgb = [p.carve([128, 8, FC], BF16, "fwg%d" % i) for i in range(2)]
        wub = [p.carve([128, 8, FC], BF16, "fwu%d" % i) for i in range(2)]
        wdb = [p.carve([128, 2, D], BF16, "fwd%d" % i) for i in range(2)]
        hid = [p.carve([128, 2, T], BF16, "hid%d" % i) for i in range(2)]
        sl = [p.carve([128, 512], F32, "sl%d" % i) for i in range(2)]
        experts = range(8) if is_moe else [None]
        it = 0
        for ex in experts:
            if is_moe:
                Wg, Wu, Wd = W["moe_w_gate"][j, ex], W["moe_w_up"][j, ex], W["moe_w_down"][j, ex]
            else:
                Wg, Wu, Wd = W["ffn_w_gate"][j], W["ffn_w_up"][j], W["ffn_w_down"][j]
            for fc in range(nfc):
                wg_, wu_, wd_, hid_ = wgb[it % 2], wub[it % 2], wdb[it % 2], hid[it % 2]
                it += 1
                load_w(wg_, lambda sv, wg_=wg_: [(wg_[:, :, :], sv)], Wg[:, fc * FC:(fc + 1) * FC].rearrange("(c p) n -> p c n", p=128), (8, FC))
                load_w(wu_, lambda sv, wu_=wu_: [(wu_[:, :, :], sv)], Wu[:, fc * FC:(fc + 1) * FC].rearrange("(c p) n -> p c n", p=128), (8, FC))
                load_w(wd_, lambda sv, wd_=wd_: [(wd_[:, :, :], sv)], Wd[fc * FC:(fc + 1) * FC, :].rearrange("(c p) n -> p c n", p=128), (2, D))
                cnt = 0
                for fs in range(2):
                    for b in range(4):
                        pg = nps()
                        pu = nps()
                        proj_fm(pg[:, :], pg, wg_, lambda c, wg_=wg_, fs=fs: wg_[:, c, fs * 128:(fs + 1) * 128], b * 512, 512)
                        proj_fm(pu[:, :], pu, wu_, lambda c, wu_=wu_, fs=fs: wu_[:, c, fs * 128:(fs + 1) * 128], b * 512, 512)
                        sl_ = sl[cnt % 2]
                        cnt += 1
                        p.op("scalar", lambda e, sl_=sl_, pg=pg: e.activation(sl_[:, :], pg[:, :], AF.Silu), reads=[pg], writes=[sl_])
                        p.op("vector", lambda e, sl_=sl_, pu=pu, hid_=hid_, fs=fs, b=b: e.tensor_tensor(hid_[:, fs, b * 512:(b + 1) * 512], sl_[:, :], pu[:, :], ALU.mult),
                             reads=[sl_, pu], writes=[(hid_, (fs, b))])
                p.fence(hid_)
                for i in range(NT):
                    for hf in range(2):
                        pd = nps()
                        for fs in range(2):
                            p.op("tensor", lambda e, pd=pd, fs=fs, i=i, hf=hf, hid_=hid_, wd_=wd_: e.matmul(
                                pd[:, :], hid_[:, fs, i * 128:(i + 1) * 128], wd_[:, fs, hf * 512:(hf + 1) * 512], start=(fs == 0), stop=(fs == 1)),
                                 reads=[hid_, wd_], writes=[pd])
                        xv = xa[:, i, hf * 512:(hf + 1) * 512]
                        if is_moe:
                            p.op("vector", lambda e, xv=xv, pd=pd, i=i, ex=ex: e.scalar_tensor_tensor(out=xv, in0=pd[:, :], scalar=rw[:, i, ex:ex + 1], in1=xv, op0=ALU.mult, op1=ALU.add),
                                 reads=[pd, rw, (xa, i)], writes=[(xa, i)])
                        else:
                            p.op("vector", lambda e, xv=xv, pd=pd: e.tensor_tensor(xv, xv, pd[:, :], ALU.add), reads=[pd, (xa, i)], writes=[(xa, i)])
        p.release(mk2)
        if debug and l == 0:
            for i in range(NT):
                p.dma(dbg["x_ffn"][i * 128:(i + 1) * 128, :], xa[:, i, :], reads=[(xa, i)], writes=[(dbg["x_ffn"], i)], is_output=True)
        mk3 = p.mark()
        wpg = p.carve([128, 8, D], BF16, "wpg")
        wpp = p.carve([128, 2, D], BF16, "wpp")
        load_w_plain(wpg, wpg, W["ple_gate"][l].rearrange("(c p) n -> p c n", p=128), (8, D))
        load_w_plain(wpp, wpp, W["ple_proj"][l].rearrange("(c p) n -> p c n", p=128), (2, D))
        xb16 = [p.carve([128, D], BF16, "xb16_%d" % i) for i in range(2)]
        pf = [p.carve([128, 256], F32, "pf%d" % i) for i in range(2)]
        pb16 = [p.carve([128, 256], BF16, "pb16_%d" % i) for i in range(2)]
        pT = [p.carve([128, 2, 128], BF16, "pT%d" % i) for i in range(2)]
        sgp = [p.carve([128, 512], F32, "sgp%d" % i) for i in range(2)]
        tmp = [p.carve([128, 512], F32, "tmpp%d" % i) for i in range(2)]
        for i in range(NT):
            xb_ = xb16[i % 2]
            p.op("gpsimd", lambda e, xb_=xb_, i=i: e.tensor_copy(xb_[:, :], xa[:, i, :]), reads=[(xa, i)], writes=[xb_])
            pt = npst()
            for c in range(8):
                p.op("tensor", lambda e, pt=pt, c=c, xb_=xb_: e.transpose(pt[:, c * 128:(c + 1) * 128], xb_[:, c * 128:(c + 1) * 128], identb[:]),
                     reads=[xb_, identb], writes=[pt])
            p.op("scalar", lambda e, pt=pt, i=i: e.copy(hT[:, :, i * 128:(i + 1) * 128], pt[:, :].rearrange("p (c t) -> p c t", c=8)),
                 reads=[pt], writes=[(hT, i)])
            pf_, pb_, pT_ = pf[i % 2], pb16[i % 2], pT[i % 2]
            p.dma(pf_[:, :], p_in[l, i * 128:(i + 1) * 128, :], writes=[pf_])
            p.op("gpsimd", lambda e, pf_=pf_, pb_=pb_: e.tensor_copy(pb_[:, :], pf_[:, :]), reads=[pf_], writes=[pb_])
            pt2 = npst()
            for c in range(2):
                p.op("tensor", lambda e, pt2=pt2, c=c, pb_=pb_: e.transpose(pt2[:, c * 128:(c + 1) * 128], pb_[:, c * 128:(c + 1) * 128], identb[:]),
                     reads=[pb_, identb], writes=[pt2])
            p.op("vector", lambda e, pt2=pt2, pT_=pT_: e.tensor_copy(pT_[:, :, :], pt2[:, 0:256].rearrange("p (c t) -> p c t", c=2)), reads=[pt2], writes=[pT_])
            for hf in range(2):
                pg = nps()
                proj_tm(pg[:, :], pg, wpg, lambda c, hf=hf: wpg[:, c, hf * 512:(hf + 1) * 512], i)
                pq = nps()
                for c in range(2):
                    p.op("tensor", lambda e, pq=pq, c=c, pT_=pT_, hf=hf: e.matmul(pq[:, :], pT_[:, c, :], wpp[:, c, hf * 512:(hf + 1) * 512], start=(c == 0), stop=(c == 1)),
                         reads=[pT_, wpp], writes=[pq])
                sg_, tm_ = sgp[hf], tmp[hf]
                p.op("scalar", lambda e, sg_=sg_, pg=pg: e.activation(sg_[:, :], pg[:, :], AF.Sigmoid), reads=[pg], writes=[sg_])
                p.op("vector", lambda e, tm_=tm_, sg_=sg_, pq=pq: e.tensor_tensor(tm_[:, :], sg_[:, :], pq[:, :], ALU.mult), reads=[sg_, pq], writes=[tm_])
                xv = xa[:, i, hf * 512:(hf + 1) * 512]
                p.op("vector", lambda e, xv=xv, tm_=tm_: e.tensor_tensor(xv, xv, tm_[:, :], ALU.add), reads=[tm_, (xa, i)], writes=[(xa, i)])
        p.release(mk3)
        if not last:
            for i in range(NT):
                p.dma(xs[i * 128:(i + 1) * 128, :], xa[:, i, :], reads=[(xa, i)], writes=[(xs, i)])
                if debug and l == 0:
                    p.dma(dbg["x_l0"][i * 128:(i + 1) * 128, :], xa[:, i, :], reads=[(xa, i)], writes=[(dbg["x_l0"], i)], is_output=True)
        else:
            mk4 = p.mark()
            junk = p.carve([128, D], F32, "fjunk")
            ob = [p.carve([128, D], F32, "fo%d" % i) for i in range(2)]
            st = [p.carve([128, 4], F32, "fst%d" % i) for i in range(2)]
            for i in range(NT):
                s_, o_ = st[i % 2], ob[i % 2]
                p.op("scalar", lambda e, s_=s_, i=i: e.activation(junk[:], xa[:, i, :], AF.Square, accum_out=s_[:, 0:1]), reads=[(xa, i)], writes=[junk, s_])
                p.op("scalar", lambda e, s_=s_: e.activation(s_[:, 1:2], s_[:, 0:1], AF.Sqrt, bias=epsc[:, 0:1], scale=1.0 / D), reads=[s_, epsc], writes=[s_])
                p.op("vector", lambda e, s_=s_: e.reciprocal(s_[:, 2:3], s_[:, 1:2]), reads=[s_], writes=[s_])
                p.op("vector", lambda e, s_=s_, o_=o_, i=i: e.scalar_tensor_tensor(out=o_[:], in0=xa[:, i, :], scalar=s_[:, 2:3], in1=rowsf[:, 0:D], op0=ALU.mult, op1=ALU.mult),
                     reads=[(xa, i), s_, rowsf], writes=[o_])
                p.dma(out_d[i * 128:(i + 1) * 128, :], o_[:, :], reads=[o_], writes=[(out_d, i)], is_output=True)
            p.release(mk4)
        p.release(mk)

    MIX = {"swa": (3, mixer_swa), "ssd": (0, mixer_ssd), "nsa": (1, mixer_nsa), "rwkv": (2, mixer_rwkv)}
    for l in range(n_layers):
        src = x_in if l == 0 else xs
        p.dma(rows[:], rows_in[:, l * ROW_PER_LAYER:(l + 1) * ROW_PER_LAYER], writes=[rows])
        mk = p.mark()
        xt = [p.carve([128, D], F32, "xin%d" % i) for i in range(2)]

        def src_tile(i, src=src, xt=xt):
            x_ = xt[i % 2]
            p.dma(x_[:, :], src[i * 128:(i + 1) * 128, :], reads=[(src, i)], writes=[x_])
            return (x_, x_[:, :], None)
        rmsnorm_to_hT(src_tile, 0 + ROW_NORM_MIX)
        p.release(mk)
        active = []
        for name in mixers:
            m, fn = MIX[name]
            fn(l)
            active.append(m)
            if debug and l == 0:
                for c in range(4):
                    p.dma(dbg["yT%d" % m][c, :, :], yT_d[m][c, :, :], reads=[yT_d[m]], writes=[(dbg["yT%d" % m], c)], is_output=True)
        phase_merge(l, sorted(active))
        phase_ffn_ple(l, last=(l == n_layers - 1))
    return p.finish()


def make_in_maps(inp, n_cores=8):
    consts = build_consts()
    rows = build_rows(inp)
    cols = build_cols(inp)
    inp = dict(inp)
    inp["nsa_peT"] = np.ascontiguousarray(np.asarray(inp["nsa_cmp_pe"]).transpose(0, 1, 3, 2))
    shared = {k: np.ascontiguousarray(np.asarray(inp[k], dtype=np.float32)) for k in WEIGHT_SPECS}
    maps = []
    for b in range(n_cores):
        m = dict(shared)
        m.update(consts)
        m["rows"] = rows
        m["cols"] = cols
        m["x"] = np.ascontiguousarray(inp["x"][b])
        m["p"] = np.ascontiguousarray(inp["p"][:, b])
        m["pos"] = np.ascontiguousarray(np.broadcast_to(np.asarray(inp["positions"][b], dtype=np.int32)[None, :], (64, T)))
        maps.append(m)
    return maps


def kernel(**inputs):
    inp = {k: np.asarray(v) for k, v in inputs.items()}
    nc = build()
    maps = make_in_maps(inp)
    res = run_bass_kernel_spmd(nc, maps, core_ids=list(range(8)))
    return np.stack([r["out"] for r in res.results], axis=0).astype(np.float32)
```

```python
import types
import numpy as np
import ml_dtypes
import concourse.bass as bass
import concourse.mybir as mybir
from concourse.bass_utils import run_bass_kernel_spmd

F32 = mybir.dt.float32
BF16 = mybir.dt.bfloat16
I32 = mybir.dt.int32
AF = mybir.ActivationFunctionType
ALU = mybir.AluOpType
AX = mybir.AxisListType

ENGS = ["sync", "scalar", "vector", "gpsimd", "tensor"]
N_DMA_SEMS = 48

T = 2048
D = 1024
NT = 16
DEPTH = 2
D_IN = 9504
D_FF = 2816
NEGB = -30000.0
OFF_Z = 0
OFF_XBC = 512
OFF_DT = 1536
OFF_NQ = 1544
OFF_NKV = 2056
OFF_NG = 2824
OFF_RW = 2848
OFF_SQ = 4640
OFF_SKV = 5152
OFF_GATE = 5408


class Buf:
    def __init__(self, t, name):
        self.t = t
        self.name = name
        self.tr = {}

    def __getitem__(self, idx):
        return self.t[idx]

    def _entries(self, key):
        if key is None:
            if None not in self.tr:
                self.tr[None] = [[], []]
            return list(self.tr.values())
        out = []
        if None in self.tr:
            out.append(self.tr[None])
        if key not in self.tr:
            self.tr[key] = [[], []]
        out.append(self.tr[key])
        return out

    def all_deps(self):
        d = []
        for w, r in self.tr.values():
            d.extend(w)
            d.extend(r)
        return d


def _freeze(fn):
    if getattr(fn, "__closure__", None) is None:
        return fn
    cells = []
    for c in fn.__closure__:
        try:
            cells.append(types.CellType(c.cell_contents))
        except ValueError:
            cells.append(c)
    return types.FunctionType(fn.__code__, fn.__globals__, fn.__name__, fn.__defaults__, tuple(cells))


def _compress(lst):
    best = {}
    for s, v in lst:
        if s not in best or best[s] < v:
            best[s] = v
    return list(best.items())


class Prog:
    def __init__(self):
        self.nc = bass.Bass("TRN2", target_bir_lowering=False)
        nc = self.nc
        self.ops = {e: [] for e in ENGS}
        self.cnt = {e: 0 for e in ENGS}
        self.esem = {e: nc.alloc_semaphore("es_" + e) for e in ENGS}
        self.dsem = [nc.alloc_semaphore("ds%d" % i) for i in range(N_DMA_SEMS)]
        self.dcnt = [0] * N_DMA_SEMS
        self.dnext = 0
        self.waited = {e: {} for e in ENGS}
        self.nbuf = 0
        self.out_deps = []
        self.arena = None
        self.aoff = 0
        self.alive = []
        self.retired = []

    def sb(self, shape, dt=F32, name=None):
        self.nbuf += 1
        name = name or "sb%d" % self.nbuf
        return Buf(self.nc.alloc_sbuf_tensor(name, list(shape), dt), name)

    def ps(self, shape, dt=F32, name=None):
        self.nbuf += 1
        name = name or "ps%d" % self.nbuf
        return Buf(self.nc.alloc_psum_tensor(name, list(shape), dt), name)

    def dram(self, name, shape, dt=F32, kind="Internal"):
        return Buf(self.nc.dram_tensor(name, list(shape), dt, kind=kind), name)

    def make_arena(self, nwords):
        self.arena = self.nc.alloc_sbuf_tensor("arena", [128, nwords], F32)
        self.awords = nwords

    def mark(self):
        return self.aoff

    def release(self, mark):
        keep = []
        for s, e, b in self.alive:
            if s >= mark:
                self.retired.append((s, e, _compress(b.all_deps())))
            else:
                keep.append((s, e, b))
        self.alive = keep
        self.aoff = mark
        if len(self.retired) > 64:
            alld = []
            lo = min(r[0] for r in self.retired)
            hi = max(r[1] for r in self.retired)
            for r in self.retired:
                alld.extend(r[2])
            self.retired = [(lo, hi, _compress(alld))]

    def carve(self, shape, dt=F32, name=None):
        esz = 4 if dt in (F32, I32) else 2
        nfree = int(np.prod(shape[1:]))
        nwords = (nfree * esz + 3) // 4
        nwords = (nwords + 7) // 8 * 8
        s = self.aoff
        e = s + nwords
        assert e <= self.awords, "arena overflow %d > %d (%s)" % (e, self.awords, name)
        self.aoff = e
        v = self.arena[0:shape[0], s:s + (nfree * esz + 3) // 4]
        if esz == 2:
            v = v.bitcast(BF16)
            v = v[:, 0:nfree]
        elif dt == I32:
            v = v.bitcast(I32)
        if len(shape) == 3:
            v = v.rearrange("p (a b) -> p a b", a=shape[1])
        elif len(shape) == 4:
            v = v.rearrange("p (a b c) -> p a b c", a=shape[1], b=shape[2])
        self.nbuf += 1
        b = Buf(v, name or "cv%d" % self.nbuf)
        inh = []
        for rs, re, rd in self.retired:
            if rs < e and re > s:
                inh.extend(rd)
        if inh:
            b.tr[None] = [_compress(inh), []]
        self.alive.append((s, e, b))
        return b

    def fence(self, b):
        d = _compress(b.all_deps())
        b.tr = {None: [d, []]}

    def _collect(self, reads, writes):
        deps = []
        for b, k in reads:
            for ent in b._entries(k):
                deps.extend(ent[0])
        for b, k in writes:
            for ent in b._entries(k):
                deps.extend(ent[0])
                deps.extend(ent[1])
        return deps

    def _record(self, reads, writes, tok):
        for b, k in reads:
            if k not in b.tr:
                b.tr[k] = [[], []]
            b.tr[k][1].append(tok)
            if len(b.tr[k][1]) > 48:
                b.tr[k][1] = _compress(b.tr[k][1])
        for b, k in writes:
            if k is None:
                b.tr = {None: [[tok], []]}
            else:
                b.tr[k] = [[tok], []]

    @staticmethod
    def _norm(lst):
        out = []
        for x in lst:
            if isinstance(x, Buf):
                out.append((x, None))
            else:
                out.append(x)
        return out

    def _waits(self, eng, deps):
        need = {}
        for s, v in deps:
            if eng == "tensor" and s == ("e", "tensor"):
                continue
            if self.waited[eng].get(s, 0) >= v:
                continue
            if need.get(s, 0) < v:
                need[s] = v
        for s, v in need.items():
            self.waited[eng][s] = v
        return list(need.items())

    def _sem(self, s):
        return self.esem[s[1]] if s[0] == "e" else self.dsem[s[1]]

    def op(self, eng, fn, reads=(), writes=()):
        reads = self._norm(reads)
        writes = self._norm(writes)
        deps = self._collect(reads, writes)
        waits = self._waits(eng, deps)
        self.cnt[eng] += 1
        tok = (("e", eng), self.cnt[eng])
        self.ops[eng].append((waits, _freeze(fn), ("e", eng), 1))
        self._record(reads, writes, tok)
        return tok

    def dma(self, out, in_, reads=(), writes=(), eng="sync", is_output=False):
        reads = self._norm(reads)
        writes = self._norm(writes)
        deps = self._collect(reads, writes)
        i = self.dnext
        self.dnext = (self.dnext + 1) % N_DMA_SEMS
        if self.dcnt[i] > 0:
            deps.append((("d", i), 16 * self.dcnt[i]))
        waits = self._waits(eng, deps)
        self.dcnt[i] += 1
        tok = (("d", i), 16 * self.dcnt[i])
        fn = lambda e, o=out, a=in_: e.dma_start(out=o, in_=a)
        self.ops[eng].append((waits, fn, ("d", i), 16))
        self._record(reads, writes, tok)
        if is_output:
            self.out_deps.append(tok)
        return tok

    def finish(self):
        nc = self.nc
        final_deps = list(self.out_deps)
        for e in ENGS:
            if self.cnt[e] > 0 and e != "sync":
                final_deps.append((("e", e), self.cnt[e]))
        for i in range(N_DMA_SEMS):
            if self.dcnt[i] > 0:
                final_deps.append((("d", i), 16 * self.dcnt[i]))
        fwaits = self._waits("sync", final_deps)
        ops = self.ops
        sem = self._sem
        with nc.Block() as block:

            def mk(engname):
                def body(e):
                    for waits, fn, s, inc in ops[engname]:
                        for ws, wv in waits:
                            e.wait_ge(sem(ws), wv)
                        ins = fn(e)
                        ins.then_inc(sem(s), inc)
                    if engname == "sync":
                        for ws, wv in fwaits:
                            e.wait_ge(sem(ws), wv)

                return body

            block.sync(mk("sync"))
            block.scalar(mk("scalar"))
            block.vector(mk("vector"))
            block.gpsimd(mk("gpsimd"))
            block.tensor(mk("tensor"))
        return nc


def _mask_tile(W, d):
    m = np.full((128, 512), NEGB, np.float32)
    s = np.arange(128)[:, None]
    t = np.arange(128)[None, :]
    for c in range(4):
        delta = c - d
        blk = m[:, c * 128:(c + 1) * 128]
        if delta < 0 or delta > W:
            continue
        if delta == 0:
            blk[:] = np.where(s <= t, 0.0, NEGB)
        elif delta < W:
            blk[:] = 0.0
        else:
            blk[:] = np.where(s > t, 0.0, NEGB)
    return m


SWA_MASK0 = 0
NSA_MASK0 = 5
N_MASKS = 13


def build_consts():
    c = {}
    masks = [_mask_tile(1, d) for d in range(-1, 4)] + [_mask_tile(4, d) for d in range(-4, 4)]
    c["c_masks"] = np.stack(masks).transpose(1, 0, 2).astype(ml_dtypes.bfloat16)
    c["c_identb"] = np.eye(128, dtype=np.float32).astype(ml_dtypes.bfloat16)
    c["c_identf"] = np.eye(128, dtype=np.float32)
    half = 32
    inv = (150000.0 ** (-np.arange(half, dtype=np.float32) / half)).astype(np.float32)
    rc = np.zeros((64, 2), np.float32)
    rc[:, 0] = np.concatenate([inv, inv])
    rc[:, 1] = np.concatenate([-np.ones(32), np.ones(32)])
    c["c_rope"] = rc
    i_ = np.arange(128)[:, None]
    j_ = np.arange(128)[None, :]
    ssd = np.zeros((128, 5, 128), np.float32)
    ssd[:, 4] = (i_ < j_)
    ssd[:, 0] = (i_ <= j_)
    ssd[:, 1] = (i_ > j_)
    ssd[:, 2] = np.where(j_ >= i_, 0.0, NEGB)
    ssd[:, 3] = 1.0
    c["c_ssd"] = ssd
    rw = np.zeros((128, 642), np.float32)
    rw[0:64, 0:64] = 1.0
    rw[64:128, 64:128] = 1.0
    rw[0:64, 128] = 1.0
    rw[64:128, 129] = 1.0
    cmk = np.ones((512,), np.float32)
    cmk[::128] = 0.0
    rw[:, 130:642] = cmk[None, :]
    c["c_rw"] = rw
    n_ = np.arange(128)[:, None]
    t_ = np.arange(T)[None, :]
    c["c_cmpmask"] = np.where((16 * n_ + 31 <= t_) & (n_ < 127), 0.0, NEGB).astype(np.float32).astype(ml_dtypes.bfloat16)
    E = np.zeros((32, 16, 128), np.float32)
    for k in range(16):
        for s_ in range(128):
            E[2 * k + s_ // 64, k, s_] = 1.0
    c["c_E"] = E.astype(ml_dtypes.bfloat16)
    sel = np.zeros((24, 24, 128), np.float32)
    for i in range(24):
        sel[i, i, :] = 1.0
    c["c_sel"] = sel.astype(ml_dtypes.bfloat16)
    jj = np.arange(32)[None, :]
    cs = (16 * np.arange(128))[:, None]
    ovl = ((cs < 64 * jj + 64) & (cs + 32 > 64 * jj)).astype(np.float32)
    ovl[127, :] = 0.0
    c["c_ovl"] = ovl
    tt = np.arange(T)[:, None]
    blk = tt // 64
    valid = jj <= blk
    forced = (jj == 0) | (jj == blk)
    vmul = (valid & ~forced).astype(np.float32)
    amask = np.where(forced, 1e30, np.where(valid, 0.0, -1e30)).astype(np.float32)
    c["c_vmul"] = np.ascontiguousarray(vmul.reshape(16, 128, 32).transpose(1, 0, 2))
    c["c_amask"] = np.ascontiguousarray(amask.reshape(16, 128, 32).transpose(1, 0, 2))
    return c


CONST_SPECS = {
    "c_masks": ([128, N_MASKS, 512], BF16),
    "c_identb": ([128, 128], BF16),
    "c_identf": ([128, 128], F32),
    "c_rope": ([64, 2], F32),
    "c_ssd": ([128, 5, 128], F32),
    "c_rw": ([128, 642], F32),
    "c_cmpmask": ([128, T], BF16),
    "c_E": ([32, 16, 128], BF16),
    "c_sel": ([24, 24, 128], BF16),
    "c_ovl": ([128, 32], F32),
    "c_vmul": ([128, 16, 32], F32),
    "c_amask": ([128, 16, 32], F32),
}

ROW_NORM_MIX = 0
ROW_NORM_FFN = 1024
ROW_SINKS = 2048
ROW_DTB = 2056
ROW_ALOG = 2064
ROW_DSK = 2072
ROW_SSDN = 2080
ROW_LNW = 2592
ROW_LNB = 3104
ROW_PER_LAYER = 3616
COL_CONVW = 0
COL_CONVB = 32
COL_MU = 40
COL_W0 = 54
COL_A0 = 58
COL_KK = 62
COL_KA = 66
COL_RK = 70
COL_PER_LAYER = 80
ROW_FINAL = DEPTH * ROW_PER_LAYER
ROW_TOTAL = ROW_FINAL + 1024


def build_rows(inp):
    rows = np.zeros((ROW_TOTAL,), np.float32)
    for l in range(DEPTH):
        o = l * ROW_PER_LAYER
        rows[o + ROW_NORM_MIX:o + ROW_NORM_MIX + 1024] = inp["norm_mix"][l]
        rows[o + ROW_NORM_FFN:o + ROW_NORM_FFN + 1024] = inp["norm_ffn"][l]
        rows[o + ROW_SINKS:o + ROW_SINKS + 8] = inp["swa_sinks"][l]
        rows[o + ROW_DTB:o + ROW_DTB + 8] = inp["ssd_dt_bias"][l]
        rows[o + ROW_ALOG:o + ROW_ALOG + 8] = inp["ssd_a_log"][l]
        rows[o + ROW_DSK:o + ROW_DSK + 8] = inp["ssd_d"][l]
        rows[o + ROW_SSDN:o + ROW_SSDN + 512] = inp["ssd_norm"][l]
        rows[o + ROW_LNW:o + ROW_LNW + 512] = inp["rwkv_ln_w"][l]
        rows[o + ROW_LNB:o + ROW_LNB + 512] = inp["rwkv_ln_b"][l]
    rows[ROW_FINAL:ROW_FINAL + 1024] = inp["norm_final"]
    return np.ascontiguousarray(np.broadcast_to(rows[None, :], (128, ROW_TOTAL)))


def build_cols(inp):
    cols = np.zeros((128, DEPTH * COL_PER_LAYER), np.float32)
    for l in range(DEPTH):
        o = l * COL_PER_LAYER
        cw = np.asarray(inp["ssd_conv_w"][l])
        cols[:, o + COL_CONVW:o + COL_CONVW + 32] = cw.reshape(4, 8, 128).transpose(2, 1, 0).reshape(128, 32)
        cols[:, o + COL_CONVB:o + COL_CONVB + 8] = np.asarray(inp["ssd_conv_b"][l]).reshape(8, 128).T
        cols[:, o + COL_MU:o + COL_MU + 14] = np.asarray(inp["rwkv_mu"][l]).reshape(14, 128).T
        for nm, co in (("rwkv_w0", COL_W0), ("rwkv_a0", COL_A0), ("rwkv_k_k", COL_KK), ("rwkv_k_a", COL_KA), ("rwkv_r_k", COL_RK)):
            cols[:, o + co:o + co + 4] = np.asarray(inp[nm][l]).reshape(4, 128).T
    return np.ascontiguousarray(cols)


WEIGHT_SPECS = {
    "w_in": [DEPTH, D, D_IN],
    "w_br_ssd": [DEPTH, 512, D],
    "w_br_nsa": [DEPTH, 512, D],
    "w_br_rwkv": [DEPTH, 512, D],
    "w_br_swa": [DEPTH, 512, D],
    "w_out": [DEPTH, D, D],
    "ffn_w_gate": [1, D, D_FF],
    "ffn_w_up": [1, D, D_FF],
    "ffn_w_down": [1, D_FF, D],
    "moe_router": [1, D, 8],
    "moe_w_gate": [1, 8, D, D_FF],
    "moe_w_up": [1, 8, D, D_FF],
    "moe_w_down": [1, 8, D_FF, D],
    "ple_proj": [DEPTH, 256, D],
    "ple_gate": [DEPTH, D, D],
    "nsa_cmp_w1": [DEPTH, 2, 32, 64, 64],
    "nsa_cmp_w2": [DEPTH, 2, 64, 64],
    "nsa_peT": [DEPTH, 2, 64, 32],
    "rwkv_w_up": [DEPTH, 64, 512],
    "rwkv_a_up": [DEPTH, 64, 512],
    "rwkv_g_up": [DEPTH, 128, 512],
}


def build(n_layers=DEPTH, mixers=("ssd", "nsa", "rwkv", "swa"), debug=False):
    p = Prog()
    nc = p.nc
    EI = "ExternalInput"
    x_in = p.dram("x", [T, D], F32, EI)
    p_in = p.dram("p", [DEPTH, T, 256], F32, EI)
    pos_in = p.dram("pos", [64, T], I32, EI)
    rows_in = p.dram("rows", [128, ROW_TOTAL], F32, EI)
    cols_in = p.dram("cols", [128, DEPTH * COL_PER_LAYER], F32, EI)
    W = {k: p.dram(k, shp, F32, EI) for k, shp in WEIGHT_SPECS.items()}
    C = {k: p.dram(k, shp, dt, EI) for k, (shp, dt) in CONST_SPECS.items()}
    out_d = p.dram("out", [T, D], F32, "ExternalOutput")
    xs = p.dram("xs", [T, D], F32)
    yT_d = [p.dram("yT%d" % m, [4, 128, T], BF16) for m in range(4)]
    dbg = {}
    if debug:
        for m in range(4):
            dbg["yT%d" % m] = p.dram("dbg_yT%d" % m, [4, 128, T], BF16, "ExternalOutput")
        dbg["x_mix"] = p.dram("dbg_x_mix", [T, D], F32, "ExternalOutput")
        dbg["x_ffn"] = p.dram("dbg_x_ffn", [T, D], F32, "ExternalOutput")
        dbg["x_l0"] = p.dram("dbg_x_l0", [T, D], F32, "ExternalOutput")

    def dump(name, ap, shape, dt, reads):
        if not debug:
            return
        d = p.dram("dbg_" + name, shape, dt, "ExternalOutput")
        p.dma(d.t.ap() if False else d[tuple(slice(None) for _ in shape)], ap, reads=reads, writes=[d], is_output=True)

    masks = p.sb([128, N_MASKS, 512], BF16, "masks")
    identb = p.sb([128, 128], BF16, "identb")
    identf = p.sb([128, 128], F32, "identf")
    ropec = p.sb([64, 2], F32, "ropec")
    rows = p.sb([128, ROW_PER_LAYER], F32, "rows_sb")
    rowsf = p.sb([128, 1024], F32, "rowsf_sb")
    hT = p.sb([128, 8, T], BF16, "hT")
    p.dma(masks[:], C["c_masks"][:], writes=[masks])
    p.dma(identb[:], C["c_identb"][:], writes=[identb])
    p.dma(identf[:], C["c_identf"][:], writes=[identf])
    p.dma(ropec[:], C["c_rope"][:], writes=[ropec])
    p.dma(rowsf[:], rows_in[:, ROW_FINAL:ROW_FINAL + 1024], writes=[rowsf])
    cols = p.sb([128, DEPTH * COL_PER_LAYER], F32, "cols_sb")
    p.dma(cols[:], cols_in[:], writes=[cols])
    cssd = p.sb([128, 5, 128], F32, "cssd")
    p.dma(cssd[:], C["c_ssd"][:], writes=[cssd])
    epsc = p.sb([128, 1], F32, "epsc")
    p.op("vector", lambda e: e.memset(epsc[:], 1e-6), writes=[epsc])

    NSTG = 2
    STGW = 2048
    stg = [p.sb([128, STGW], F32, "stg%d" % i) for i in range(NSTG)]
    stgi = [0]

    p.make_arena((nc.sbuf_bytes_remaining - 2048) // 4)

    psb = [p.ps([128, 512], F32, "psb%d" % i) for i in range(6)]
    pst = [p.ps([128, 1024], BF16, "pst%d" % i) for i in range(2)]
    psi = [0]
    psti = [0]

    pli = [0]

    def nps():
        b = psb[psi[0] % 4]
        psi[0] += 1
        return b

    def npl():
        b = psb[4 + pli[0] % 2]
        pli[0] += 1
        return b

    def npst():
        b = pst[psti[0] % 2]
        psti[0] += 1
        return b

    cast_rr = [0]

    def load_w(dst, dst_ap_fn, src_ap, shape, key=None, parts=128):
        a, b = shape
        assert a * b <= STGW, (a, b)
        s = stg[stgi[0] % NSTG]
        stgi[0] += 1
        sv = s[0:parts, 0:a * b].rearrange("p (a b) -> p a b", a=a)
        p.dma(sv, src_ap, writes=[s])
        for ent in dst_ap_fn(sv):
            if len(ent) == 3:
                dbuf, o_ap, i_ap = ent
            else:
                dbuf = dst
                o_ap, i_ap = ent
            eng = "gpsimd"
            p.op(eng, lambda e, o=o_ap, i=i_ap: e.tensor_copy(o, i), reads=[s], writes=[(dbuf, key)])

    def load_w_plain(dst, dview, src_ap, shape, key=None):
        a, b = shape
        bc = max(1, STGW // a)
        for b0 in range(0, b, bc):
            bw = min(bc, b - b0)
            load_w(dst, lambda sv, b0=b0, bw=bw: [(dview[:, :, b0:b0 + bw], sv)],
                   src_ap[:, :, b0:b0 + bw], (a, bw), key)

    def win_cols(l, c0, n):
        return W["w_in"][l, :, c0:c0 + n].rearrange("(c p) n -> p c n", p=128)

    def rmsnorm_to_hT(src_tile_fn, grow0, also=None):
        mk = p.mark()
        hb = [p.carve([128, D], BF16, "hb%d" % i) for i in range(2)]
        junk = p.carve([128, D], F32, "junk")
        st = [p.carve([128, 4], F32, "st%d" % i) for i in range(2)]
        for i in range(NT):
            xb, xap, xk = src_tile_fn(i)
            s_ = st[i % 2]
            h_ = hb[i % 2]
            p.op("scalar", lambda e, xap=xap, s_=s_: e.activation(junk[:], xap, AF.Square, accum_out=s_[:, 0:1]),
                 reads=[(xb, xk)], writes=[junk, s_])
            p.op("scalar", lambda e, s_=s_: e.activation(s_[:, 1:2], s_[:, 0:1], AF.Sqrt, bias=epsc[:, 0:1], scale=1.0 / D),
                 reads=[s_, epsc], writes=[s_])
            p.op("vector", lambda e, s_=s_: e.reciprocal(s_[:, 2:3], s_[:, 1:2]), reads=[s_], writes=[s_])
            p.op("vector", lambda e, xap=xap, s_=s_, h_=h_: e.scalar_tensor_tensor(
                out=h_[:], in0=xap, scalar=s_[:, 2:3], in1=rows[:, grow0:grow0 + D], op0=ALU.mult, op1=ALU.mult),
                 reads=[(xb, xk), s_, rows], writes=[h_])
            if also is not None:
                also(i, s_, xb, xap, xk)
            pt = npst()
            for c in range(8):
                p.op("tensor", lambda e, pt=pt, c=c, h_=h_: e.transpose(pt[:, c * 128:(c + 1) * 128], h_[:, c * 128:(c + 1) * 128], identb[:]),
                     reads=[h_, identb], writes=[pt])
            eng = "vector" if i % 2 == 0 else "scalar"
            dstv = hT[:, :, i * 128:(i + 1) * 128]
            srcv = pt[:, :].rearrange("p (c t) -> p c t", c=8)
            if eng == "vector":
                p.op("vector", lambda e, dstv=dstv, srcv=srcv: e.tensor_copy(dstv, srcv), reads=[pt], writes=[(hT, i)])
            else:
                p.op("scalar", lambda e, dstv=dstv, srcv=srcv: e.copy(dstv, srcv), reads=[pt], writes=[(hT, i)])
        p.release(mk)

    def hT_reads(t0, tw):
        return [(hT, i) for i in range(t0 // 128, (t0 + tw + 127) // 128)]

    def proj_fm(ps_ap, psbuf, wbuf, wview_fn, t0, tw, wkey=None):
        for c in range(8):
            p.op("tensor", lambda e, c=c: e.matmul(ps_ap, wview_fn(c), hT[:, c, t0:t0 + tw], start=(c == 0), stop=(c == 7)),
                 reads=[(wbuf, wkey)] + hT_reads(t0, tw), writes=[psbuf])

    def proj_tm(ps_ap, psbuf, wbuf, wview_fn, i, wkey=None):
        for c in range(8):
            p.op("tensor", lambda e, c=c: e.matmul(ps_ap, hT[:, c, i * 128:(i + 1) * 128], wview_fn(c), start=(c == 0), stop=(c == 7)),
                 reads=[(wbuf, wkey), (hT, i)], writes=[psbuf])

    def attn_group(qT, qkey, h, b, kT, g, vext, pairs, PT, den_extra_col, out_cb):
        ps2 = npl()
        n = len(pairs)

        def emit_S(idx):
            j, extra = pairs[idx]
            ps1 = nps()
            nx = len(extra)
            p.op("tensor", lambda e: e.matmul(ps1[:, :], kT[0:64, g, j * 128:(j + 1) * 128], qT[0:64, h, b * 512:(b + 1) * 512],
                                              start=True, stop=(nx == 0)),
                 reads=[(kT, None), (qT, qkey)], writes=[ps1])
            for xi, (la, ra, rd) in enumerate(extra):
                p.op("tensor", lambda e: e.matmul(ps1[:, :], la, ra, start=False, stop=(xi == nx - 1)), reads=rd, writes=[ps1])
            pt_ = PT[idx % len(PT)]
            p.op("scalar", lambda e: e.activation(pt_[:, :], ps1[:, :], AF.Exp, scale=0.125), reads=[ps1], writes=[pt_])

        def emit_PV(idx):
            j, extra = pairs[idx]
            pt_ = PT[idx % len(PT)]
            p.op("tensor", lambda e: e.matmul(ps2[:, :], vext[:, j, g, :], pt_[:, :], start=(idx == 0), stop=(idx == n - 1)),
                 reads=[pt_, (vext, None)], writes=[ps2])

        emit_S(0)
        if n > 1:
            emit_S(1)
        for idx in range(n):
            if idx + 2 < n:
                emit_S(idx + 2)
            emit_PV(idx)
        out_cb(ps2)

    def mixer_swa(l):
        mk = p.mark()
        qT = p.carve([64, 8, T], BF16, "swa_qT")
        kT = p.carve([64, 2, T], BF16, "swa_kT")
        vext = p.carve([128, NT, 2, 128], BF16, "swa_v")
        cosT = p.carve([64, T], F32, "cosT")
        sinT = p.carve([64, T], F32, "sinT")
        PT = [p.carve([128, 512], BF16, "PT%d" % i) for i in range(3)]
        sinke = p.carve([128, 8], F32, "sinke")
        mk2 = p.mark()
        posi = p.carve([64, T], I32, "posi")
        ang = p.carve([64, T], F32, "ang")
        tmpf = p.carve([64, T], F32, "tmpf")
        tmpi = p.carve([64, T], I32, "tmpi")
        p.dma(posi[:], pos_in[:], writes=[posi])
        p.op("vector", lambda e: e.tensor_copy(ang[:], posi[:]), reads=[posi], writes=[ang])
        p.op("vector", lambda e: e.tensor_scalar(ang[:], ang[:], ropec[:, 0:1], None, ALU.mult), reads=[ang, ropec], writes=[ang])
        TWO_PI = 2.0 * np.pi
        for shift, dst in ((0.0, sinT), (np.pi / 2, cosT)):
            p.op("vector", lambda e, shift=shift: e.tensor_scalar(tmpf[:], ang[:], float(shift), 1.0 / TWO_PI, ALU.add, ALU.mult),
                 reads=[ang], writes=[tmpf])
            p.op("vector", lambda e: e.tensor_copy(tmpi[:], tmpf[:]), reads=[tmpf], writes=[tmpi])
            p.op("vector", lambda e: e.tensor_copy(tmpf[:], tmpi[:]), reads=[tmpi], writes=[tmpf])
            p.op("vector", lambda e: e.scalar_tensor_tensor(out=tmpf[:], in0=tmpf[:], scalar=-TWO_PI, in1=ang[:], op0=ALU.mult, op1=ALU.add),
                 reads=[tmpf, ang], writes=[tmpf])
            p.op("vector", lambda e, shift=shift: e.tensor_scalar(tmpf[:], tmpf[:], float(shift), 3.14159, ALU.add, ALU.min),
                 reads=[tmpf], writes=[tmpf])
            p.op("vector", lambda e: e.tensor_scalar(tmpf[:], tmpf[:], -3.14159, None, ALU.max), reads=[tmpf], writes=[tmpf])
            p.op("scalar", lambda e, dst=dst: e.activation(dst[:], tmpf[:], AF.Sin), reads=[tmpf], writes=[dst])
        p.op("vector", lambda e: e.tensor_scalar(sinT[:], sinT[:], ropec[:, 1:2], None, ALU.mult), reads=[sinT, ropec], writes=[sinT])
        p.release(mk2)
        ro = 0 + ROW_SINKS
        p.op("scalar", lambda e: e.activation(sinke[:], rows[:, ro:ro + 8], AF.Exp), reads=[rows], writes=[sinke])
        wq = p.carve([128, 8, 512], BF16, "swa_wq")
        wqs = p.carve([128, 8, 512], BF16, "swa_wqs")
        wkv = p.carve([128, 8, 256], BF16, "swa_wkv")
        wks = p.carve([128, 8, 128], BF16, "swa_wks")
        for h0 in range(0, 8, 4):
            def cast_q(sv, h0=h0):
                s4 = sv.rearrange("p c (h d) -> p c h d", d=64)
                ops_ = [(wq[:, :, h0 * 64:(h0 + 4) * 64], sv)]
                d4 = wqs[:, :, h0 * 64:(h0 + 4) * 64].rearrange("p c (h d) -> p c h d", d=64)
                ops_.append((wqs, d4[:, :, :, 0:32], s4[:, :, :, 32:64]))
                ops_.append((wqs, d4[:, :, :, 32:64], s4[:, :, :, 0:32]))
                return ops_
            load_w(wq, cast_q, win_cols(l, OFF_SQ + h0 * 64, 256), (8, 256))
        p.fence(wq)

        def cast_kv(sv):
            ops_ = [(wkv[:, :, :], sv)]
            s4 = sv[:, :, 0:128].rearrange("p c (h d) -> p c h d", d=64)
            d4 = wks[:, :, :].rearrange("p c (h d) -> p c h d", d=64)
            ops_.append((wks, d4[:, :, :, 0:32], s4[:, :, :, 32:64]))
            ops_.append((wks, d4[:, :, :, 32:64], s4[:, :, :, 0:32]))
            return ops_
        load_w(wkv, cast_kv, win_cols(l, OFF_SKV, 256), (8, 256))
        t1 = [p.carve([64, 512], F32, "rt1_%d" % i) for i in range(2)]
        t2 = [p.carve([64, 512], F32, "rt2_%d" % i) for i in range(2)]
        cnt = 0
        for dstT, nh, wa, wb, wbufa, wbufb in ((kT, 2, wkv, wks, wkv, wks), (qT, 8, wq, wqs, wq, wqs)):
            for h in range(nh):
                for b in range(4):
                    pa = nps()
                    pb = nps()
                    proj_fm(pa[0:64, :], pa, wbufa, lambda c, wa=wa, h=h: wa[:, c, h * 64:(h + 1) * 64], b * 512, 512)
                    proj_fm(pb[0:64, :], pb, wbufb, lambda c, wb=wb, h=h: wb[:, c, h * 64:(h + 1) * 64], b * 512, 512)
                    a_ = t1[cnt % 2]
                    b_ = t2[cnt % 2]
                    cnt += 1
                    p.op("vector", lambda e, a_=a_, pa=pa, b=b: e.tensor_tensor(a_[:, :], pa[0:64, :], cosT[:, b * 512:(b + 1) * 512], ALU.mult),
                         reads=[pa, cosT], writes=[a_])
                    p.op("vector", lambda e, b_=b_, pb=pb, b=b: e.tensor_tensor(b_[:, :], pb[0:64, :], sinT[:, b * 512:(b + 1) * 512], ALU.mult),
                         reads=[pb, sinT], writes=[b_])
                    p.op("gpsimd", lambda e, a_=a_, b_=b_, dstT=dstT, h=h, b=b: e.tensor_tensor(dstT[:, h, b * 512:(b + 1) * 512], a_[:, :], b_[:, :], ALU.add),
                         reads=[a_, b_], writes=[(dstT, (h, b))])
        p.fence(kT)
        p.op("gpsimd", lambda e: e.memset(vext[:, :, :, 64:128], 1.0), writes=[vext])
        for i in range(NT):
            pv = nps()
            proj_tm(pv[:, 0:128], pv, wkv, lambda c: wkv[:, c, 128:256], i)
            p.op("vector", lambda e, pv=pv, i=i: e.tensor_copy(vext[:, i, :, 0:64], pv[:, 0:128].rearrange("p (g d) -> p g d", g=2)),
                 reads=[pv], writes=[(vext, i)])
        p.fence(vext)
        rec = [p.carve([128, 512], F32, "rec%d" % i) for i in range(2)]
        yo = [p.carve([64, 512], BF16, "yo%d" % i) for i in range(2)]
        cnt = 0
        for h in range(8):
            g = h // 4
            for b in range(4):
                pairs = []
                for j in range(4 * b - 1, 4 * b + 4):
                    if j < 0:
                        continue
                    d = j - 4 * b
                    pairs.append((j, [(identb[:, :], masks[:, SWA_MASK0 + d + 1, :], [identb, masks])]))
                r_ = rec[cnt % 2]
                y_ = yo[cnt % 2]
                cnt += 1

                def fin(ps2, r_=r_, y_=y_, h=h, b=b):
                    p.op("vector", lambda e: e.tensor_scalar(r_[64:128, :], ps2[64:128, :], sinke[64:128, h:h + 1], None, ALU.add),
                         reads=[ps2, sinke], writes=[r_])
                    p.op("vector", lambda e: e.reciprocal(r_[64:128, :], r_[64:128, :]), reads=[r_], writes=[r_])
                    p.op("vector", lambda e: e.tensor_tensor(y_[:, :], ps2[0:64, :], r_[64:128, :], ALU.mult), reads=[ps2, r_], writes=[y_])
                    p.dma(yT_d[3][h // 2, (h % 2) * 64:(h % 2) * 64 + 64, b * 512:(b + 1) * 512], y_[:, :], reads=[y_], writes=[(yT_d[3], (h, b))])
                attn_group(qT, (h, b), h, b, kT, g, vext, pairs, PT, None, fin)
        p.fence(yT_d[3])
        p.release(mk)


    def mixer_nsa(l):
        mk = p.mark()
        cmpmask = p.carve([128, T], BF16, "cmpmask")
        Emat = p.carve([32, 16, 128], BF16, "Emat")
        Sel = p.carve([24, 24, 128], BF16, "Sel")
        ovl = p.carve([128, 32], F32, "ovl")
        vmul = p.carve([128, 16, 32], F32, "vmul")
        amask = p.carve([128, 16, 32], F32, "amask")
        p.dma(cmpmask[:, :], C["c_cmpmask"][:, :], writes=[cmpmask])
        p.dma(Emat[:, :, :], C["c_E"][:, :, :], writes=[Emat])
        p.dma(Sel[:, :, :], C["c_sel"][:, :, :], writes=[Sel])
        p.dma(ovl[:, :], C["c_ovl"][:, :], writes=[ovl])
        p.dma(vmul[:, :, :], C["c_vmul"][:, :, :], writes=[vmul])
        p.dma(amask[:, :, :], C["c_amask"][:, :, :], writes=[amask])
        gT = p.carve([24, T], BF16, "nsa_gT")
        wg = p.carve([128, 8, 24], BF16, "nsa_wg")
        load_w(wg, lambda sv: [(wg[:, :, :], sv)], win_cols(l, OFF_NG, 24), (8, 24))
        for b in range(4):
            pg = nps()
            proj_fm(pg[0:24, :], pg, wg, lambda c: wg[:, c, :], b * 512, 512)
            p.op("scalar", lambda e, pg=pg, b=b: e.activation(gT[:, b * 512:(b + 1) * 512], pg[0:24, :], AF.Sigmoid), reads=[pg], writes=[(gT, b)])
        p.fence(gT)
        w1 = [p.carve([64, 32, 64], BF16, "cw1_%d" % i) for i in range(2)]
        w2 = [p.carve([64, 64], BF16, "cw2_%d" % i) for i in range(2)]
        peT = [p.carve([64, 32], BF16, "cpe_%d" % i) for i in range(2)]
        for kv in range(2):
            load_w(w1[kv], lambda sv, kv=kv: [(w1[kv][:, :, :], sv[0:64, :, :])],
                   W["nsa_cmp_w1"][l, kv].rearrange("l d f -> d l f"), (32, 64), parts=64)
            load_w(w2[kv], lambda sv, kv=kv: [(w2[kv][:, :], sv[0:64, 0, :])], W["nsa_cmp_w2"][l, kv].unsqueeze(1), (1, 64), parts=64)
            load_w(peT[kv], lambda sv, kv=kv: [(peT[kv][:, :], sv[0:64, 0, :])], W["nsa_peT"][l, kv].unsqueeze(1), (1, 32), parts=64)
        qT = p.carve([64, 4, T], BF16, "nsa_qT")
        ycmp = p.carve([64, 4, T], BF16, "nsa_ycmp")
        ksT = p.carve([64, 1, T], BF16, "nsa_ksT")
        kwT = p.carve([64, 1, T], BF16, "nsa_kwT")
        vs = p.carve([128, NT, 1, 128], BF16, "nsa_vs")
        vw = p.carve([128, NT, 1, 128], BF16, "nsa_vw")
        impT = p.carve([32, T], F32, "nsa_impT")
        selbT = p.carve([32, 1, T], BF16, "nsa_selbT")
        kcmpT = p.carve([64, 1, 128], BF16, "nsa_kcmpT")
        vcmp = p.carve([128, 64], BF16, "nsa_vcmp")
        for g in range(2):
            mk_g = p.mark()
            kcvT = p.carve([64, 2, T], BF16, "nsa_kcvT")
            hidb = [p.carve([64, 128], BF16, "nsa_hid%d" % i) for i in range(2)]
            wq = p.carve([128, 8, 256], BF16, "nsa_wq")
            wkv = p.carve([128, 8, 6, 64], BF16, "nsa_wkv")
            load_w(wq, lambda sv: [(wq[:, :, :], sv)], win_cols(l, OFF_NQ + g * 256, 256), (8, 256))
            for part in range(6):
                load_w(wkv, lambda sv, part=part: [(wkv[:, :, part, :], sv)], win_cols(l, OFF_NKV + part * 128 + g * 64, 64), (8, 64), key=part)
            p.fence(wkv)
            cnt = 0
            for hh in range(4):
                for b in range(4):
                    pa = nps()
                    proj_fm(pa[0:64, :], pa, wq, lambda c, hh=hh: wq[:, c, hh * 64:(hh + 1) * 64], b * 512, 512)
                    eng = "vector" if cnt % 2 == 0 else "scalar"
                    cnt += 1
                    if eng == "vector":
                        p.op("vector", lambda e, pa=pa, hh=hh, b=b: e.tensor_copy(qT[:, hh, b * 512:(b + 1) * 512], pa[0:64, :]), reads=[pa], writes=[(qT, (hh, b))])
                    else:
                        p.op("scalar", lambda e, pa=pa, hh=hh, b=b: e.copy(qT[:, hh, b * 512:(b + 1) * 512], pa[0:64, :]), reads=[pa], writes=[(qT, (hh, b))])
            for part, dstT, di in ((0, kcvT, 0), (1, kcvT, 1), (2, ksT, 0), (4, kwT, 0)):
                for b in range(4):
                    pa = nps()
                    proj_fm(pa[0:64, :], pa, wkv, lambda c, part=part: wkv[:, c, part, :], b * 512, 512)
                    p.op("vector", lambda e, pa=pa, dstT=dstT, di=di, b=b: e.tensor_copy(dstT[:, di, b * 512:(b + 1) * 512], pa[0:64, :]), reads=[pa], writes=[(dstT, (di, b))])
            p.fence(kcvT)
            p.fence(ksT)
            p.fence(kwT)
            for part, vdst in ((3, vs), (5, vw)):
                p.op("gpsimd", lambda e, vdst=vdst: e.memset(vdst[:, :, :, 64:128], 1.0), writes=[vdst])
                for i in range(NT):
                    pv = nps()
                    proj_tm(pv[:, 0:64], pv, wkv, lambda c, part=part: wkv[:, c, part, :], i)
                    p.op("vector", lambda e, pv=pv, i=i, vdst=vdst: e.tensor_copy(vdst[:, i, 0, 0:64], pv[:, 0:64]), reads=[pv], writes=[(vdst, i)])
                p.fence(vdst)
            p.op("gpsimd", lambda e: e.memset(kcmpT[:, :, :], 0.0), writes=[kcmpT])
            p.op("gpsimd", lambda e: e.memset(vcmp[:, :], 0.0), writes=[vcmp])
            for kv in range(2):
                ph = nps()
                src3 = kcvT[:, kv, :].rearrange("p (n s) -> p n s", s=16)
                for ll in range(32):
                    rhs = src3[:, 0:127, ll] if ll < 16 else src3[:, 1:128, ll - 16]
                    p.op("tensor", lambda e, ph=ph, ll=ll, rhs=rhs, kv=kv: e.matmul(ph[0:64, 0:127], w1[kv][:, ll, :], rhs, start=(ll == 0), stop=False),
                         reads=[w1[kv], kcvT], writes=[ph])
                    p.op("tensor", lambda e, ph=ph, ll=ll, kv=kv: e.matmul(ph[0:64, 0:127], w1[kv][:, ll, :], peT[kv][:, ll:ll + 1].to_broadcast([64, 127]), start=False, stop=(ll == 31)),
                         reads=[w1[kv], peT[kv]], writes=[ph])
                hb = hidb[kv]
                p.op("scalar", lambda e, hb=hb, ph=ph: e.activation(hb[:, 0:127], ph[0:64, 0:127], AF.Silu), reads=[ph], writes=[hb])
                pc = nps()
                if kv == 0:
                    p.op("tensor", lambda e, pc=pc, hb=hb: e.matmul(pc[0:64, 0:127], w2[0][:, :], hb[:, 0:127], start=True, stop=True), reads=[w2[0], hb], writes=[pc])
                    p.op("vector", lambda e, pc=pc: e.tensor_copy(kcmpT[:, 0, 0:127], pc[0:64, 0:127]), reads=[pc], writes=[kcmpT])
                else:
                    p.op("tensor", lambda e, pc=pc, hb=hb: e.matmul(pc[0:127, 0:64], hb[:, 0:127], w2[1][:, :], start=True, stop=True), reads=[w2[1], hb], writes=[pc])
                    p.op("vector", lambda e, pc=pc: e.tensor_copy(vcmp[0:127, :], pc[0:127, 0:64]), reads=[pc], writes=[vcmp])
            p.release(mk_g)
            PT = [p.carve([128, 512], BF16, "nPT%d" % i) for i in range(3)]
            PTf = [p.carve([128, 512], F32, "nPTf%d" % i) for i in range(2)]
            recd = p.carve([128, 512], F32, "nrecd")
            pn = p.carve([128, 512], F32, "npn")
            pnb = p.carve([128, 512], BF16, "npnb")
            gs = [p.carve([128, 512], F32, "ngs%d" % i) for i in range(2)]
            rec = [p.carve([128, 512], F32, "nrec%d" % i) for i in range(2)]
            tsel = [p.carve([64, 512], F32, "ntsel%d" % i) for i in range(2)]
            yo = [p.carve([64, 512], BF16, "nyo%d" % i) for i in range(2)]
            sc = [p.carve([128, 32], F32, "nsc%d" % i) for i in range(2)]
            m8 = [p.carve([128, 8], F32, "nm8%d" % i) for i in range(2)]
            psimp = psb[4]
            recd2 = [recd, p.carve([128, 512], F32, "nrecd2")]
            pn2 = [pn, p.carve([128, 512], F32, "npn2")]
            pnb2 = [pnb, p.carve([128, 512], BF16, "npnb2")]
            units = [(b, hh) for b in range(4) for hh in range(4)]

            def cmpA(i):
                b, hh = units[i]
                bs = slice(b * 512, (b + 1) * 512)
                ps1 = nps()
                p.op("tensor", lambda e: e.matmul(ps1[:, :], kcmpT[:, 0, :], qT[:, hh, bs], start=True, stop=False),
                     reads=[kcmpT, (qT, (hh, b))], writes=[ps1])
                p.op("tensor", lambda e: e.matmul(ps1[:, :], identb[:, :], cmpmask[:, bs], start=False, stop=True),
                     reads=[identb, cmpmask], writes=[ps1])
                ptf = PTf[i % 2]
                p.op("scalar", lambda e: e.activation(ptf[:, :], ps1[:, :], AF.Exp, scale=0.125), reads=[ps1], writes=[ptf])
                psd = nps()
                p.op("tensor", lambda e: e.matmul(psd[:, :], cssd[:, 3, :], ptf[:, :], start=True, stop=True), reads=[cssd, ptf], writes=[psd])
                rd, pn_, pnb_ = recd2[i % 2], pn2[i % 2], pnb2[i % 2]
                p.op("vector", lambda e: e.tensor_scalar(rd[:, :], psd[:, :], 1e-30, None, ALU.max), reads=[psd], writes=[rd])
                p.op("vector", lambda e: e.reciprocal(rd[:, :], rd[:, :]), reads=[rd], writes=[rd])
                p.op("vector", lambda e: e.tensor_tensor(pn_[:, :], ptf[:, :], rd[:, :], ALU.mult), reads=[ptf, rd], writes=[pn_])
                p.op("gpsimd", lambda e: e.tensor_copy(pnb_[:, :], pn_[:, :]), reads=[pn_], writes=[pnb_])

            def cmpB(i):
                b, hh = units[i]
                h = g * 4 + hh
                bs = slice(b * 512, (b + 1) * 512)
                pn_, pnb_ = pn2[i % 2], pnb2[i % 2]
                p.op("tensor", lambda e: e.matmul(psimp[0:32, :], ovl[:, :], pn_[:, :], start=(hh == 0), stop=(hh == 3)), reads=[ovl, pn_], writes=[psimp])
                pso = nps()
                p.op("tensor", lambda e: e.matmul(pso[0:64, :], vcmp[:, :], pnb_[:, :], start=True, stop=True), reads=[vcmp, pnb_], writes=[pso])
                pgb = nps()
                p.op("tensor", lambda e: e.matmul(pgb[:, :], Sel[:, 0 * 8 + h, :], gT[:, bs], start=True, stop=True), reads=[Sel, gT], writes=[pgb])
                gs_ = gs[hh % 2]
                p.op("scalar", lambda e: e.copy(gs_[0:64, :], pgb[0:64, :]), reads=[pgb], writes=[gs_])
                p.op("vector", lambda e: e.tensor_tensor(ycmp[:, hh, bs], pso[0:64, :], gs_[0:64, :], ALU.mult),
                     reads=[pso, gs_], writes=[(ycmp, (hh, b))])
                if hh == 3:
                    p.op("scalar", lambda e: e.copy(impT[:, bs], psimp[0:32, :]), reads=[psimp], writes=[(impT, b)])

            cmpA(0)
            for i in range(len(units)):
                if i + 1 < len(units):
                    cmpA(i + 1)
                cmpB(i)
            p.fence(impT)
            for i in range(NT):
                ts_ = slice(i * 128, (i + 1) * 128)
                pt1 = nps()
                p.op("tensor", lambda e, pt1=pt1, ts_=ts_: e.transpose(pt1[:, 0:32], impT[:, ts_], identf[0:32, 0:32]), reads=[impT, identf], writes=[pt1])
                sc_ = sc[i % 2]
                m8_ = m8[i % 2]
                p.op("vector", lambda e, sc_=sc_, pt1=pt1, i=i: e.tensor_tensor(sc_[:, :], pt1[:, 0:32], vmul[:, i, :], ALU.mult), reads=[pt1, vmul], writes=[sc_])
                p.op("vector", lambda e, sc_=sc_, i=i: e.tensor_tensor(sc_[:, :], sc_[:, :], amask[:, i, :], ALU.add), reads=[sc_, amask], writes=[sc_])
                p.op("vector", lambda e, sc_=sc_, m8_=m8_: e.max(m8_[:, :], sc_[:, :]), reads=[sc_], writes=[m8_])
                p.op("vector", lambda e, sc_=sc_, m8_=m8_: e.tensor_scalar(sc_[:, :], sc_[:, :], m8_[:, 7:8], None, ALU.is_ge), reads=[sc_, m8_], writes=[sc_])
                p.op("vector", lambda e, sc_=sc_: e.tensor_scalar(sc_[:, :], sc_[:, :], -1.0, -NEGB, ALU.add, ALU.mult), reads=[sc_], writes=[sc_])
                pt2 = nps()
                p.op("tensor", lambda e, pt2=pt2, sc_=sc_: e.transpose(pt2[0:32, 0:128], sc_[:, :], identf[:, :]), reads=[sc_, identf], writes=[pt2])
                p.op("scalar", lambda e, pt2=pt2, ts_=ts_: e.copy(selbT[:, 0, ts_], pt2[0:32, 0:128]), reads=[pt2], writes=[(selbT, i)])
            p.fence(selbT)
            cnt = 0
            for hh in range(4):
                h = g * 4 + hh
                for b in range(4):
                    bs = slice(b * 512, (b + 1) * 512)
                    res = {}
                    for br in (1, 2):
                        pairs = []
                        if br == 1:
                            for j in range(0, 4 * b + 4):
                                extra = [(Emat[:, j, :], selbT[:, 0, bs], [Emat, selbT])]
                                if j >= 4 * b:
                                    extra.append((identb[:, :], masks[:, NSA_MASK0 + (j - 4 * b) + 4, :], [identb, masks]))
                                pairs.append((j, extra))
                            kT_, v_ = ksT, vs
                        else:
                            for j in range(max(0, 4 * b - 4), 4 * b + 4):
                                pairs.append((j, [(identb[:, :], masks[:, NSA_MASK0 + (j - 4 * b) + 4, :], [identb, masks])]))
                            kT_, v_ = kwT, vw
                        r_ = rec[br - 1]
                        ts2 = tsel[br - 1]

                        def fin(ps2, r_=r_, ts2=ts2, br=br, h=h, bs=bs):
                            pgb = nps()
                            p.op("tensor", lambda e: e.matmul(pgb[:, :], Sel[:, br * 8 + h, :], gT[:, bs], start=True, stop=True), reads=[Sel, gT], writes=[pgb])
                            p.op("vector", lambda e: e.reciprocal(r_[64:128, :], ps2[64:128, :]), reads=[ps2], writes=[r_])
                            p.op("vector", lambda e: e.tensor_tensor(r_[64:128, :], r_[64:128, :], pgb[64:128, :], ALU.mult), reads=[r_, pgb], writes=[r_])
                            p.op("vector", lambda e: e.tensor_tensor(ts2[:, :], ps2[0:64, :], r_[64:128, :], ALU.mult), reads=[ps2, r_], writes=[ts2])
                        attn_group(qT, (hh, b), hh, b, kT_, 0, v_, pairs, PT, None, fin)
                    y_ = yo[cnt % 2]
                    cnt += 1
                    p.op("gpsimd", lambda e, hh=hh, bs=bs: e.tensor_tensor(tsel[0][:, :], tsel[0][:, :], ycmp[:, hh, bs], ALU.add), reads=[tsel[0], (ycmp, (hh, b))], writes=[tsel[0]])
                    p.op("gpsimd", lambda e, y_=y_: e.tensor_tensor(y_[:, :], tsel[0][:, :], tsel[1][:, :], ALU.add), reads=[tsel[0], tsel[1]], writes=[y_])
                    p.dma(yT_d[1][h // 2, (h % 2) * 64:(h % 2) * 64 + 64, bs], y_[:, :], reads=[y_], writes=[(yT_d[1], (h, b))])
            p.fence(qT)
            p.fence(ycmp)
            p.release(mk_g)
        p.fence(yT_d[1])
        p.release(mk)


    def mixer_rwkv(l):
        mk = p.mark()
        ro = 0
        co = l * COL_PER_LAYER
        crw = p.carve([128, 642], F32, "crw")
        p.dma(crw[:, :], C["c_rw"][:, :], writes=[crw])
        bones = crw[:, 0:128]
        hsel = crw[:, 128:130]
        cmask = crw[:, 130:642]
        colv = lambda cidx: cols[:, co + cidx:co + cidx + 1]
        lora_in = p.carve([128, T], BF16, "rw_lora_in")
        sgT = p.carve([128, T], BF16, "rw_sgT")
        wup = p.carve([128, 512], BF16, "rw_wup")
        aup = p.carve([128, 512], BF16, "rw_aup")
        gup = p.carve([128, 512], BF16, "rw_gup")
        p.op("gpsimd", lambda e: e.memset(wup[:, :], 0.0), writes=[wup])
        p.op("gpsimd", lambda e: e.memset(aup[:, :], 0.0), writes=[aup])
        load_w(wup, lambda sv: [(wup[0:64, :], sv[0:64, 0, :])], W["rwkv_w_up"][l].unsqueeze(1), (1, 512), parts=64)
        sA = stg[stgi[0] % NSTG]
        stgi[0] += 1
        p.dma(sA[64:128, 0:512], W["rwkv_a_up"][l], writes=[sA])
        p.op("gpsimd", lambda e: e.tensor_copy(aup[64:128, :], sA[64:128, 0:512]), reads=[sA], writes=[aup])
        load_w(gup, lambda sv: [(gup[:, :], sv[:, 0, :])], W["rwkv_g_up"][l].unsqueeze(1), (1, 512))
        wch = [p.carve([128, 8, 128], BF16, "rw_wch%d" % i) for i in range(3)]
        ur = [[p.carve([128, 520], F32, "rw_ur%d_%d" % (q, i)) for i in range(2)] for q in range(3)]
        dtmp = p.carve([128, 512], F32, "rw_dtmp")

        def proj_lerp(chunk, tb, wbuf, urq, dst_ap, dst_buf, dst_key, act=None):
            un = urq[tb % 2]
            uo = urq[(tb + 1) % 2]
            if tb == 0:
                p.op("gpsimd", lambda e: e.memset(un[:, 0:1], 0.0), writes=[(un, "c")])
            else:
                p.op("gpsimd", lambda e: e.tensor_copy(un[:, 0:1], uo[:, 512:513]), reads=[uo], writes=[(un, "c")])
            pa = nps()
            proj_fm(pa[:, :], pa, wbuf, lambda c: wbuf[:, c, :], tb * 512, 512)
            p.op("scalar", lambda e: e.copy(un[:, 1:513], pa[:, :]), reads=[pa], writes=[(un, "d")])
            p.fence(un)
            p.op("vector", lambda e: e.tensor_tensor(dtmp[:, :], un[:, 0:512], un[:, 1:513], ALU.subtract), reads=[un], writes=[dtmp])
            p.op("vector", lambda e: e.scalar_tensor_tensor(out=dst_ap, in0=dtmp[:, :], scalar=colv(COL_MU + chunk), in1=un[:, 1:513], op0=ALU.mult, op1=ALU.add),
                 reads=[dtmp, un, cols], writes=[(dst_buf, dst_key)])

        lx = p.carve([128, 512], F32, "rw_lx")
        for q, chunk in enumerate((12, 13)):
            load_w(wch[q], lambda sv, q=q: [(wch[q][:, :, :], sv)], win_cols(l, OFF_RW + chunk * 128, 128), (8, 128))
            for tb in range(4):
                bs = slice(tb * 512, (tb + 1) * 512)
                proj_lerp(chunk, tb, wch[q], ur[q], lx[:, :], lx, None)
                if chunk == 12:
                    p.op("scalar", lambda e, bs=bs: e.activation(lora_in[0:64, bs], lx[0:64, :], AF.Tanh), reads=[lx], writes=[(lora_in, ("w", tb))])
                    p.op("vector", lambda e, bs=bs: e.tensor_copy(lora_in[64:128, bs], lx[64:128, :]), reads=[lx], writes=[(lora_in, ("a", tb))])
                else:
                    p.op("scalar", lambda e, bs=bs: e.activation(sgT[:, bs], lx[:, :], AF.Sigmoid), reads=[lx], writes=[(sgT, tb)])
        p.fence(lora_in)
        p.fence(sgT)
        def B_(name):
            return p.carve([128, 512], F32, "rw_" + name)
        rT, kT, vT = B_("rT"), B_("kT"), B_("vT")
        lw, av, kk, kmod, bb, cl, eg, egi, egm, tq, rkr = [B_(n) for n in ("lw", "av", "kk", "kmod", "bb", "cl", "eg", "egi", "egm", "tq", "rkr")]
        KR = p.carve([128, 4, 2, 128], F32, "rw_KR")
        bt, kt = B_("bt"), B_("kt")
        mA = {n: [B_(n + "A"), B_(n + "B")] for n in ("b", "k", "kap", "r")}
        NCI = 4
        M1s = [lw, av, kk, kmod]
        M2s = [bb, cl, egi, egm]
        M3s = [p.carve([128, 256], F32, "rw_M3_%d" % i) for i in range(NCI)]
        PPs = [[tq, tq], [kt, kt], [B_("PP_2")] * 2, [B_("PP_3")] * 2]
        Zbs = [p.carve([128, 2, 128], F32, "rw_Z%d" % i) for i in range(NCI)]
        BKtms = [p.carve([128, 4, 128], F32, "rw_BKtm%d" % i) for i in range(NCI)]
        Vtms = [p.carve([128, 128], F32, "rw_Vtm%d" % i) for i in range(NCI)]
        print("rwkv arena words used", p.aoff, "of", p.awords)
        Ast = p.carve([128, 64], F32, "rw_A")
        Yn = p.carve([128, 128], F32, "rw_Yn")
        Us = p.carve([128, 128], F32, "rw_U")
        t1 = p.carve([128, 64], F32, "rw_t1")
        osb = p.carve([128, 128], F32, "rw_osb")
        oc = p.carve([128, 128], F32, "rw_oc")
        sq = p.carve([128, 128], F32, "rw_sq")
        st4 = p.carve([128, 8], F32, "rw_st4")
        ssb = p.carve([128, 2], F32, "rw_ssb")
        ybf = p.carve([128, 128], BF16, "rw_ybf")
        ysb = [p.carve([128, 128], BF16, "rw_ysb%d" % i) for i in range(2)]
        gne = p.carve([128, 1], F32, "rw_gne")
        p.op("vector", lambda e: e.memset(gne[:, :], 64e-5), writes=[gne])
        msk4 = lambda: None
        v2 = lambda ap: ap.rearrange("p (h d) -> p h d", h=2)
        for hp in range(4):
            for q, chunk in enumerate((hp, 4 + hp, 8 + hp)):
                load_w(wch[q], lambda sv, q=q: [(wch[q][:, :, :], sv)], win_cols(l, OFF_RW + chunk * 128, 128), (8, 128))
            p.op("vector", lambda e: e.memset(Ast[:, :], 0.0), writes=[Ast])
            for tb in range(4):
                bs = slice(tb * 512, (tb + 1) * 512)
                for q, (chunk, dst) in enumerate(((hp, rT), (4 + hp, kT), (8 + hp, vT))):
                    proj_lerp(chunk, tb, wch[q], ur[q], dst[:, :], dst, None)
                pw = nps()
                p.op("tensor", lambda e, pw=pw, bs=bs: e.matmul(pw[:, :], wup[:, hp * 128:(hp + 1) * 128], lora_in[:, bs], start=True, stop=True), reads=[wup, lora_in], writes=[pw])
                p.op("scalar", lambda e, pw=pw: e.activation(lw[:, :], pw[:, :], AF.Sigmoid, bias=colv(COL_W0 + hp)), reads=[pw, cols], writes=[lw])
                p.op("vector", lambda e: e.tensor_scalar(lw[:, :], lw[:, :], -0.6065306597126334, None, ALU.mult), reads=[lw], writes=[lw])
                pa_ = nps()
                p.op("tensor", lambda e, pa_=pa_, bs=bs: e.matmul(pa_[:, :], aup[:, hp * 128:(hp + 1) * 128], lora_in[:, bs], start=True, stop=True), reads=[aup, lora_in], writes=[pa_])
                p.op("scalar", lambda e, pa_=pa_: e.activation(av[:, :], pa_[:, :], AF.Sigmoid, bias=colv(COL_A0 + hp)), reads=[pa_, cols], writes=[av])
                p.op("vector", lambda e: e.tensor_scalar(kk[:, :], kT[:, :], colv(COL_KK + hp), None, ALU.mult), reads=[kT, cols], writes=[kk])
                p.op("gpsimd", lambda e: e.tensor_tensor(tq[:, :], kk[:, :], kk[:, :], ALU.mult), reads=[kk], writes=[tq])
                pss = nps()
                p.op("tensor", lambda e, pss=pss: e.matmul(pss[:, :], bones, tq[:, :], start=True, stop=True), reads=[crw, tq], writes=[pss])
                p.op("scalar", lambda e, pss=pss: e.activation(tq[:, :], pss[:, :], AF.Sqrt), reads=[pss], writes=[tq])
                p.op("vector", lambda e: e.tensor_scalar(tq[:, :], tq[:, :], 1e-12, None, ALU.max), reads=[tq], writes=[tq])
                p.op("vector", lambda e: e.reciprocal(tq[:, :], tq[:, :]), reads=[tq], writes=[tq])
                p.op("vector", lambda e: e.tensor_tensor(kk[:, :], kk[:, :], tq[:, :], ALU.mult), reads=[kk, tq], writes=[kk])
                p.op("gpsimd", lambda e: e.tensor_scalar(tq[:, :], av[:, :], -1.0, colv(COL_KA + hp), ALU.add, ALU.mult), reads=[av, cols, tq], writes=[tq])
                p.op("vector", lambda e: e.scalar_tensor_tensor(out=kmod[:, :], in0=tq[:, :], scalar=1.0, in1=kT[:, :], op0=ALU.add, op1=ALU.mult), reads=[tq, kT], writes=[kmod])
                p.op("gpsimd", lambda e: e.tensor_tensor(bb[:, :], kk[:, :], av[:, :], ALU.mult), reads=[kk, av], writes=[bb])
                p.op("vector", lambda e: e.scalar_tensor_tensor(out=rkr[:, :], in0=rT[:, :], scalar=colv(COL_RK + hp), in1=kmod[:, :], op0=ALU.mult, op1=ALU.mult), reads=[rT, kmod, cols], writes=[rkr])
                p.op("vector", lambda e: e.tensor_tensor_scan(cl[:, :], cmask, lw[:, :], 0.0, ALU.mult, ALU.add), reads=[crw, lw], writes=[cl])
                p.op("scalar", lambda e: e.activation(eg[:, :], cl[:, :], AF.Exp), reads=[cl], writes=[eg])
                p.op("scalar", lambda e: e.activation(egi[:, :], cl[:, :], AF.Exp, scale=-1.0), reads=[cl], writes=[egi])
                p.op("gpsimd", lambda e: e.tensor_tensor(egm[:, :], cl[:, :], lw[:, :], ALU.subtract), reads=[cl, lw], writes=[egm])
                p.op("scalar", lambda e: e.activation(egm[:, :], egm[:, :], AF.Exp), reads=[egm], writes=[egm])
                v4 = lambda ap: ap.rearrange("p (c t) -> p c t", c=4)
                p.op("vector", lambda e: e.tensor_tensor(KR[:, :, 0, :], v4(kk[:, :]), v4(egm[:, :]), ALU.mult), reads=[kk, egm], writes=[(KR, 0)])
                p.op("vector", lambda e: e.tensor_tensor(KR[:, :, 1, :], v4(rT[:, :]), v4(eg[:, :]), ALU.mult), reads=[rT, eg], writes=[(KR, 1)])
                p.fence(KR)
                p.op("gpsimd", lambda e: e.tensor_tensor(bt[:, :], bb[:, :], egi[:, :], ALU.mult), reads=[bb, egi], writes=[bt])
                p.op("gpsimd", lambda e: e.tensor_tensor(kt[:, :], kmod[:, :], egi[:, :], ALU.mult), reads=[kmod, egi], writes=[kt])
                for X in range(2):
                    hcol = crw[:, 128 + X:129 + X]
                    p.op("vector", lambda e, X=X, hcol=hcol: e.tensor_scalar(mA["b"][X][:, :], bt[:, :], hcol, None, ALU.mult), reads=[bt, crw], writes=[mA["b"][X]])
                    p.op("scalar", lambda e, X=X, hcol=hcol: e.activation(mA["k"][X][:, :], kt[:, :], AF.Copy, scale=hcol), reads=[kt, crw], writes=[mA["k"][X]])
                    p.op("vector", lambda e, X=X, hcol=hcol: e.tensor_scalar(v4(mA["kap"][X][:, :]), KR[:, :, 0, :], hcol, None, ALU.mult), reads=[KR, crw], writes=[mA["kap"][X]])
                    p.op("scalar", lambda e, X=X, hcol=hcol: e.activation(v4(mA["r"][X][:, :]), KR[:, :, 1, :], AF.Copy, scale=hcol), reads=[KR, crw], writes=[mA["r"][X]])
                for cc in range(4):
                    c = tb * 4 + cc
                    cs = slice(cc * 128, (cc + 1) * 128)
                    M1, M2, M3, Zb, BKtm, Vtm = M1s[cc], M2s[cc], M3s[cc], Zbs[cc], BKtms[cc], Vtms[cc]
                    ts_ = slice(c * 128, (c + 1) * 128)
                    b1, b2, b3 = nps(), nps(), nps()
                    krv = KR[:, cc, :, :].rearrange("p a t -> p (a t)")
                    for X in range(2):
                        p.op("tensor", lambda e, X=X: e.matmul(b1[:, X * 256:(X + 1) * 256], mA["b"][X][:, cs], krv, start=True, stop=True), reads=[mA["b"][X], KR], writes=[b1])
                        p.op("tensor", lambda e, X=X: e.matmul(b2[:, X * 256:(X + 1) * 256], mA["k"][X][:, cs], krv, start=True, stop=True), reads=[mA["k"][X], KR], writes=[b2])
                        p.op("tensor", lambda e, X=X: e.matmul(b3[:, X * 128:(X + 1) * 128], mA["kap"][X][:, cs], bt[:, cs], start=True, stop=True), reads=[mA["kap"][X], bt], writes=[b3])
                    msi = lambda ap: ap.rearrange("p (x m t) -> p x m t", x=2, m=2)
                    for m_, cidx in ((0, 4), (1, 0)):
                        mk_ = cssd[:, cidx, :].unsqueeze(1).to_broadcast([128, 2, 128])
                        p.op("vector", lambda e, m_=m_, mk_=mk_: e.tensor_tensor(msi(M1[:, :])[:, :, m_, :], msi(b1[:, :])[:, :, m_, :], mk_, ALU.mult), reads=[b1, cssd], writes=[(M1, m_)])
                        p.op("vector", lambda e, m_=m_, mk_=mk_: e.tensor_tensor(msi(M2[:, :])[:, :, m_, :], msi(b2[:, :])[:, :, m_, :], mk_, ALU.mult), reads=[b2, cssd], writes=[(M2, m_)])
                    p.fence(M1)
                    p.fence(M2)
                    p.op("vector", lambda e: e.tensor_tensor(v2(M3[:, :]), v2(b3[:, 0:256]), cssd[:, 1, :].unsqueeze(1).to_broadcast([128, 2, 128]), ALU.mult), reads=[b3, cssd], writes=[M3])
                    lbt = lambda X: M1[:, X * 256:X * 256 + 128]
                    p.op("gpsimd", lambda e: e.tensor_tensor(Zb[:, :, :], identf[:, :].unsqueeze(1).to_broadcast([128, 2, 128]), msi(M1[:, :])[:, :, 0, :], ALU.subtract), reads=[identf, M1], writes=[Zb])
                    ptr = nps()
                    for ti, src in enumerate((mA["b"][0], mA["b"][1], mA["k"][0], mA["k"][1])):
                        p.op("tensor", lambda e, ti=ti, src=src: e.transpose(ptr[:, ti * 128:(ti + 1) * 128], src[:, cs], identf[:, :]), reads=[src, identf], writes=[ptr])
                    p.op("scalar", lambda e, ptr=ptr: e.copy(BKtm[:, :, :], ptr[:, :].rearrange("p (a t) -> p a t", a=4)), reads=[ptr], writes=[BKtm])
                    pv_ = nps()
                    p.op("tensor", lambda e, pv_=pv_: e.transpose(pv_[:, 0:128], vT[:, cs], identf[:, :]), reads=[vT, identf], writes=[pv_])
                    p.op("vector", lambda e, pv_=pv_: e.tensor_copy(Vtm[:, :], pv_[:, 0:128]), reads=[pv_], writes=[Vtm])
                Pn_ = [[M3s[cc][:, 0:128], M3s[cc][:, 128:256]] for cc in range(4)]
                Pt_ = [[M1s[cc][:, 0:128], M1s[cc][:, 256:384]] for cc in range(4)]
                Pb_ = [[M3s[cc], M1s[cc]] for cc in range(4)]
                for lev in range(1, 7):
                    for cc in range(4):
                        Zb = Zbs[cc]
                        Pn, Pt = Pn_[cc], Pt_[cc]
                        pbn, pbt = Pb_[cc]
                        pq = nps()
                        for X in range(2):
                            p.op("tensor", lambda e, X=X: e.matmul(pq[:, X * 256:X * 256 + 128], Pt[X], Pn[X], start=True, stop=True), reads=[pbn, pbt], writes=[pq])
                            p.op("tensor", lambda e, X=X: e.matmul(pq[:, X * 256 + 128:X * 256 + 256], Pn[X], Pt[X], start=True, stop=True), reads=[pbn, pbt], writes=[pq])
                        pp_ = PPs[cc][lev % 2]
                        if cc % 2 == 0:
                            p.op("scalar", lambda e: e.copy(pp_[:, :], pq[:, :]), reads=[pq], writes=[pp_])
                        else:
                            p.op("vector", lambda e: e.tensor_copy(pp_[:, :], pq[:, :]), reads=[pq], writes=[pp_])
                        Pn_[cc] = [pp_[:, 0:128], pp_[:, 256:384]]
                        Pt_[cc] = [pp_[:, 128:256], pp_[:, 384:512]]
                        Pb_[cc] = [pp_, pp_]
                        Pn = Pn_[cc]
                        pz = nps()
                        for X in range(2):
                            p.op("tensor", lambda e, X=X: e.matmul(pz[:, X * 128:(X + 1) * 128], Pn[X], Zb[:, X, :], start=True, stop=True), reads=[pp_, Zb], writes=[pz])
                        p.op("vector", lambda e: e.tensor_tensor(Zb[:, :, :], Zb[:, :, :], v2(pz[:, 0:256]), ALU.add), reads=[pz, Zb], writes=[Zb])
                def seq_part(cc):
                    c = tb * 4 + cc
                    cs = slice(cc * 128, (cc + 1) * 128)
                    ts_ = slice(c * 128, (c + 1) * 128)
                    M1, M2, M3, Zb, BKtm, Vtm = M1s[cc], M2s[cc], M3s[cc], Zbs[cc], BKtms[cc], Vtms[cc]
                    py = nps()
                    for X in range(2):
                        p.op("tensor", lambda e, X=X, py=py: e.matmul(py[:, X * 64:(X + 1) * 64], mA["kap"][X][:, cs], Ast[:, :], start=True, stop=False), reads=[mA["kap"][X], Ast], writes=[py])
                        p.op("tensor", lambda e, X=X, py=py: e.matmul(py[:, X * 64:(X + 1) * 64], M2[:, X * 256:X * 256 + 128], Vtm[:, X * 64:(X + 1) * 64], start=False, stop=True), reads=[M2, Vtm], writes=[py])
                    p.op("vector", lambda e, py=py: e.tensor_scalar(Yn[:, :], py[:, 0:128], -1.0, None, ALU.mult), reads=[py], writes=[Yn])
                    pu = nps()
                    for X in range(2):
                        p.op("tensor", lambda e, X=X, pu=pu: e.matmul(pu[:, X * 64:(X + 1) * 64], Zb[:, X, :], Yn[:, X * 64:(X + 1) * 64], start=True, stop=True), reads=[Zb, Yn], writes=[pu])
                    p.op("vector", lambda e, pu=pu: e.tensor_copy(Us[:, :], pu[:, 0:128]), reads=[pu], writes=[Us])
                    po = npl()
                    for X in range(2):
                        p.op("tensor", lambda e, X=X, po=po: e.matmul(po[:, X * 64:(X + 1) * 64], mA["r"][X][:, cs], Ast[:, :], start=True, stop=False), reads=[mA["r"][X], Ast], writes=[po])
                        p.op("tensor", lambda e, X=X, po=po: e.matmul(po[:, X * 64:(X + 1) * 64], M1[:, X * 256 + 128:X * 256 + 256], Us[:, X * 64:(X + 1) * 64], start=False, stop=False), reads=[M1, Us], writes=[po])
                        p.op("tensor", lambda e, X=X, po=po: e.matmul(po[:, X * 64:(X + 1) * 64], M2[:, X * 256 + 128:X * 256 + 256], Vtm[:, X * 64:(X + 1) * 64], start=False, stop=True), reads=[M2, Vtm], writes=[po])
                    pi_ = nps()
                    seq = [(0, Us, 0), (1, Us, 1), (2, Vtm, 0), (3, Vtm, 1)]
                    for si, (ti, rb, X) in enumerate(seq):
                        p.op("tensor", lambda e, si=si, ti=ti, rb=rb, X=X, pi_=pi_: e.matmul(pi_[:, 0:64], BKtm[:, ti, :], rb[:, X * 64:(X + 1) * 64], start=(si == 0), stop=(si == 3)), reads=[BKtm, rb], writes=[pi_])
                    gC = eg[:, cc * 128 + 127:cc * 128 + 128]
                    p.op("vector", lambda e, pi_=pi_, gC=gC: e.tensor_scalar(t1[:, :], pi_[:, 0:64], gC, None, ALU.mult), reads=[pi_, eg], writes=[t1])
                    p.op("vector", lambda e, gC=gC: e.scalar_tensor_tensor(out=Ast[:, :], in0=Ast[:, :], scalar=gC, in1=t1[:, :], op0=ALU.mult, op1=ALU.add), reads=[t1, eg, Ast], writes=[Ast])
                    return po

                def epi_part(cc, po):
                    c = tb * 4 + cc
                    cs = slice(cc * 128, (cc + 1) * 128)
                    ts_ = slice(c * 128, (c + 1) * 128)
                    M1, M2, M3, Zb, BKtm, Vtm = M1s[cc], M2s[cc], M3s[cc], Zbs[cc], BKtms[cc], Vtms[cc]
                    p.op("scalar", lambda e, po=po: e.copy(osb[:, :], po[:, 0:128]), reads=[po], writes=[osb])
                    p.op("vector", lambda e: e.tensor_reduce(out=st4[:, 0:2], in_=v2(osb[:, :]), axis=AX.X, op=ALU.add), reads=[osb], writes=[(st4, 0)])
                    p.op("vector", lambda e: e.tensor_scalar(st4[:, 2:4], st4[:, 0:2], -1.0 / 64, None, ALU.mult), reads=[(st4, 0)], writes=[(st4, 1)])
                    p.op("vector", lambda e: e.tensor_tensor(v2(oc[:, :]), v2(osb[:, :]), st4[:, 2:4].unsqueeze(2).to_broadcast([128, 2, 64]), ALU.add), reads=[osb, (st4, 1)], writes=[oc])
                    p.op("gpsimd", lambda e: e.tensor_tensor(sq[:, :], oc[:, :], oc[:, :], ALU.mult), reads=[oc], writes=[sq])
                    p.op("vector", lambda e: e.tensor_reduce(out=st4[:, 4:6], in_=v2(sq[:, :]), axis=AX.X, op=ALU.add), reads=[sq], writes=[(st4, 2)])
                    p.op("scalar", lambda e: e.activation(st4[:, 6:8], st4[:, 4:6], AF.Sqrt, bias=gne[:, 0:1], scale=1.0 / 64), reads=[(st4, 2), gne], writes=[(st4, 3)])
                    p.op("vector", lambda e: e.reciprocal(st4[:, 6:8], st4[:, 6:8]), reads=[(st4, 3)], writes=[(st4, 3)])
                    p.op("vector", lambda e: e.tensor_tensor(v2(oc[:, :]), v2(oc[:, :]), st4[:, 6:8].unsqueeze(2).to_broadcast([128, 2, 64]), ALU.mult), reads=[oc, (st4, 3)], writes=[oc])
                    p.op("gpsimd", lambda e: e.tensor_tensor(oc[:, :], oc[:, :], rows[:, ro + ROW_LNW + hp * 128:ro + ROW_LNW + (hp + 1) * 128], ALU.mult), reads=[oc, rows], writes=[oc])
                    p.op("gpsimd", lambda e: e.tensor_tensor(oc[:, :], oc[:, :], rows[:, ro + ROW_LNB + hp * 128:ro + ROW_LNB + (hp + 1) * 128], ALU.add), reads=[oc, rows], writes=[oc])
                    pb_ = nps()
                    p.op("tensor", lambda e, pb_=pb_: e.matmul(pb_[:, 0:2], rkr[:, cs], hsel, start=True, stop=True), reads=[rkr, crw], writes=[pb_])
                    p.op("vector", lambda e, pb_=pb_: e.tensor_copy(ssb[:, :], pb_[:, 0:2]), reads=[pb_], writes=[ssb])
                    p.op("vector", lambda e: e.tensor_tensor(v2(sq[:, :]), v2(Vtm[:, :]), ssb[:, :].unsqueeze(2).to_broadcast([128, 2, 64]), ALU.mult), reads=[Vtm, ssb, sq], writes=[sq])
                    p.op("gpsimd", lambda e: e.tensor_tensor(oc[:, :], oc[:, :], sq[:, :], ALU.add), reads=[oc, sq], writes=[oc])
                    pg_ = nps()
                    p.op("tensor", lambda e, pg_=pg_, ts_=ts_: e.matmul(pg_[:, 0:128], sgT[:, ts_], gup[:, hp * 128:(hp + 1) * 128], start=True, stop=True), reads=[sgT, gup], writes=[pg_])
                    p.op("vector", lambda e, pg_=pg_: e.tensor_tensor(ybf[:, :], oc[:, :], pg_[:, 0:128], ALU.mult), reads=[oc, pg_], writes=[ybf])
                    pt = npst()
                    p.op("tensor", lambda e, pt=pt: e.transpose(pt[:, 0:128], ybf[:, :], identb[:]), reads=[ybf, identb], writes=[pt])
                    ys_ = ysb[c % 2]
                    p.op("scalar", lambda e, pt=pt, ys_=ys_: e.copy(ys_[:, :], pt[:, 0:128]), reads=[pt], writes=[ys_])
                    p.dma(yT_d[2][hp, :, ts_], ys_[:, :], reads=[ys_], writes=[(yT_d[2], (hp, c))])
                pos_ = {}
                for step in range(5):
                    if step < 4:
                        pos_[step] = seq_part(step)
                    if step >= 1:
                        epi_part(step - 1, pos_[step - 1])
        if l == 0:
            for nm, bf in (("rT", rT), ("kT", kT), ("vT", vT), ("lw", lw), ("av", av), ("kk", kk), ("kmod", kmod), ("cl", cl), ("rkr", rkr)):
                dump("rw_" + nm, bf[:, :], [128, 512], F32, [bf])
            dump("rw_osb", osb[:, :], [128, 128], F32, [osb])
            dump("rw_oc", oc[:, :], [128, 128], F32, [oc])
            dump("rw_Us", Us[:, :], [128, 128], F32, [Us])
            dump("rw_A", Ast[:, :], [128, 64], F32, [Ast])
        p.fence(yT_d[2])
        p.release(mk)

    def mixer_ssd(l):
        mk = p.mark()
        ro = 0
        co = l * COL_PER_LAYER
        xbcT = p.carve([128, 8, T], BF16, "xbcT")
        Xtm = p.carve([128, NT, 512], BF16, "Xtm")
        Btm = p.carve([128, NT, 256], BF16, "Btm")
        dt_all = p.carve([128, NT, 8], F32, "dt_all")
        a_all = p.carve([128, NT, 8], F32, "a_all")
        acum = p.carve([128, NT, 8], F32, "acum")
        tot = p.carve([128, NT, 8], F32, "tot")
        ea = p.carve([128, NT, 8], F32, "ea")
        dts = p.carve([128, NT, 8], F32, "dts")
        cd = p.carve([128, NT, 8], F32, "cd")
        Arow = p.carve([128, 8], F32, "Arow")
        mk2 = p.mark()
        xr = [p.carve([128, T + 8], F32, "xr%d" % i) for i in range(2)]
        cacc = [p.carve([128, T], F32, "cacc%d" % i) for i in range(2)]
        wx = [p.carve([128, 8, 128], BF16, "wx%d" % i) for i in range(2)]
        for ch in range(8):
            xr_, ca_, wx_ = xr[ch % 2], cacc[ch % 2], wx[ch % 2]
            load_w(wx_, lambda sv, wx_=wx_: [(wx_[:, :, :], sv)], win_cols(l, OFF_XBC + ch * 128, 128), (8, 128))
            p.op("vector", lambda e, xr_=xr_: e.memset(xr_[:, 0:4], 0.0), writes=[(xr_, "z")])
            for b in range(4):
                pa = nps()
                proj_fm(pa[:, :], pa, wx_, lambda c, wx_=wx_: wx_[:, c, :], b * 512, 512)
                p.op("scalar", lambda e, xr_=xr_, pa=pa, b=b: e.copy(xr_[:, 4 + b * 512:4 + (b + 1) * 512], pa[:, :]), reads=[pa], writes=[(xr_, b)])
            p.fence(xr_)
            for k in range(4):
                wcol = cols[:, co + COL_CONVW + ch * 4 + k:co + COL_CONVW + ch * 4 + k + 1]
                if k == 0:
                    p.op("vector", lambda e, ca_=ca_, xr_=xr_, wcol=wcol: e.tensor_scalar(ca_[:, :], xr_[:, 1:1 + T], wcol, None, ALU.mult),
                         reads=[xr_, cols], writes=[ca_])
                else:
                    p.op("vector", lambda e, ca_=ca_, xr_=xr_, wcol=wcol, k=k: e.scalar_tensor_tensor(out=ca_[:, :], in0=xr_[:, 1 + k:1 + k + T], scalar=wcol, in1=ca_[:, :], op0=ALU.mult, op1=ALU.add),
                         reads=[xr_, cols, ca_], writes=[ca_])
            p.op("scalar", lambda e, ca_=ca_, ch=ch: e.activation(xbcT[:, ch, :], ca_[:, :], AF.Silu, bias=cols[:, co + COL_CONVB + ch:co + COL_CONVB + ch + 1]),
                 reads=[ca_, cols], writes=[(xbcT, ch)])
        p.fence(xbcT)
        p.release(mk2)
        import os as _os
        _stop = int(_os.environ.get("SSD_STOP", "99"))
        if _stop <= 1:
            p.release(mk)
            return
        for i in range(NT):
            pt = npst()
            for c in range(6):
                p.op("tensor", lambda e, pt=pt, c=c, i=i: e.transpose(pt[:, c * 128:(c + 1) * 128], xbcT[:, c, i * 128:(i + 1) * 128], identb[:]),
                     reads=[xbcT, identb], writes=[pt])
            p.op("vector", lambda e, pt=pt, i=i: e.tensor_copy(Xtm[:, i, :], pt[:, 0:512]), reads=[pt], writes=[(Xtm, i)])
            p.op("vector", lambda e, pt=pt, i=i: e.tensor_copy(Btm[:, i, :], pt[:, 512:768]), reads=[pt], writes=[(Btm, i)])
        p.fence(Xtm)
        p.fence(Btm)
        if _stop <= 2:
            p.release(mk)
            return
        wdt = p.carve([128, 8, 8], BF16, "wdt")
        load_w(wdt, lambda sv: [(wdt[:, :, :], sv)], win_cols(l, OFF_DT, 8), (8, 8))
        pd = nps()
        for i in range(NT):
            proj_tm(pd[:, i * 8:(i + 1) * 8], pd, wdt, lambda c: wdt[:, c, :], i)
        b3 = lambda r0: rows[:, ro + r0:ro + r0 + 8].unsqueeze(1).to_broadcast([128, NT, 8])
        p.op("vector", lambda e: e.tensor_tensor(dt_all[:, :, :], pd[:, 0:128].rearrange("p (i h) -> p i h", h=8), b3(ROW_DTB), ALU.add),
             reads=[pd, rows], writes=[dt_all])
        p.op("scalar", lambda e: e.activation(dt_all[:, :, :], dt_all[:, :, :], AF.Exp), reads=[dt_all], writes=[dt_all])
        p.op("scalar", lambda e: e.activation(dt_all[:, :, :], dt_all[:, :, :], AF.Ln, bias=cssd[:, 3, 0:1]), reads=[dt_all, cssd], writes=[dt_all])
        p.op("scalar", lambda e: e.activation(Arow[:, :], rows[:, ro + ROW_ALOG:ro + ROW_ALOG + 8], AF.Exp), reads=[rows], writes=[Arow])
        p.op("vector", lambda e: e.tensor_scalar(Arow[:, :], Arow[:, :], -1.0, None, ALU.mult), reads=[Arow], writes=[Arow])
        p.op("vector", lambda e: e.tensor_tensor(a_all[:, :, :], dt_all[:, :, :], Arow[:, :].unsqueeze(1).to_broadcast([128, NT, 8]), ALU.mult),
             reads=[dt_all, Arow], writes=[a_all])
        pc = nps()
        pt_ = nps()
        for i in range(NT):
            p.op("tensor", lambda e, i=i: e.matmul(pc[:, i * 8:(i + 1) * 8], cssd[:, 0, :], a_all[:, i, :], start=True, stop=True), reads=[cssd, a_all], writes=[pc])
            p.op("tensor", lambda e, i=i: e.matmul(pt_[:, i * 8:(i + 1) * 8], cssd[:, 3, :], a_all[:, i, :], start=True, stop=True), reads=[cssd, a_all], writes=[pt_])
        v3 = lambda ps_: ps_[:, 0:128].rearrange("p (i h) -> p i h", h=8)
        p.op("vector", lambda e: e.tensor_copy(acum[:, :, :], v3(pc)), reads=[pc], writes=[acum])
        p.op("vector", lambda e: e.tensor_copy(tot[:, :, :], v3(pt_)), reads=[pt_], writes=[tot])
        p.op("scalar", lambda e: e.activation(ea[:, :, :], acum[:, :, :], AF.Exp), reads=[acum], writes=[ea])
        p.op("scalar", lambda e: e.activation(cd[:, :, :], tot[:, :, :], AF.Exp), reads=[tot], writes=[cd])
        p.op("vector", lambda e: e.tensor_tensor(dts[:, :, :], tot[:, :, :], acum[:, :, :], ALU.subtract), reads=[tot, acum], writes=[dts])
        p.op("scalar", lambda e: e.activation(dts[:, :, :], dts[:, :, :], AF.Exp), reads=[dts], writes=[dts])
        p.op("vector", lambda e: e.tensor_tensor(dts[:, :, :], dts[:, :, :], dt_all[:, :, :], ALU.mult), reads=[dts, dt_all], writes=[dts])
        if _stop <= 3:
            p.release(mk)
            return
        if l == 0 and _stop == 98:
            dump("ssd_Xtm", Xtm[:, :, :], [128, NT, 512], BF16, [Xtm])
            dump("ssd_Btm", Btm[:, :, :], [128, NT, 256], BF16, [Btm])
            dump("ssd_dt", dt_all[:, :, :], [128, NT, 8], F32, [dt_all])
            dump("ssd_acum", acum[:, :, :], [128, NT, 8], F32, [acum])
            dump("ssd_tot", tot[:, :, :], [128, NT, 8], F32, [tot])
            dump("ssd_xbcT", xbcT[:, :, :], [128, 8, T], BF16, [xbcT])
        wz = p.carve([128, 8, 512], BF16, "wz")
        load_w_plain(wz, wz, win_cols(l, OFF_Z, 512), (8, 512))
        M1 = [p.carve([128, 128], F32, "M1_%d" % i) for i in range(4)]
        Eb = [p.carve([128, 512], F32, "Eb%d" % i) for i in range(2)]
        CBs = [p.carve([128, 128], F32, "CBs%d" % i) for i in range(2)]
        Wt = [p.carve([128, 512], BF16, "Wt%d" % i) for i in range(2)]
        Xdt = [p.carve([128, 512], BF16, "Xdt%d" % i) for i in range(2)]
        Xds = [p.carve([128, 512], BF16, "Xds%d" % i) for i in range(2)]
        prev = p.carve([128, 512], F32, "prev")
        prevb = [p.carve([128, 512], BF16, "prevb%d" % i) for i in range(2)]
        ptmp = p.carve([128, 512], F32, "ptmp")
        y1 = [p.carve([128, 512], F32, "y1_%d" % i) for i in range(2)]
        y2 = [p.carve([128, 512], F32, "y2_%d" % i) for i in range(2)]
        sz = [p.carve([128, 512], F32, "sz%d" % i) for i in range(2)]
        ss = [p.carve([128, 4], F32, "ss%d" % i) for i in range(2)]
        junk = p.carve([128, 256], F32, "sjunk")
        yn = [p.carve([128, 512], BF16, "yn%d" % i) for i in range(2)]
        yst = [p.carve([128, 4, 128], BF16, "yst%d" % i) for i in range(2)]
        p.op("vector", lambda e: e.memset(prev[:, :], 0.0), writes=[prev])
        p.op("vector", lambda e: e.memset(prevb[0][:, :], 0.0), writes=[prevb[0]])
        bc8 = lambda ap8: ap8.unsqueeze(2).to_broadcast([128, 8, 64])
        v8 = lambda ap: ap.rearrange("p (h d) -> p h d", d=64)
        Eb2 = [Eb, [p.carve([128, 512], F32, "Eb2_%d" % i) for i in range(2)]]
        CBs2 = [CBs, [p.carve([128, 128], F32, "CBs2_%d" % i) for i in range(2)]]
        Wt2 = [Wt, [p.carve([128, 512], BF16, "Wt2_%d" % i) for i in range(2)]]

        def stage1(c):
            k = c % 2
            tsl = slice(c * 128, (c + 1) * 128)
            p.op("vector", lambda e: e.tensor_tensor(v8(Xdt[k][:, :]), v8(Xtm[:, c, :]), bc8(dt_all[:, c, :]), ALU.mult),
                 reads=[Xtm, dt_all], writes=[Xdt[k]])
            p.op("gpsimd", lambda e: e.tensor_tensor(v8(Xds[k][:, :]), v8(Xtm[:, c, :]), bc8(dts[:, c, :]), ALU.mult),
                 reads=[Xtm, dts], writes=[Xds[k]])
            for g in range(2):
                pcb = nps()
                p.op("tensor", lambda e: e.matmul(pcb[:, 0:128], xbcT[:, 4 + g, tsl], xbcT[:, 6 + g, tsl], start=True, stop=True),
                     reads=[xbcT], writes=[pcb])
                cb_ = CBs2[k][g]
                p.op("scalar", lambda e: e.copy(cb_[:, :], pcb[:, 0:128]), reads=[pcb], writes=[cb_])
                pseg = nps()
                for hh in range(4):
                    h = g * 4 + hh
                    m1 = M1[hh]
                    p.op("vector", lambda e: e.tensor_scalar(m1[:, :], cssd[:, 1, :], a_all[:, c, h:h + 1], None, ALU.mult),
                         reads=[cssd, a_all], writes=[m1])
                    p.op("tensor", lambda e: e.matmul(pseg[:, hh * 128:(hh + 1) * 128], m1[:, :], cssd[:, 0, :], start=True, stop=False),
                         reads=[m1, cssd], writes=[pseg])
                    p.op("tensor", lambda e: e.matmul(pseg[:, hh * 128:(hh + 1) * 128], identf[:, :], cssd[:, 2, :], start=False, stop=True),
                         reads=[identf, cssd], writes=[pseg])
                eb = Eb2[k][g]
                p.op("scalar", lambda e: e.activation(eb[:, :], pseg[:, :], AF.Exp), reads=[pseg], writes=[eb])
                wt = Wt2[k][g]
                p.op("vector", lambda e: e.tensor_tensor(wt[:, :].rearrange("p (h l) -> p h l", h=4), eb[:, :].rearrange("p (h l) -> p h l", h=4),
                                                         cb_[:, :].unsqueeze(1).to_broadcast([128, 4, 128]), ALU.mult),
                     reads=[eb, cb_], writes=[wt])

        pending_tail = []
        stage1(0)
        for c in range(NT):
            k = c % 2
            pb_c = prevb[c % 2]
            pb_n = prevb[(c + 1) % 2]
            tsl = slice(c * 128, (c + 1) * 128)
            if c + 1 < NT:
                stage1(c + 1)
            pyd = psb[4]
            for g in range(2):
                wt = Wt2[k][g]
                for hh in range(4):
                    h = g * 4 + hh
                    p.op("tensor", lambda e: e.matmul(pyd[:, h * 64:(h + 1) * 64], wt[:, hh * 128:(hh + 1) * 128], Xdt[k][:, h * 64:(h + 1) * 64], start=True, stop=True),
                         reads=[wt, Xdt[k]], writes=[pyd])
            pst_ = nps()
            pyo = psb[5]
            for g in range(2):
                p.op("tensor", lambda e, g=g, c=c, k=k: e.matmul(pst_[:, g * 256:(g + 1) * 256], Btm[:, c, g * 128:(g + 1) * 128], Xds[k][:, g * 256:(g + 1) * 256], start=True, stop=True),
                     reads=[Btm, Xds[k]], writes=[pst_])
                p.op("tensor", lambda e, g=g, tsl=tsl, pb_c=pb_c: e.matmul(pyo[:, g * 256:(g + 1) * 256], xbcT[:, 6 + g, tsl], pb_c[:, g * 256:(g + 1) * 256], start=True, stop=True),
                     reads=[xbcT, pb_c], writes=[pyo])
            y1_ = y1[k]
            y2_ = y2[k]
            p.op("vector", lambda e, y1_=y1_, c=c: e.tensor_tensor(v8(y1_[:, :]), v8(pyo[:, :]), bc8(ea[:, c, :]), ALU.mult), reads=[pyo, ea], writes=[y1_])
            p.op("vector", lambda e, y1_=y1_: e.tensor_tensor(y1_[:, :], y1_[:, :], pyd[:, :], ALU.add), reads=[pyd, y1_], writes=[y1_])
            p.op("gpsimd", lambda e, y2_=y2_, c=c: e.tensor_tensor(v8(y2_[:, :]), v8(Xtm[:, c, :]), bc8(rows[:, ro + ROW_DSK:ro + ROW_DSK + 8]), ALU.mult),
                 reads=[Xtm, rows], writes=[y2_])
            p.op("gpsimd", lambda e, y2_=y2_, y1_=y1_: e.tensor_tensor(y2_[:, :], y2_[:, :], y1_[:, :], ALU.add), reads=[y1_, y2_], writes=[y2_])
            if _stop <= 4 + c:
                break
            if l == 0 and c in (0, 1) and _stop == 98:
                dump("ssd_y2_%d" % c, y2_[:, :], [128, 512], F32, [y2_])
                dump("ssd_y1_%d" % c, y1_[:, :], [128, 512], F32, [y1_])
            if c < NT - 1:
                p.op("vector", lambda e, c=c: e.tensor_tensor(v8(ptmp[:, :]), v8(prev[:, :]), bc8(cd[:, c, :]), ALU.mult), reads=[prev, cd], writes=[ptmp])
                p.op("vector", lambda e: e.tensor_tensor(prev[:, :], ptmp[:, :], pst_[:, :], ALU.add), reads=[ptmp, pst_], writes=[prev])
                p.op("scalar", lambda e, pb_n=pb_n: e.copy(pb_n[:, :], prev[:, :]), reads=[prev], writes=[pb_n])
            pz = nps()
            proj_tm(pz[:, :], pz, wz, lambda cc: wz[:, cc, :], c)
            while pending_tail:
                pending_tail.pop(0)()
            sz_ = sz[k]
            ss_ = ss[k]
            p.op("scalar", lambda e, sz_=sz_, pz=pz: e.activation(sz_[:, :], pz[:, :], AF.Silu), reads=[pz], writes=[sz_])
            p.op("vector", lambda e, sz_=sz_, y2_=y2_: e.tensor_tensor(sz_[:, :], sz_[:, :], y2_[:, :], ALU.mult), reads=[y2_, sz_], writes=[sz_])
            for g in range(2):
                p.op("scalar", lambda e, sz_=sz_, ss_=ss_, g=g: e.activation(junk[:, :], sz_[:, g * 256:(g + 1) * 256], AF.Square, accum_out=ss_[:, g:g + 1]),
                     reads=[sz_], writes=[junk, (ss_, g)])
            p.fence(ss_)
            p.op("scalar", lambda e, ss_=ss_: e.activation(ss_[:, 2:4], ss_[:, 0:2], AF.Sqrt, bias=epsc[:, 0:1], scale=1.0 / 256), reads=[ss_, epsc], writes=[ss_])
            p.op("vector", lambda e, ss_=ss_: e.reciprocal(ss_[:, 2:4], ss_[:, 2:4]), reads=[ss_], writes=[ss_])
            yn_ = yn[k]
            for g in range(2):
                p.op("vector", lambda e, yn_=yn_, sz_=sz_, ss_=ss_, g=g: e.scalar_tensor_tensor(
                    out=yn_[:, g * 256:(g + 1) * 256], in0=sz_[:, g * 256:(g + 1) * 256], scalar=ss_[:, 2 + g:3 + g],
                    in1=rows[:, ro + ROW_SSDN + g * 256:ro + ROW_SSDN + (g + 1) * 256], op0=ALU.mult, op1=ALU.mult),
                     reads=[sz_, ss_, rows], writes=[(yn_, g)])
            p.fence(yn_)
            def tail(c=c, k=k, yn_=yn_):
                pt = npst()
                for cc in range(4):
                    p.op("tensor", lambda e: e.transpose(pt[:, cc * 128:(cc + 1) * 128], yn_[:, cc * 128:(cc + 1) * 128], identb[:]),
                         reads=[yn_, identb], writes=[pt])
                ys_ = yst[k]
                p.op("scalar", lambda e: e.copy(ys_[:, :, :], pt[:, 0:512].rearrange("p (c t) -> p c t", c=4)), reads=[pt], writes=[ys_])
                p.dma(yT_d[0][:, :, c * 128:(c + 1) * 128].rearrange("c p t -> p c t"), ys_[:, :, :], reads=[ys_], writes=[(yT_d[0], c)])
            pending_tail.append(tail)
        for t_ in pending_tail:
            t_()
        p.fence(yT_d[0])
        p.release(mk)

    def phase_merge(l, active):
        mk = p.mark()
        mergedT = p.carve([128, 8, T], BF16, "mergedT")
        mk_w = p.mark()
        wg = [p.carve([128, 8, 512], BF16, "mwg%d" % i) for i in range(2)]
        wb = [p.carve([128, 4, 512], BF16, "mwb%d" % i) for i in range(2)]
        acc = [p.carve([128, 512], F32, "macc%d" % i) for i in range(2)]
        sg = [p.carve([128, 512], F32, "msg%d" % i) for i in range(2)]
        tm = [p.carve([128, 512], F32, "mtm%d" % i) for i in range(2)]
        brw = [W["w_br_ssd"], W["w_br_nsa"], W["w_br_rwkv"], W["w_br_swa"]]
        mk2 = p.mark()
        cnt = 0
        wcnt = 0
        for th in range(2):
            p.release(mk2)
            yT = {}
            for m in active:
                yT[m] = p.carve([128, 4, T // 2], BF16, "yTs%d" % m)
                for c in range(4):
                    p.dma(yT[m][:, c, :], yT_d[m][c, :, th * 1024:(th + 1) * 1024], reads=[yT_d[m]], writes=[(yT[m], c)])
                p.fence(yT[m])
            for dc in range(8):
                wg_ = wg[wcnt % 2]
                wb_ = wb[wcnt % 2]
                wcnt += 1
                for m in active:
                    load_w(wg_, lambda sv, m=m, wg_=wg_: [(wg_[:, :, m * 128:(m + 1) * 128], sv)],
                           win_cols(l, OFF_GATE + m * 1024 + dc * 128, 128), (8, 128), key=m)
                    load_w(wb_, lambda sv, m=m, wb_=wb_: [(wb_[:, :, m * 128:(m + 1) * 128], sv)],
                           brw[m][l, :, dc * 128:(dc + 1) * 128].rearrange("(c p) n -> p c n", p=128), (4, 128), key=m)
                for bb in range(2):
                    b = th * 2 + bb
                    acc_ = acc[cnt % 2]
                    cnt += 1
                    for mi, m in enumerate(active):
                        pg = nps()
                        proj_fm(pg[:, :], pg, wg_, lambda c, m=m, wg_=wg_: wg_[:, c, m * 128:(m + 1) * 128], b * 512, 512, wkey=m)
                        sg_ = sg[mi % 2]
                        p.op("scalar", lambda e, sg_=sg_, pg=pg: e.activation(sg_[:, :], pg[:, :], AF.Sigmoid), reads=[pg], writes=[sg_])
                        pb_ = nps()
                        for kc in range(4):
                            p.op("tensor", lambda e, pb_=pb_, kc=kc, m=m, wb_=wb_, bb=bb, yTm=yT[m]: e.matmul(
                                pb_[:, :], wb_[:, kc, m * 128:(m + 1) * 128], yTm[:, kc, bb * 512:(bb + 1) * 512], start=(kc == 0), stop=(kc == 3)),
                                 reads=[(wb_, m), yT[m]], writes=[pb_])
                        last = (mi == len(active) - 1)
                        if mi == 0:
                            dst = mergedT[:, dc, b * 512:(b + 1) * 512] if last else acc_[:, :]
                            p.op("vector", lambda e, dst=dst, pb_=pb_, sg_=sg_: e.tensor_tensor(dst, pb_[:, :], sg_[:, :], ALU.mult),
                                 reads=[pb_, sg_], writes=[(mergedT, (dc, b)) if last else acc_])
                        else:
                            tm_ = tm[mi % 2]
                            p.op("vector", lambda e, tm_=tm_, pb_=pb_, sg_=sg_: e.tensor_tensor(tm_[:, :], pb_[:, :], sg_[:, :], ALU.mult),
                                 reads=[pb_, sg_], writes=[tm_])
                            dst = mergedT[:, dc, b * 512:(b + 1) * 512] if last else acc_[:, :]
                            p.op("vector", lambda e, dst=dst, tm_=tm_, acc_=acc_: e.tensor_tensor(dst, tm_[:, :], acc_[:, :], ALU.add),
                                 reads=[tm_, acc_], writes=[(mergedT, (dc, b)) if last else acc_])
        p.fence(mergedT)
        p.release(mk_w)
        wo = p.carve([128, 8, D], BF16, "wo")
        load_w_plain(wo, wo, W["w_out"][l].rearrange("(c p) n -> p c n", p=128), (8, D))
        xt = [p.carve([128, D], F32, "xt%d" % i) for i in range(2)]
        src = x_in if l == 0 else xs
        for i in range(NT):
            x_ = xt[i % 2]
            p.dma(x_[:, :], src[i * 128:(i + 1) * 128, :], reads=[(src, i)], writes=[x_])
            for hf in range(2):
                po = nps()
                for kc in range(8):
                    p.op("tensor", lambda e, po=po, kc=kc, i=i, hf=hf: e.matmul(po[:, :], mergedT[:, kc, i * 128:(i + 1) * 128], wo[:, kc, hf * 512:(hf + 1) * 512],
                                                                              start=(kc == 0), stop=(kc == 7)),
                         reads=[mergedT, wo], writes=[po])
                p.op("vector", lambda e, x_=x_, po=po, hf=hf: e.tensor_tensor(x_[:, hf * 512:(hf + 1) * 512], x_[:, hf * 512:(hf + 1) * 512], po[:, :], ALU.add),
                     reads=[po, x_], writes=[x_])
            p.dma(xs[i * 128:(i + 1) * 128, :], x_[:, :], reads=[x_], writes=[(xs, i)])
            if debug and l == 0:
                p.dma(dbg["x_mix"][i * 128:(i + 1) * 128, :], x_[:, :], reads=[x_], writes=[(dbg["x_mix"], i)], is_output=True)
        p.release(mk)

    def phase_ffn_ple(l, last):
        mk = p.mark()
        xa = p.carve([128, NT, D], F32, "xacc")
        for i in range(NT):
            p.dma(xa[:, i, :], xs[i * 128:(i + 1) * 128, :], reads=[(xs, i)], writes=[(xa, i)])
        is_moe = (l % 2 == 1)
        j = l // 2
        rw = None
        if is_moe:
            rw = p.carve([128, NT, 8], F32, "rw")
        mk_r = p.mark()
        if is_moe:
            rt = p.carve([128, 8, 8], F32, "router")
            p.dma(rt[:, :, :], W["moe_router"][j].rearrange("(c p) n -> p c n", p=128), writes=[rt])
            hf32 = [p.carve([128, D], F32, "hf32_%d" % i) for i in range(2)]
            hTf = [p.carve([128, 8, 128], F32, "hTf%d" % i) for i in range(2)]
            m8 = [p.carve([128, 8], F32, "m8_%d" % i) for i in range(2)]
            lg = [p.carve([128, 8], F32, "lg_%d" % i) for i in range(2)]
            wv = [p.carve([128, 4], F32, "wv_%d" % i) for i in range(2)]
            e1 = [p.carve([128, 8], F32, "e1_%d" % i) for i in range(2)]
            grow = 0 + ROW_NORM_FFN

            def also(i, s_, xb, xap, xk):
                hf_ = hf32[i % 2]
                p.op("vector", lambda e: e.scalar_tensor_tensor(out=hf_[:], in0=xap, scalar=s_[:, 2:3], in1=rows[:, grow:grow + D], op0=ALU.mult, op1=ALU.mult),
                     reads=[(xb, xk), s_, rows], writes=[hf_])
                hT_ = hTf[i % 2]
                for half in range(2):
                    pp = nps()
                    for c in range(4):
                        cc = half * 4 + c
                        p.op("tensor", lambda e, pp=pp, c=c, cc=cc: e.transpose(pp[:, c * 128:(c + 1) * 128], hf_[:, cc * 128:(cc + 1) * 128], identf[:]),
                             reads=[hf_, identf], writes=[pp])
                    p.op("vector", lambda e, pp=pp, half=half: e.tensor_copy(hT_[:, half * 4:(half + 1) * 4, :], pp[:, :].rearrange("p (c t) -> p c t", c=4)),
                         reads=[pp], writes=[(hT_, half)])
                p.fence(hT_)
                pl = nps()
                for c in range(8):
                    p.op("tensor", lambda e, c=c, pl=pl: e.matmul(pl[:, 0:8], hT_[:, c, :], rt[:, c, :], start=(c == 0), stop=(c == 7)),
                         reads=[hT_, rt], writes=[pl])
                lg_ = lg[i % 2]
                m8_ = m8[i % 2]
                wv_ = wv[i % 2]
                e1_ = e1[i % 2]
                p.op("vector", lambda e: e.tensor_copy(lg_[:, :], pl[:, 0:8]), reads=[pl], writes=[lg_])
                p.op("vector", lambda e: e.max(m8_[:, :], lg_[:, :]), reads=[lg_], writes=[m8_])
                p.op("vector", lambda e: e.tensor_tensor(wv_[:, 0:1], m8_[:, 0:1], m8_[:, 1:2], ALU.subtract), reads=[m8_], writes=[wv_])
                p.op("scalar", lambda e: e.activation(wv_[:, 1:2], wv_[:, 0:1], AF.Sigmoid), reads=[wv_], writes=[wv_])
                p.op("scalar", lambda e: e.activation(wv_[:, 2:3], wv_[:, 0:1], AF.Sigmoid, scale=-1.0), reads=[wv_], writes=[wv_])
                p.op("vector", lambda e: e.tensor_scalar(e1_[:, :], lg_[:, :], m8_[:, 0:1], wv_[:, 1:2], ALU.is_equal, ALU.mult),
                     reads=[lg_, m8_, wv_], writes=[e1_])
                p.op("vector", lambda e: e.tensor_scalar(rw[:, i, :], lg_[:, :], m8_[:, 1:2], wv_[:, 2:3], ALU.is_equal, ALU.mult),
                     reads=[lg_, m8_, wv_], writes=[(rw, i)])
                p.op("vector", lambda e: e.tensor_tensor(rw[:, i, :], rw[:, i, :], e1_[:, :], ALU.add), reads=[e1_, (rw, i)], writes=[(rw, i)])
        else:
            also = None
        rmsnorm_to_hT(lambda i: (xa, xa[:, i, :], i), 0 + ROW_NORM_FFN, also)
        if is_moe:
            p.fence(rw)
        p.release(mk_r)
        mk2 = p.mark()
        FC = 256
        nfc = D_FF // FC
        wgb = [p.carve([128, 8, FC], BF16, "fwg%d" % i) for i in range(2)]
        wub = [p.carve([128, 8, FC], BF16, "fwu%d" % i) for i in range(2)]
        wdb = [p.carve([128, 2, D], BF16, "fwd%d" % i) for i in range(2)]
        hid = [p.carve([128, 2, T], BF16, "hid%d" % i) for i in range(2)]
        sl = [p.carve([128, 512], F32, "sl%d" % i) for i in range(2)]
        experts = list(range(8)) if is_moe else [None]
        funits = [(ex, fc) for ex in experts for fc in range(nfc)]

        def wsel(ex):
            if is_moe:
                return W["moe_w_gate"][j, ex], W["moe_w_up"][j, ex], W["moe_w_down"][j, ex]
            return W["ffn_w_gate"][j], W["ffn_w_up"][j], W["ffn_w_down"][j]

        def up_part(it):
            ex, fc = funits[it]
            Wg, Wu, Wd = wsel(ex)
            wg_, wu_, wd_, hid_ = wgb[it % 2], wub[it % 2], wdb[it % 2], hid[it % 2]
            load_w(wg_, lambda sv: [(wg_[:, :, :], sv)], Wg[:, fc * FC:(fc + 1) * FC].rearrange("(c p) n -> p c n", p=128), (8, FC))
            load_w(wu_, lambda sv: [(wu_[:, :, :], sv)], Wu[:, fc * FC:(fc + 1) * FC].rearrange("(c p) n -> p c n", p=128), (8, FC))
            load_w(wd_, lambda sv: [(wd_[:, :, :], sv)], Wd[fc * FC:(fc + 1) * FC, :].rearrange("(c p) n -> p c n", p=128), (2, D))
            cnt = 0
            for fs in range(2):
                for b in range(4):
                    pg = nps()
                    pu = nps()
                    proj_fm(pg[:, :], pg, wg_, lambda c, wg_=wg_, fs=fs: wg_[:, c, fs * 128:(fs + 1) * 128], b * 512, 512)
                    proj_fm(pu[:, :], pu, wu_, lambda c, wu_=wu_, fs=fs: wu_[:, c, fs * 128:(fs + 1) * 128], b * 512, 512)
                    sl_ = sl[cnt % 2]
                    cnt += 1
                    p.op("scalar", lambda e: e.activation(sl_[:, :], pg[:, :], AF.Silu), reads=[pg], writes=[sl_])
                    p.op("vector", lambda e: e.tensor_tensor(hid_[:, fs, b * 512:(b + 1) * 512], sl_[:, :], pu[:, :], ALU.mult),
                         reads=[sl_, pu], writes=[(hid_, (fs, b))])
            p.fence(hid_)

        def down_part(it):
            ex, fc = funits[it]
            wd_, hid_ = wdb[it % 2], hid[it % 2]
            for i in range(NT):
                for hf in range(2):
                    pd = nps()
                    for fs in range(2):
                        p.op("tensor", lambda e: e.matmul(pd[:, :], hid_[:, fs, i * 128:(i + 1) * 128], wd_[:, fs, hf * 512:(hf + 1) * 512], start=(fs == 0), stop=(fs == 1)),
                             reads=[hid_, wd_], writes=[pd])
                    xv = xa[:, i, hf * 512:(hf + 1) * 512]
                    if is_moe:
                        p.op("vector", lambda e: e.scalar_tensor_tensor(out=xv, in0=pd[:, :], scalar=rw[:, i, ex:ex + 1], in1=xv, op0=ALU.mult, op1=ALU.add),
                             reads=[pd, rw, (xa, i)], writes=[(xa, i)])
                    else:
                        p.op("vector", lambda e: e.tensor_tensor(xv, xv, pd[:, :], ALU.add), reads=[pd, (xa, i)], writes=[(xa, i)])

        up_part(0)
        for it in range(len(funits)):
            if it + 1 < len(funits):
                up_part(it + 1)
            down_part(it)
        p.release(mk2)
        if debug and l == 0:
            for i in range(NT):
                p.dma(dbg["x_ffn"][i * 128:(i + 1) * 128, :], xa[:, i, :], reads=[(xa, i)], writes=[(dbg["x_ffn"], i)], is_output=True)
        mk3 = p.mark()
        wpg = p.carve([128, 8, D], BF16, "wpg")
        wpp = p.carve([128, 2, D], BF16, "wpp")
        load_w_plain(wpg, wpg, W["ple_gate"][l].rearrange("(c p) n -> p c n", p=128), (8, D))
        load_w_plain(wpp, wpp, W["ple_proj"][l].rearrange("(c p) n -> p c n", p=128), (2, D))
        xb16 = [p.carve([128, D], BF16, "xb16_%d" % i) for i in range(2)]
        pf = [p.carve([128, 256], F32, "pf%d" % i) for i in range(2)]
        pb16 = [p.carve([128, 256], BF16, "pb16_%d" % i) for i in range(2)]
        pT = [p.carve([128, 2, 128], BF16, "pT%d" % i) for i in range(2)]
        sgp = [p.carve([128, 512], F32, "sgp%d" % i) for i in range(2)]
        tmp = [p.carve([128, 512], F32, "tmpp%d" % i) for i in range(2)]
        for i in range(NT):
            xb_ = xb16[i % 2]
            p.op("gpsimd", lambda e, xb_=xb_, i=i: e.tensor_copy(xb_[:, :], xa[:, i, :]), reads=[(xa, i)], writes=[xb_])
            pt = npst()
            for c in range(8):
                p.op("tensor", lambda e, pt=pt, c=c, xb_=xb_: e.transpose(pt[:, c * 128:(c + 1) * 128], xb_[:, c * 128:(c + 1) * 128], identb[:]),
                     reads=[xb_, identb], writes=[pt])
            p.op("scalar", lambda e, pt=pt, i=i: e.copy(hT[:, :, i * 128:(i + 1) * 128], pt[:, :].rearrange("p (c t) -> p c t", c=8)),
                 reads=[pt], writes=[(hT, i)])
            pf_, pb_, pT_ = pf[i % 2], pb16[i % 2], pT[i % 2]
            p.dma(pf_[:, :], p_in[l, i * 128:(i + 1) * 128, :], writes=[pf_])
            p.op("gpsimd", lambda e, pf_=pf_, pb_=pb_: e.tensor_copy(pb_[:, :], pf_[:, :]), reads=[pf_], writes=[pb_])
            pt2 = npst()
            for c in range(2):
                p.op("tensor", lambda e, pt2=pt2, c=c, pb_=pb_: e.transpose(pt2[:, c * 128:(c + 1) * 128], pb_[:, c * 128:(c + 1) * 128], identb[:]),
                     reads=[pb_, identb], writes=[pt2])
            p.op("vector", lambda e, pt2=pt2, pT_=pT_: e.tensor_copy(pT_[:, :, :], pt2[:, 0:256].rearrange("p (c t) -> p c t", c=2)), reads=[pt2], writes=[pT_])
            for hf in range(2):
                pg = nps()
                proj_tm(pg[:, :], pg, wpg, lambda c, hf=hf: wpg[:, c, hf * 512:(hf + 1) * 512], i)
                pq = nps()
                for c in range(2):
                    p.op("tensor", lambda e, pq=pq, c=c, pT_=pT_, hf=hf: e.matmul(pq[:, :], pT_[:, c, :], wpp[:, c, hf * 512:(hf + 1) * 512], start=(c == 0), stop=(c == 1)),
                         reads=[pT_, wpp], writes=[pq])
                sg_, tm_ = sgp[hf], tmp[hf]
                p.op("scalar", lambda e, sg_=sg_, pg=pg: e.activation(sg_[:, :], pg[:, :], AF.Sigmoid), reads=[pg], writes=[sg_])
                p.op("vector", lambda e, tm_=tm_, sg_=sg_, pq=pq: e.tensor_tensor(tm_[:, :], sg_[:, :], pq[:, :], ALU.mult), reads=[sg_, pq], writes=[tm_])
                xv = xa[:, i, hf * 512:(hf + 1) * 512]
                p.op("vector", lambda e, xv=xv, tm_=tm_: e.tensor_tensor(xv, xv, tm_[:, :], ALU.add), reads=[tm_, (xa, i)], writes=[(xa, i)])
        p.release(mk3)
        if not last:
            for i in range(NT):
                p.dma(xs[i * 128:(i + 1) * 128, :], xa[:, i, :], reads=[(xa, i)], writes=[(xs, i)])
                if debug and l == 0:
                    p.dma(dbg["x_l0"][i * 128:(i + 1) * 128, :], xa[:, i, :], reads=[(xa, i)], writes=[(dbg["x_l0"], i)], is_output=True)
        else:
            mk4 = p.mark()
            junk = p.carve([128, D], F32, "fjunk")
            ob = [p.carve([128, D], F32, "fo%d" % i) for i in range(2)]
            st = [p.carve([128, 4], F32, "fst%d" % i) for i in range(2)]
            for i in range(NT):
                s_, o_ = st[i % 2], ob[i % 2]
                p.op("scalar", lambda e, s_=s_, i=i: e.activation(junk[:], xa[:, i, :], AF.Square, accum_out=s_[:, 0:1]), reads=[(xa, i)], writes=[junk, s_])
                p.op("scalar", lambda e, s_=s_: e.activation(s_[:, 1:2], s_[:, 0:1], AF.Sqrt, bias=epsc[:, 0:1], scale=1.0 / D), reads=[s_, epsc], writes=[s_])
                p.op("vector", lambda e, s_=s_: e.reciprocal(s_[:, 2:3], s_[:, 1:2]), reads=[s_], writes=[s_])
                p.op("vector", lambda e, s_=s_, o_=o_, i=i: e.scalar_tensor_tensor(out=o_[:], in0=xa[:, i, :], scalar=s_[:, 2:3], in1=rowsf[:, 0:D], op0=ALU.mult, op1=ALU.mult),
                     reads=[(xa, i), s_, rowsf], writes=[o_])
                p.dma(out_d[i * 128:(i + 1) * 128, :], o_[:, :], reads=[o_], writes=[(out_d, i)], is_output=True)
            p.release(mk4)
        p.release(mk)

    MIX = {"swa": (3, mixer_swa), "ssd": (0, mixer_ssd), "nsa": (1, mixer_nsa), "rwkv": (2, mixer_rwkv)}
    for l in range(n_layers):
        src = x_in if l == 0 else xs
        p.dma(rows[:], rows_in[:, l * ROW_PER_LAYER:(l + 1) * ROW_PER_LAYER], writes=[rows])
        mk = p.mark()
        xt = [p.carve([128, D], F32, "xin%d" % i) for i in range(2)]

        def src_tile(i, src=src, xt=xt):
            x_ = xt[i % 2]
            p.dma(x_[:, :], src[i * 128:(i + 1) * 128, :], reads=[(src, i)], writes=[x_])
            return (x_, x_[:, :], None)
        rmsnorm_to_hT(src_tile, 0 + ROW_NORM_MIX)
        p.release(mk)
        active = []
        for name in mixers:
            m, fn = MIX[name]
            fn(l)
            active.append(m)
            if debug and l == 0:
                for c in range(4):
                    p.dma(dbg["yT%d" % m][c, :, :], yT_d[m][c, :, :], reads=[yT_d[m]], writes=[(dbg["yT%d" % m], c)], is_output=True)
        phase_merge(l, sorted(active))
        phase_ffn_ple(l, last=(l == n_layers - 1))
    return p.finish()


def make_in_maps(inp, n_cores=8):
    consts = build_consts()
    rows = build_rows(inp)
    cols = build_cols(inp)
    inp = dict(inp)
    inp["nsa_peT"] = np.ascontiguousarray(np.asarray(inp["nsa_cmp_pe"]).transpose(0, 1, 3, 2))
    shared = {k: np.ascontiguousarray(np.asarray(inp[k], dtype=np.float32)) for k in WEIGHT_SPECS}
    maps = []
    for b in range(n_cores):
        m = dict(shared)
        m.update(consts)
        m["rows"] = rows
        m["cols"] = cols
        m["x"] = np.ascontiguousarray(inp["x"][b])
        m["p"] = np.ascontiguousarray(inp["p"][:, b])
        m["pos"] = np.ascontiguousarray(np.broadcast_to(np.asarray(inp["positions"][b], dtype=np.int32)[None, :], (64, T)))
        maps.append(m)
    return maps


def kernel(**inputs):
    inp = {k: np.asarray(v) for k, v in inputs.items()}
    nc = build()
    maps = make_in_maps(inp)
    res = run_bass_kernel_spmd(nc, maps, core_ids=list(range(8)))
    return np.stack([r["out"] for r in res.results], axis=0).astype(np.float32)
```

```python
import types
import numpy as np
import ml_dtypes
import concourse.bass as bass
import concourse.mybir as mybir
from concourse.bass_utils import run_bass_kernel_spmd

F32 = mybir.dt.float32
BF16 = mybir.dt.bfloat16
I32 = mybir.dt.int32
AF = mybir.ActivationFunctionType
ALU = mybir.AluOpType
AX = mybir.AxisListType

ENGS = ["sync", "scalar", "vector", "gpsimd", "tensor"]
N_DMA_SEMS = 48

T = 2048
D = 1024
NT = 16
DEPTH = 2
D_IN = 9504
D_FF = 2816
NEGB = -30000.0
OFF_Z = 0
OFF_XBC = 512
OFF_DT = 1536
OFF_NQ = 1544
OFF_NKV = 2056
OFF_NG = 2824
OFF_RW = 2848
OFF_SQ = 4640
OFF_SKV = 5152
OFF_GATE = 5408


class Buf:
    def __init__(self, t, name):
        self.t = t
        self.name = name
        self.tr = {}

    def __getitem__(self, idx):
        return self.t[idx]

    def _entries(self, key):
        if key is None:
            if None not in self.tr:
                self.tr[None] = [[], []]
            return list(self.tr.values())
        out = []
        if None in self.tr:
            out.append(self.tr[None])
        if key not in self.tr:
            self.tr[key] = [[], []]
        out.append(self.tr[key])
        return out

    def all_deps(self):
        d = []
        for w, r in self.tr.values():
            d.extend(w)
            d.extend(r)
        return d


def _freeze(fn):
    if getattr(fn, "__closure__", None) is None:
        return fn
    cells = []
    for c in fn.__closure__:
        try:
            cells.append(types.CellType(c.cell_contents))
        except ValueError:
            cells.append(c)
    return types.FunctionType(fn.__code__, fn.__globals__, fn.__name__, fn.__defaults__, tuple(cells))


def _compress(lst):
    best = {}
    for s, v in lst:
        if s not in best or best[s] < v:
            best[s] = v
    return list(best.items())


class Prog:
    def __init__(self):
        self.nc = bass.Bass("TRN2", target_bir_lowering=False)
        nc = self.nc
        self.ops = {e: [] for e in ENGS}
        self.cnt = {e: 0 for e in ENGS}
        self.esem = {e: nc.alloc_semaphore("es_" + e) for e in ENGS}
        self.dsem = [nc.alloc_semaphore("ds%d" % i) for i in range(N_DMA_SEMS)]
        self.dcnt = [0] * N_DMA_SEMS
        self.dnext = 0
        self.waited = {e: {} for e in ENGS}
        self.nbuf = 0
        self.out_deps = []
        self.arena = None
        self.aoff = 0
        self.alive = []
        self.retired = []

    def sb(self, shape, dt=F32, name=None):
        self.nbuf += 1
        name = name or "sb%d" % self.nbuf
        return Buf(self.nc.alloc_sbuf_tensor(name, list(shape), dt), name)

    def ps(self, shape, dt=F32, name=None):
        self.nbuf += 1
        name = name or "ps%d" % self.nbuf
        return Buf(self.nc.alloc_psum_tensor(name, list(shape), dt), name)

    def dram(self, name, shape, dt=F32, kind="Internal"):
        return Buf(self.nc.dram_tensor(name, list(shape), dt, kind=kind), name)

    def make_arena(self, nwords):
        self.arena = self.nc.alloc_sbuf_tensor("arena", [128, nwords], F32)
        self.awords = nwords

    def mark(self):
        return self.aoff

    def release(self, mark):
        keep = []
        for s, e, b in self.alive:
            if s >= mark:
                self.retired.append((s, e, _compress(b.all_deps())))
            else:
                keep.append((s, e, b))
        self.alive = keep
        self.aoff = mark
        if len(self.retired) > 64:
            alld = []
            lo = min(r[0] for r in self.retired)
            hi = max(r[1] for r in self.retired)
            for r in self.retired:
                alld.extend(r[2])
            self.retired = [(lo, hi, _compress(alld))]

    def carve(self, shape, dt=F32, name=None):
        esz = 4 if dt in (F32, I32) else 2
        nfree = int(np.prod(shape[1:]))
        nwords = (nfree * esz + 3) // 4
        nwords = (nwords + 7) // 8 * 8
        s = self.aoff
        e = s + nwords
        assert e <= self.awords, "arena overflow %d > %d (%s)" % (e, self.awords, name)
        self.aoff = e
        v = self.arena[0:shape[0], s:s + (nfree * esz + 3) // 4]
        if esz == 2:
            v = v.bitcast(BF16)
            v = v[:, 0:nfree]
        elif dt == I32:
            v = v.bitcast(I32)
        if len(shape) == 3:
            v = v.rearrange("p (a b) -> p a b", a=shape[1])
        elif len(shape) == 4:
            v = v.rearrange("p (a b c) -> p a b c", a=shape[1], b=shape[2])
        self.nbuf += 1
        b = Buf(v, name or "cv%d" % self.nbuf)
        inh = []
        for rs, re, rd in self.retired:
            if rs < e and re > s:
                inh.extend(rd)
        if inh:
            b.tr[None] = [_compress(inh), []]
        self.alive.append((s, e, b))
        return b

    def fence(self, b):
        d = _compress(b.all_deps())
        b.tr = {None: [d, []]}

    def _collect(self, reads, writes):
        deps = []
        for b, k in reads:
            for ent in b._entries(k):
                deps.extend(ent[0])
        for b, k in writes:
            for ent in b._entries(k):
                deps.extend(ent[0])
                deps.extend(ent[1])
        return deps

    def _record(self, reads, writes, tok):
        for b, k in reads:
            if k not in b.tr:
                b.tr[k] = [[], []]
            b.tr[k][1].append(tok)
            if len(b.tr[k][1]) > 48:
                b.tr[k][1] = _compress(b.tr[k][1])
        for b, k in writes:
            if k is None:
                b.tr = {None: [[tok], []]}
            else:
                b.tr[k] = [[tok], []]

    @staticmethod
    def _norm(lst):
        out = []
        for x in lst:
            if isinstance(x, Buf):
                out.append((x, None))
            else:
                out.append(x)
        return out

    def _waits(self, eng, deps):
        need = {}
        for s, v in deps:
            if eng == "tensor" and s == ("e", "tensor"):
                continue
            if self.waited[eng].get(s, 0) >= v:
                continue
            if need.get(s, 0) < v:
                need[s] = v
        for s, v in need.items():
            self.waited[eng][s] = v
        return list(need.items())

    def _sem(self, s):
        return self.esem[s[1]] if s[0] == "e" else self.dsem[s[1]]

    def op(self, eng, fn, reads=(), writes=()):
        reads = self._norm(reads)
        writes = self._norm(writes)
        deps = self._collect(reads, writes)
        waits = self._waits(eng, deps)
        self.cnt[eng] += 1
        tok = (("e", eng), self.cnt[eng])
        self.ops[eng].append((waits, _freeze(fn), ("e", eng), 1))
        self._record(reads, writes, tok)
        return tok

    def dma(self, out, in_, reads=(), writes=(), eng="sync", is_output=False):
        reads = self._norm(reads)
        writes = self._norm(writes)
        deps = self._collect(reads, writes)
        i = self.dnext
        self.dnext = (self.dnext + 1) % N_DMA_SEMS
        if self.dcnt[i] > 0:
            deps.append((("d", i), 16 * self.dcnt[i]))
        waits = self._waits(eng, deps)
        self.dcnt[i] += 1
        tok = (("d", i), 16 * self.dcnt[i])
        fn = lambda e, o=out, a=in_: e.dma_start(out=o, in_=a)
        self.ops[eng].append((waits, fn, ("d", i), 16))
        self._record(reads, writes, tok)
        if is_output:
            self.out_deps.append(tok)
        return tok

    def finish(self):
        nc = self.nc
        final_deps = list(self.out_deps)
        for e in ENGS:
            if self.cnt[e] > 0 and e != "sync":
                final_deps.append((("e", e), self.cnt[e]))
        for i in range(N_DMA_SEMS):
            if self.dcnt[i] > 0:
                final_deps.append((("d", i), 16 * self.dcnt[i]))
        fwaits = self._waits("sync", final_deps)
        ops = self.ops
        sem = self._sem
        with nc.Block() as block:

            def mk(engname):
                def body(e):
                    for waits, fn, s, inc in ops[engname]:
                        for ws, wv in waits:
                            e.wait_ge(sem(ws), wv)
                        ins = fn(e)
                        ins.then_inc(sem(s), inc)
                    if engname == "sync":
                        for ws, wv in fwaits:
                            e.wait_ge(sem(ws), wv)

                return body

            block.sync(mk("sync"))
            block.scalar(mk("scalar"))
            block.vector(mk("vector"))
            block.gpsimd(mk("gpsimd"))
            block.tensor(mk("tensor"))
        return nc


def _mask_tile(W, d):
    m = np.full((128, 512), NEGB, np.float32)
    s = np.arange(128)[:, None]
    t = np.arange(128)[None, :]
    for c in range(4):
        delta = c - d
        blk = m[:, c * 128:(c + 1) * 128]
        if delta < 0 or delta > W:
            continue
        if delta == 0:
            blk[:] = np.where(s <= t, 0.0, NEGB)
        elif delta < W:
            blk[:] = 0.0
        else:
            blk[:] = np.where(s > t, 0.0, NEGB)
    return m


SWA_MASK0 = 0
NSA_MASK0 = 5
N_MASKS = 13


def build_consts():
    c = {}
    masks = [_mask_tile(1, d) for d in range(-1, 4)] + [_mask_tile(4, d) for d in range(-4, 4)]
    c["c_masks"] = np.stack(masks).transpose(1, 0, 2).astype(ml_dtypes.bfloat16)
    c["c_identb"] = np.eye(128, dtype=np.float32).astype(ml_dtypes.bfloat16)
    c["c_identf"] = np.eye(128, dtype=np.float32)
    half = 32
    inv = (150000.0 ** (-np.arange(half, dtype=np.float32) / half)).astype(np.float32)
    rc = np.zeros((64, 2), np.float32)
    rc[:, 0] = np.concatenate([inv, inv])
    rc[:, 1] = np.concatenate([-np.ones(32), np.ones(32)])
    c["c_rope"] = rc
    i_ = np.arange(128)[:, None]
    j_ = np.arange(128)[None, :]
    ssd = np.zeros((128, 5, 128), np.float32)
    ssd[:, 4] = (i_ < j_)
    ssd[:, 0] = (i_ <= j_)
    ssd[:, 1] = (i_ > j_)
    ssd[:, 2] = np.where(j_ >= i_, 0.0, NEGB)
    ssd[:, 3] = 1.0
    c["c_ssd"] = ssd
    rw = np.zeros((128, 642), np.float32)
    rw[0:64, 0:64] = 1.0
    rw[64:128, 64:128] = 1.0
    rw[0:64, 128] = 1.0
    rw[64:128, 129] = 1.0
    cmk = np.ones((512,), np.float32)
    cmk[::128] = 0.0
    rw[:, 130:642] = cmk[None, :]
    c["c_rw"] = rw
    n_ = np.arange(128)[:, None]
    t_ = np.arange(T)[None, :]
    c["c_cmpmask"] = np.where((16 * n_ + 31 <= t_) & (n_ < 127), 0.0, NEGB).astype(np.float32).astype(ml_dtypes.bfloat16)
    E = np.zeros((32, 16, 128), np.float32)
    for k in range(16):
        for s_ in range(128):
            E[2 * k + s_ // 64, k, s_] = 1.0
    c["c_E"] = E.astype(ml_dtypes.bfloat16)
    sel = np.zeros((24, 24, 128), np.float32)
    for i in range(24):
        sel[i, i, :] = 1.0
    c["c_sel"] = sel.astype(ml_dtypes.bfloat16)
    jj = np.arange(32)[None, :]
    cs = (16 * np.arange(128))[:, None]
    ovl = ((cs < 64 * jj + 64) & (cs + 32 > 64 * jj)).astype(np.float32)
    ovl[127, :] = 0.0
    c["c_ovl"] = ovl
    tt = np.arange(T)[:, None]
    blk = tt // 64
    valid = jj <= blk
    forced = (jj == 0) | (jj == blk)
    vmul = (valid & ~forced).astype(np.float32)
    amask = np.where(forced, 1e30, np.where(valid, 0.0, -1e30)).astype(np.float32)
    c["c_vmul"] = np.ascontiguousarray(vmul.reshape(16, 128, 32).transpose(1, 0, 2))
    c["c_amask"] = np.ascontiguousarray(amask.reshape(16, 128, 32).transpose(1, 0, 2))
    return c


CONST_SPECS = {
    "c_masks": ([128, N_MASKS, 512], BF16),
    "c_identb": ([128, 128], BF16),
    "c_identf": ([128, 128], F32),
    "c_rope": ([64, 2], F32),
    "c_ssd": ([128, 5, 128], F32),
    "c_rw": ([128, 642], F32),
    "c_cmpmask": ([128, T], BF16),
    "c_E": ([32, 16, 128], BF16),
    "c_sel": ([24, 24, 128], BF16),
    "c_ovl": ([128, 32], F32),
    "c_vmul": ([128, 16, 32], F32),
    "c_amask": ([128, 16, 32], F32),
}

ROW_NORM_MIX = 0
ROW_NORM_FFN = 1024
ROW_SINKS = 2048
ROW_DTB = 2056
ROW_ALOG = 2064
ROW_DSK = 2072
ROW_SSDN = 2080
ROW_LNW = 2592
ROW_LNB = 3104
ROW_PER_LAYER = 3616
COL_CONVW = 0
COL_CONVB = 32
COL_MU = 40
COL_W0 = 54
COL_A0 = 58
COL_KK = 62
COL_KA = 66
COL_RK = 70
COL_PER_LAYER = 80
ROW_FINAL = DEPTH * ROW_PER_LAYER
ROW_TOTAL = ROW_FINAL + 1024


def build_rows(inp):
    rows = np.zeros((ROW_TOTAL,), np.float32)
    for l in range(DEPTH):
        o = l * ROW_PER_LAYER
        rows[o + ROW_NORM_MIX:o + ROW_NORM_MIX + 1024] = inp["norm_mix"][l]
        rows[o + ROW_NORM_FFN:o + ROW_NORM_FFN + 1024] = inp["norm_ffn"][l]
        rows[o + ROW_SINKS:o + ROW_SINKS + 8] = inp["swa_sinks"][l]
        rows[o + ROW_DTB:o + ROW_DTB + 8] = inp["ssd_dt_bias"][l]
        rows[o + ROW_ALOG:o + ROW_ALOG + 8] = inp["ssd_a_log"][l]
        rows[o + ROW_DSK:o + ROW_DSK + 8] = inp["ssd_d"][l]
        rows[o + ROW_SSDN:o + ROW_SSDN + 512] = inp["ssd_norm"][l]
        rows[o + ROW_LNW:o + ROW_LNW + 512] = inp["rwkv_ln_w"][l]
        rows[o + ROW_LNB:o + ROW_LNB + 512] = inp["rwkv_ln_b"][l]
    rows[ROW_FINAL:ROW_FINAL + 1024] = inp["norm_final"]
    return np.ascontiguousarray(np.broadcast_to(rows[None, :], (128, ROW_TOTAL)))


def build_cols(inp):
    cols = np.zeros((128, DEPTH * COL_PER_LAYER), np.float32)
    for l in range(DEPTH):
        o = l * COL_PER_LAYER
        cw = np.asarray(inp["ssd_conv_w"][l])
        cols[:, o + COL_CONVW:o + COL_CONVW + 32] = cw.reshape(4, 8, 128).transpose(2, 1, 0).reshape(128, 32)
        cols[:, o + COL_CONVB:o + COL_CONVB + 8] = np.asarray(inp["ssd_conv_b"][l]).reshape(8, 128).T
        cols[:, o + COL_MU:o + COL_MU + 14] = np.asarray(inp["rwkv_mu"][l]).reshape(14, 128).T
        for nm, co in (("rwkv_w0", COL_W0), ("rwkv_a0", COL_A0), ("rwkv_k_k", COL_KK), ("rwkv_k_a", COL_KA), ("rwkv_r_k", COL_RK)):
            cols[:, o + co:o + co + 4] = np.asarray(inp[nm][l]).reshape(4, 128).T
    return np.ascontiguousarray(cols)


WEIGHT_SPECS = {
    "w_in": [DEPTH, D, D_IN],
    "w_br_ssd": [DEPTH, 512, D],
    "w_br_nsa": [DEPTH, 512, D],
    "w_br_rwkv": [DEPTH, 512, D],
    "w_br_swa": [DEPTH, 512, D],
    "w_out": [DEPTH, D, D],
    "ffn_w_gate": [1, D, D_FF],
    "ffn_w_up": [1, D, D_FF],
    "ffn_w_down": [1, D_FF, D],
    "moe_router": [1, D, 8],
    "moe_w_gate": [1, 8, D, D_FF],
    "moe_w_up": [1, 8, D, D_FF],
    "moe_w_down": [1, 8, D_FF, D],
    "ple_proj": [DEPTH, 256, D],
    "ple_gate": [DEPTH, D, D],
    "nsa_cmp_w1": [DEPTH, 2, 32, 64, 64],
    "nsa_cmp_w2": [DEPTH, 2, 64, 64],
    "nsa_peT": [DEPTH, 2, 64, 32],
    "rwkv_w_up": [DEPTH, 64, 512],
    "rwkv_a_up": [DEPTH, 64, 512],
    "rwkv_g_up": [DEPTH, 128, 512],
}


def build(n_layers=DEPTH, mixers=("ssd", "nsa", "rwkv", "swa"), debug=False):
    p = Prog()
    nc = p.nc
    EI = "ExternalInput"
    x_in = p.dram("x", [T, D], F32, EI)
    p_in = p.dram("p", [DEPTH, T, 256], F32, EI)
    pos_in = p.dram("pos", [64, T], I32, EI)
    rows_in = p.dram("rows", [128, ROW_TOTAL], F32, EI)
    cols_in = p.dram("cols", [128, DEPTH * COL_PER_LAYER], F32, EI)
    W = {k: p.dram(k, shp, F32, EI) for k, shp in WEIGHT_SPECS.items()}
    C = {k: p.dram(k, shp, dt, EI) for k, (shp, dt) in CONST_SPECS.items()}
    out_d = p.dram("out", [T, D], F32, "ExternalOutput")
    xs = p.dram("xs", [T, D], F32)
    yT_d = [p.dram("yT%d" % m, [4, 128, T], BF16) for m in range(4)]
    dbg = {}
    if debug:
        for m in range(4):
            dbg["yT%d" % m] = p.dram("dbg_yT%d" % m, [4, 128, T], BF16, "ExternalOutput")
        dbg["x_mix"] = p.dram("dbg_x_mix", [T, D], F32, "ExternalOutput")
        dbg["x_ffn"] = p.dram("dbg_x_ffn", [T, D], F32, "ExternalOutput")
        dbg["x_l0"] = p.dram("dbg_x_l0", [T, D], F32, "ExternalOutput")

    def dump(name, ap, shape, dt, reads):
        if not debug:
            return
        d = p.dram("dbg_" + name, shape, dt, "ExternalOutput")
        p.dma(d.t.ap() if False else d[tuple(slice(None) for _ in shape)], ap, reads=reads, writes=[d], is_output=True)

    masks = p.sb([128, N_MASKS, 512], BF16, "masks")
    identb = p.sb([128, 128], BF16, "identb")
    identf = p.sb([128, 128], F32, "identf")
    ropec = p.sb([64, 2], F32, "ropec")
    rows = p.sb([128, ROW_PER_LAYER], F32, "rows_sb")
    rowsf = p.sb([128, 1024], F32, "rowsf_sb")
    hT = p.sb([128, 8, T], BF16, "hT")
    p.dma(masks[:], C["c_masks"][:], writes=[masks])
    p.dma(identb[:], C["c_identb"][:], writes=[identb])
    p.dma(identf[:], C["c_identf"][:], writes=[identf])
    p.dma(ropec[:], C["c_rope"][:], writes=[ropec])
    p.dma(rowsf[:], rows_in[:, ROW_FINAL:ROW_FINAL + 1024], writes=[rowsf])
    cols = p.sb([128, DEPTH * COL_PER_LAYER], F32, "cols_sb")
    p.dma(cols[:], cols_in[:], writes=[cols])
    cssd = p.sb([128, 5, 128], F32, "cssd")
    p.dma(cssd[:], C["c_ssd"][:], writes=[cssd])
    epsc = p.sb([128, 1], F32, "epsc")
    p.op("vector", lambda e: e.memset(epsc[:], 1e-6), writes=[epsc])

    NSTG = 2
    STGW = 2048
    stg = [p.sb([128, STGW], F32, "stg%d" % i) for i in range(NSTG)]
    stgi = [0]

    p.make_arena((nc.sbuf_bytes_remaining - 2048) // 4)

    psb = [p.ps([128, 512], F32, "psb%d" % i) for i in range(6)]
    pst = [p.ps([128, 1024], BF16, "pst%d" % i) for i in range(2)]
    psi = [0]
    psti = [0]

    pli = [0]

    def nps():
        b = psb[psi[0] % 4]
        psi[0] += 1
        return b

    def npl():
        b = psb[4 + pli[0] % 2]
        pli[0] += 1
        return b

    def npst():
        b = pst[psti[0] % 2]
        psti[0] += 1
        return b

    cast_rr = [0]

    def load_w(dst, dst_ap_fn, src_ap, shape, key=None, parts=128):
        a, b = shape
        assert a * b <= STGW, (a, b)
        s = stg[stgi[0] % NSTG]
        stgi[0] += 1
        sv = s[0:parts, 0:a * b].rearrange("p (a b) -> p a b", a=a)
        p.dma(sv, src_ap, writes=[s])
        for ent in dst_ap_fn(sv):
            if len(ent) == 3:
                dbuf, o_ap, i_ap = ent
            else:
                dbuf = dst
                o_ap, i_ap = ent
            eng = "gpsimd"
            p.op(eng, lambda e, o=o_ap, i=i_ap: e.tensor_copy(o, i), reads=[s], writes=[(dbuf, key)])

    def load_w_plain(dst, dview, src_ap, shape, key=None):
        a, b = shape
        bc = max(1, STGW // a)
        for b0 in range(0, b, bc):
            bw = min(bc, b - b0)
            load_w(dst, lambda sv, b0=b0, bw=bw: [(dview[:, :, b0:b0 + bw], sv)],
                   src_ap[:, :, b0:b0 + bw], (a, bw), key)

    def win_cols(l, c0, n):
        return W["w_in"][l, :, c0:c0 + n].rearrange("(c p) n -> p c n", p=128)

    def rmsnorm_to_hT(src_tile_fn, grow0, also=None):
        mk = p.mark()
        hb = [p.carve([128, D], BF16, "hb%d" % i) for i in range(2)]
        junk = p.carve([128, D], F32, "junk")
        st = [p.carve([128, 4], F32, "st%d" % i) for i in range(2)]
        for i in range(NT):
            xb, xap, xk = src_tile_fn(i)
            s_ = st[i % 2]
            h_ = hb[i % 2]
            p.op("scalar", lambda e, xap=xap, s_=s_: e.activation(junk[:], xap, AF.Square, accum_out=s_[:, 0:1]),
                 reads=[(xb, xk)], writes=[junk, s_])
            p.op("scalar", lambda e, s_=s_: e.activation(s_[:, 1:2], s_[:, 0:1], AF.Sqrt, bias=epsc[:, 0:1], scale=1.0 / D),
                 reads=[s_, epsc], writes=[s_])
            p.op("vector", lambda e, s_=s_: e.reciprocal(s_[:, 2:3], s_[:, 1:2]), reads=[s_], writes=[s_])
            p.op("vector", lambda e, xap=xap, s_=s_, h_=h_: e.scalar_tensor_tensor(
                out=h_[:], in0=xap, scalar=s_[:, 2:3], in1=rows[:, grow0:grow0 + D], op0=ALU.mult, op1=ALU.mult),
                 reads=[(xb, xk), s_, rows], writes=[h_])
            if also is not None:
                also(i, s_, xb, xap, xk)
            pt = npst()
            for c in range(8):
                p.op("tensor", lambda e, pt=pt, c=c, h_=h_: e.transpose(pt[:, c * 128:(c + 1) * 128], h_[:, c * 128:(c + 1) * 128], identb[:]),
                     reads=[h_, identb], writes=[pt])
            eng = "vector" if i % 2 == 0 else "scalar"
            dstv = hT[:, :, i * 128:(i + 1) * 128]
            srcv = pt[:, :].rearrange("p (c t) -> p c t", c=8)
            if eng == "vector":
                p.op("vector", lambda e, dstv=dstv, srcv=srcv: e.tensor_copy(dstv, srcv), reads=[pt], writes=[(hT, i)])
            else:
                p.op("scalar", lambda e, dstv=dstv, srcv=srcv: e.copy(dstv, srcv), reads=[pt], writes=[(hT, i)])
        p.release(mk)

    def hT_reads(t0, tw):
        return [(hT, i) for i in range(t0 // 128, (t0 + tw + 127) // 128)]

    def proj_fm(ps_ap, psbuf, wbuf, wview_fn, t0, tw, wkey=None):
        for c in range(8):
            p.op("tensor", lambda e, c=c: e.matmul(ps_ap, wview_fn(c), hT[:, c, t0:t0 + tw], start=(c == 0), stop=(c == 7)),
                 reads=[(wbuf, wkey)] + hT_reads(t0, tw), writes=[psbuf])

    def proj_tm(ps_ap, psbuf, wbuf, wview_fn, i, wkey=None):
        for c in range(8):
            p.op("tensor", lambda e, c=c: e.matmul(ps_ap, hT[:, c, i * 128:(i + 1) * 128], wview_fn(c), start=(c == 0), stop=(c == 7)),
                 reads=[(wbuf, wkey), (hT, i)], writes=[psbuf])

    def attn_group(qT, qkey, h, b, kT, g, vext, pairs, PT, den_extra_col, out_cb):
        ps2 = npl()
        n = len(pairs)

        def emit_S(idx):
            j, extra = pairs[idx]
            ps1 = nps()
            nx = len(extra)
            p.op("tensor", lambda e: e.matmul(ps1[:, :], kT[0:64, g, j * 128:(j + 1) * 128], qT[0:64, h, b * 512:(b + 1) * 512],
                                              start=True, stop=(nx == 0)),
                 reads=[(kT, None), (qT, qkey)], writes=[ps1])
            for xi, (la, ra, rd) in enumerate(extra):
                p.op("tensor", lambda e: e.matmul(ps1[:, :], la, ra, start=False, stop=(xi == nx - 1)), reads=rd, writes=[ps1])
            pt_ = PT[idx % len(PT)]
            p.op("scalar", lambda e: e.activation(pt_[:, :], ps1[:, :], AF.Exp, scale=0.125), reads=[ps1], writes=[pt_])

        def emit_PV(idx):
            j, extra = pairs[idx]
            pt_ = PT[idx % len(PT)]
            p.op("tensor", lambda e: e.matmul(ps2[:, :], vext[:, j, g, :], pt_[:, :], start=(idx == 0), stop=(idx == n - 1)),
                 reads=[pt_, (vext, None)], writes=[ps2])

        look = len(PT) - 1
        for i0 in range(min(look, n)):
            emit_S(i0)
        for idx in range(n):
            if idx + look < n:
                emit_S(idx + look)
            emit_PV(idx)
        out_cb(ps2)

    def mixer_swa(l):
        mk = p.mark()
        qT = p.carve([64, 8, T], BF16, "swa_qT")
        kT = p.carve([64, 2, T], BF16, "swa_kT")
        vext = p.carve([128, NT, 2, 128], BF16, "swa_v")
        cosT = p.carve([64, T], F32, "cosT")
        sinT = p.carve([64, T], F32, "sinT")
        PT = [p.carve([128, 512], BF16, "PT%d" % i) for i in range(4)]
        sinke = p.carve([128, 8], F32, "sinke")
        mk2 = p.mark()
        posi = p.carve([64, T], I32, "posi")
        ang = p.carve([64, T], F32, "ang")
        tmpf = p.carve([64, T], F32, "tmpf")
        tmpi = p.carve([64, T], I32, "tmpi")
        p.dma(posi[:], pos_in[:], writes=[posi])
        p.op("vector", lambda e: e.tensor_copy(ang[:], posi[:]), reads=[posi], writes=[ang])
        p.op("vector", lambda e: e.tensor_scalar(ang[:], ang[:], ropec[:, 0:1], None, ALU.mult), reads=[ang, ropec], writes=[ang])
        TWO_PI = 2.0 * np.pi
        for shift, dst in ((0.0, sinT), (np.pi / 2, cosT)):
            p.op("vector", lambda e, shift=shift: e.tensor_scalar(tmpf[:], ang[:], float(shift), 1.0 / TWO_PI, ALU.add, ALU.mult),
                 reads=[ang], writes=[tmpf])
            p.op("vector", lambda e: e.tensor_copy(tmpi[:], tmpf[:]), reads=[tmpf], writes=[tmpi])
            p.op("vector", lambda e: e.tensor_copy(tmpf[:], tmpi[:]), reads=[tmpi], writes=[tmpf])
            p.op("vector", lambda e: e.scalar_tensor_tensor(out=tmpf[:], in0=tmpf[:], scalar=-TWO_PI, in1=ang[:], op0=ALU.mult, op1=ALU.add),
                 reads=[tmpf, ang], writes=[tmpf])
            p.op("vector", lambda e, shift=shift: e.tensor_scalar(tmpf[:], tmpf[:], float(shift), 3.14159, ALU.add, ALU.min),
                 reads=[tmpf], writes=[tmpf])
            p.op("vector", lambda e: e.tensor_scalar(tmpf[:], tmpf[:], -3.14159, None, ALU.max), reads=[tmpf], writes=[tmpf])
            p.op("scalar", lambda e, dst=dst: e.activation(dst[:], tmpf[:], AF.Sin), reads=[tmpf], writes=[dst])
        p.op("vector", lambda e: e.tensor_scalar(sinT[:], sinT[:], ropec[:, 1:2], None, ALU.mult), reads=[sinT, ropec], writes=[sinT])
        p.release(mk2)
        ro = 0 + ROW_SINKS
        p.op("scalar", lambda e: e.activation(sinke[:], rows[:, ro:ro + 8], AF.Exp), reads=[rows], writes=[sinke])
        wq = p.carve([128, 8, 512], BF16, "swa_wq")
        wqs = p.carve([128, 8, 512], BF16, "swa_wqs")
        wkv = p.carve([128, 8, 256], BF16, "swa_wkv")
        wks = p.carve([128, 8, 128], BF16, "swa_wks")
        for h0 in range(0, 8, 4):
            def cast_q(sv, h0=h0):
                s4 = sv.rearrange("p c (h d) -> p c h d", d=64)
                ops_ = [(wq[:, :, h0 * 64:(h0 + 4) * 64], sv)]
                d4 = wqs[:, :, h0 * 64:(h0 + 4) * 64].rearrange("p c (h d) -> p c h d", d=64)
                ops_.append((wqs, d4[:, :, :, 0:32], s4[:, :, :, 32:64]))
                ops_.append((wqs, d4[:, :, :, 32:64], s4[:, :, :, 0:32]))
                return ops_
            load_w(wq, cast_q, win_cols(l, OFF_SQ + h0 * 64, 256), (8, 256))
        p.fence(wq)

        def cast_kv(sv):
            ops_ = [(wkv[:, :, :], sv)]
            s4 = sv[:, :, 0:128].rearrange("p c (h d) -> p c h d", d=64)
            d4 = wks[:, :, :].rearrange("p c (h d) -> p c h d", d=64)
            ops_.append((wks, d4[:, :, :, 0:32], s4[:, :, :, 32:64]))
            ops_.append((wks, d4[:, :, :, 32:64], s4[:, :, :, 0:32]))
            return ops_
        load_w(wkv, cast_kv, win_cols(l, OFF_SKV, 256), (8, 256))
        t1 = [p.carve([64, 512], F32, "rt1_%d" % i) for i in range(2)]
        t2 = [p.carve([64, 512], F32, "rt2_%d" % i) for i in range(2)]
        cnt = 0
        for dstT, nh, wa, wb, wbufa, wbufb in ((kT, 2, wkv, wks, wkv, wks), (qT, 8, wq, wqs, wq, wqs)):
            for h in range(nh):
                for b in range(4):
                    pa = nps()
                    pb = nps()
                    proj_fm(pa[0:64, :], pa, wbufa, lambda c, wa=wa, h=h: wa[:, c, h * 64:(h + 1) * 64], b * 512, 512)
                    proj_fm(pb[0:64, :], pb, wbufb, lambda c, wb=wb, h=h: wb[:, c, h * 64:(h + 1) * 64], b * 512, 512)
                    a_ = t1[cnt % 2]
                    b_ = t2[cnt % 2]
                    cnt += 1
                    p.op("vector", lambda e, a_=a_, pa=pa, b=b: e.tensor_tensor(a_[:, :], pa[0:64, :], cosT[:, b * 512:(b + 1) * 512], ALU.mult),
                         reads=[pa, cosT], writes=[a_])
                    p.op("vector", lambda e, b_=b_, pb=pb, b=b: e.tensor_tensor(b_[:, :], pb[0:64, :], sinT[:, b * 512:(b + 1) * 512], ALU.mult),
                         reads=[pb, sinT], writes=[b_])
                    p.op("gpsimd", lambda e, a_=a_, b_=b_, dstT=dstT, h=h, b=b: e.tensor_tensor(dstT[:, h, b * 512:(b + 1) * 512], a_[:, :], b_[:, :], ALU.add),
                         reads=[a_, b_], writes=[(dstT, (h, b))])
        p.fence(kT)
        p.op("gpsimd", lambda e: e.memset(vext[:, :, :, 64:128], 1.0), writes=[vext])
        for i in range(NT):
            pv = nps()
            proj_tm(pv[:, 0:128], pv, wkv, lambda c: wkv[:, c, 128:256], i)
            p.op("vector", lambda e, pv=pv, i=i: e.tensor_copy(vext[:, i, :, 0:64], pv[:, 0:128].rearrange("p (g d) -> p g d", g=2)),
                 reads=[pv], writes=[(vext, i)])
        p.fence(vext)
        rec = [p.carve([128, 512], F32, "rec%d" % i) for i in range(2)]
        yo = [p.carve([64, 512], BF16, "yo%d" % i) for i in range(2)]
        cnt = 0
        for h in range(8):
            g = h // 4
            for b in range(4):
                pairs = []
                for j in range(4 * b - 1, 4 * b + 4):
                    if j < 0:
                        continue
                    d = j - 4 * b
                    pairs.append((j, [(identb[:, :], masks[:, SWA_MASK0 + d + 1, :], [identb, masks])]))
                r_ = rec[cnt % 2]
                y_ = yo[cnt % 2]
                cnt += 1

                def fin(ps2, r_=r_, y_=y_, h=h, b=b):
                    p.op("vector", lambda e: e.tensor_scalar(r_[64:128, :], ps2[64:128, :], sinke[64:128, h:h + 1], None, ALU.add),
                         reads=[ps2, sinke], writes=[r_])
                    p.op("vector", lambda e: e.reciprocal(r_[64:128, :], r_[64:128, :]), reads=[r_], writes=[r_])
                    p.op("vector", lambda e: e.tensor_tensor(y_[:, :], ps2[0:64, :], r_[64:128, :], ALU.mult), reads=[ps2, r_], writes=[y_])
                    p.dma(yT_d[3][h // 2, (h % 2) * 64:(h % 2) * 64 + 64, b * 512:(b + 1) * 512], y_[:, :], reads=[y_], writes=[(yT_d[3], (h, b))])
                attn_group(qT, (h, b), h, b, kT, g, vext, pairs, PT, None, fin)
        p.fence(yT_d[3])
        p.release(mk)


    def mixer_nsa(l):
        mk = p.mark()
        cmpmask = p.carve([128, T], BF16, "cmpmask")
        Emat = p.carve([32, 16, 128], BF16, "Emat")
        Sel = p.carve([24, 24, 128], BF16, "Sel")
        ovl = p.carve([128, 32], F32, "ovl")
        vmul = p.carve([128, 16, 32], F32, "vmul")
        amask = p.carve([128, 16, 32], F32, "amask")
        p.dma(cmpmask[:, :], C["c_cmpmask"][:, :], writes=[cmpmask])
        p.dma(Emat[:, :, :], C["c_E"][:, :, :], writes=[Emat])
        p.dma(Sel[:, :, :], C["c_sel"][:, :, :], writes=[Sel])
        p.dma(ovl[:, :], C["c_ovl"][:, :], writes=[ovl])
        p.dma(vmul[:, :, :], C["c_vmul"][:, :, :], writes=[vmul])
        p.dma(amask[:, :, :], C["c_amask"][:, :, :], writes=[amask])
        gT = p.carve([24, T], BF16, "nsa_gT")
        wg = p.carve([128, 8, 24], BF16, "nsa_wg")
        load_w(wg, lambda sv: [(wg[:, :, :], sv)], win_cols(l, OFF_NG, 24), (8, 24))
        for b in range(4):
            pg = nps()
            proj_fm(pg[0:24, :], pg, wg, lambda c: wg[:, c, :], b * 512, 512)
            p.op("scalar", lambda e, pg=pg, b=b: e.activation(gT[:, b * 512:(b + 1) * 512], pg[0:24, :], AF.Sigmoid), reads=[pg], writes=[(gT, b)])
        p.fence(gT)
        w1 = [p.carve([64, 32, 64], BF16, "cw1_%d" % i) for i in range(2)]
        w2 = [p.carve([64, 64], BF16, "cw2_%d" % i) for i in range(2)]
        peT = [p.carve([64, 32], BF16, "cpe_%d" % i) for i in range(2)]
        for kv in range(2):
            load_w(w1[kv], lambda sv, kv=kv: [(w1[kv][:, :, :], sv[0:64, :, :])],
                   W["nsa_cmp_w1"][l, kv].rearrange("l d f -> d l f"), (32, 64), parts=64)
            load_w(w2[kv], lambda sv, kv=kv: [(w2[kv][:, :], sv[0:64, 0, :])], W["nsa_cmp_w2"][l, kv].unsqueeze(1), (1, 64), parts=64)
            load_w(peT[kv], lambda sv, kv=kv: [(peT[kv][:, :], sv[0:64, 0, :])], W["nsa_peT"][l, kv].unsqueeze(1), (1, 32), parts=64)
        qT = p.carve([64, 4, T], BF16, "nsa_qT")
        ycmp = p.carve([64, 4, T], BF16, "nsa_ycmp")
        ksT = p.carve([64, 1, T], BF16, "nsa_ksT")
        kwT = p.carve([64, 1, T], BF16, "nsa_kwT")
        vs = p.carve([128, NT, 1, 128], BF16, "nsa_vs")
        vw = p.carve([128, NT, 1, 128], BF16, "nsa_vw")
        impT = p.carve([32, T], F32, "nsa_impT")
        selbT = p.carve([32, 1, T], BF16, "nsa_selbT")
        kcmpT = p.carve([64, 1, 128], BF16, "nsa_kcmpT")
        vcmp = p.carve([128, 64], BF16, "nsa_vcmp")
        for g in range(2):
            mk_g = p.mark()
            kcvT = p.carve([64, 2, T], BF16, "nsa_kcvT")
            hidb = [p.carve([64, 128], BF16, "nsa_hid%d" % i) for i in range(2)]
            wq = p.carve([128, 8, 256], BF16, "nsa_wq")
            wkv = p.carve([128, 8, 6, 64], BF16, "nsa_wkv")
            load_w(wq, lambda sv: [(wq[:, :, :], sv)], win_cols(l, OFF_NQ + g * 256, 256), (8, 256))
            for part in range(6):
                load_w(wkv, lambda sv, part=part: [(wkv[:, :, part, :], sv)], win_cols(l, OFF_NKV + part * 128 + g * 64, 64), (8, 64), key=part)
            p.fence(wkv)
            cnt = 0
            for hh in range(4):
                for b in range(4):
                    pa = nps()
                    proj_fm(pa[0:64, :], pa, wq, lambda c, hh=hh: wq[:, c, hh * 64:(hh + 1) * 64], b * 512, 512)
                    eng = "vector" if cnt % 2 == 0 else "scalar"
                    cnt += 1
                    if eng == "vector":
                        p.op("vector", lambda e, pa=pa, hh=hh, b=b: e.tensor_copy(qT[:, hh, b * 512:(b + 1) * 512], pa[0:64, :]), reads=[pa], writes=[(qT, (hh, b))])
                    else:
                        p.op("scalar", lambda e, pa=pa, hh=hh, b=b: e.copy(qT[:, hh, b * 512:(b + 1) * 512], pa[0:64, :]), reads=[pa], writes=[(qT, (hh, b))])
            for part, dstT, di in ((0, kcvT, 0), (1, kcvT, 1), (2, ksT, 0), (4, kwT, 0)):
                for b in range(4):
                    pa = nps()
                    proj_fm(pa[0:64, :], pa, wkv, lambda c, part=part: wkv[:, c, part, :], b * 512, 512)
                    p.op("vector", lambda e, pa=pa, dstT=dstT, di=di, b=b: e.tensor_copy(dstT[:, di, b * 512:(b + 1) * 512], pa[0:64, :]), reads=[pa], writes=[(dstT, (di, b))])
            p.fence(kcvT)
            p.fence(ksT)
            p.fence(kwT)
            for part, vdst in ((3, vs), (5, vw)):
                p.op("gpsimd", lambda e, vdst=vdst: e.memset(vdst[:, :, :, 64:128], 1.0), writes=[vdst])
                for i in range(NT):
                    pv = nps()
                    proj_tm(pv[:, 0:64], pv, wkv, lambda c, part=part: wkv[:, c, part, :], i)
                    p.op("vector", lambda e, pv=pv, i=i, vdst=vdst: e.tensor_copy(vdst[:, i, 0, 0:64], pv[:, 0:64]), reads=[pv], writes=[(vdst, i)])
                p.fence(vdst)
            p.op("gpsimd", lambda e: e.memset(kcmpT[:, :, :], 0.0), writes=[kcmpT])
            p.op("gpsimd", lambda e: e.memset(vcmp[:, :], 0.0), writes=[vcmp])
            for kv in range(2):
                ph = nps()
                src3 = kcvT[:, kv, :].rearrange("p (n s) -> p n s", s=16)
                for ll in range(32):
                    rhs = src3[:, 0:127, ll] if ll < 16 else src3[:, 1:128, ll - 16]
                    p.op("tensor", lambda e, ph=ph, ll=ll, rhs=rhs, kv=kv: e.matmul(ph[0:64, 0:127], w1[kv][:, ll, :], rhs, start=(ll == 0), stop=False),
                         reads=[w1[kv], kcvT], writes=[ph])
                    p.op("tensor", lambda e, ph=ph, ll=ll, kv=kv: e.matmul(ph[0:64, 0:127], w1[kv][:, ll, :], peT[kv][:, ll:ll + 1].to_broadcast([64, 127]), start=False, stop=(ll == 31)),
                         reads=[w1[kv], peT[kv]], writes=[ph])
                hb = hidb[kv]
                p.op("scalar", lambda e, hb=hb, ph=ph: e.activation(hb[:, 0:127], ph[0:64, 0:127], AF.Silu), reads=[ph], writes=[hb])
                pc = nps()
                if kv == 0:
                    p.op("tensor", lambda e, pc=pc, hb=hb: e.matmul(pc[0:64, 0:127], w2[0][:, :], hb[:, 0:127], start=True, stop=True), reads=[w2[0], hb], writes=[pc])
                    p.op("vector", lambda e, pc=pc: e.tensor_copy(kcmpT[:, 0, 0:127], pc[0:64, 0:127]), reads=[pc], writes=[kcmpT])
                else:
                    p.op("tensor", lambda e, pc=pc, hb=hb: e.matmul(pc[0:127, 0:64], hb[:, 0:127], w2[1][:, :], start=True, stop=True), reads=[w2[1], hb], writes=[pc])
                    p.op("vector", lambda e, pc=pc: e.tensor_copy(vcmp[0:127, :], pc[0:127, 0:64]), reads=[pc], writes=[vcmp])
            p.release(mk_g)
            PT = [p.carve([128, 512], BF16, "nPT%d" % i) for i in range(3)]
            PTf = [p.carve([128, 512], F32, "nPTf%d" % i) for i in range(2)]
            recd = p.carve([128, 512], F32, "nrecd")
            pn = p.carve([128, 512], F32, "npn")
            pnb = p.carve([128, 512], BF16, "npnb")
            gs = [p.carve([128, 512], F32, "ngs%d" % i) for i in range(2)]
            rec = [p.carve([128, 512], F32, "nrec%d" % i) for i in range(2)]
            tsel = [p.carve([64, 512], F32, "ntsel%d" % i) for i in range(2)]
            yo = [p.carve([64, 512], BF16, "nyo%d" % i) for i in range(2)]
            sc = [p.carve([128, 32], F32, "nsc%d" % i) for i in range(2)]
            m8 = [p.carve([128, 8], F32, "nm8%d" % i) for i in range(2)]
            psimp = psb[4]
            recd2 = [recd, p.carve([128, 512], F32, "nrecd2")]
            pn2 = [pn, p.carve([128, 512], F32, "npn2")]
            pnb2 = [pnb, p.carve([128, 512], BF16, "npnb2")]
            units = [(b, hh) for b in range(4) for hh in range(4)]

            def cmpA(i):
                b, hh = units[i]
                bs = slice(b * 512, (b + 1) * 512)
                ps1 = nps()
                p.op("tensor", lambda e: e.matmul(ps1[:, :], kcmpT[:, 0, :], qT[:, hh, bs], start=True, stop=False),
                     reads=[kcmpT, (qT, (hh, b))], writes=[ps1])
                p.op("tensor", lambda e: e.matmul(ps1[:, :], identb[:, :], cmpmask[:, bs], start=False, stop=True),
                     reads=[identb, cmpmask], writes=[ps1])
                ptf = PTf[i % 2]
                p.op("scalar", lambda e: e.activation(ptf[:, :], ps1[:, :], AF.Exp, scale=0.125), reads=[ps1], writes=[ptf])
                psd = nps()
                p.op("tensor", lambda e: e.matmul(psd[:, :], cssd[:, 3, :], ptf[:, :], start=True, stop=True), reads=[cssd, ptf], writes=[psd])
                rd, pn_, pnb_ = recd2[i % 2], pn2[i % 2], pnb2[i % 2]
                p.op("vector", lambda e: e.tensor_scalar(rd[:, :], psd[:, :], 1e-30, None, ALU.max), reads=[psd], writes=[rd])
                p.op("vector", lambda e: e.reciprocal(rd[:, :], rd[:, :]), reads=[rd], writes=[rd])
                p.op("vector", lambda e: e.tensor_tensor(pn_[:, :], ptf[:, :], rd[:, :], ALU.mult), reads=[ptf, rd], writes=[pn_])
                p.op("gpsimd", lambda e: e.tensor_copy(pnb_[:, :], pn_[:, :]), reads=[pn_], writes=[pnb_])

            def cmpB(i):
                b, hh = units[i]
                h = g * 4 + hh
                bs = slice(b * 512, (b + 1) * 512)
                pn_, pnb_ = pn2[i % 2], pnb2[i % 2]
                p.op("tensor", lambda e: e.matmul(psimp[0:32, :], ovl[:, :], pn_[:, :], start=(hh == 0), stop=(hh == 3)), reads=[ovl, pn_], writes=[psimp])
                pso = nps()
                p.op("tensor", lambda e: e.matmul(pso[0:64, :], vcmp[:, :], pnb_[:, :], start=True, stop=True), reads=[vcmp, pnb_], writes=[pso])
                pgb = nps()
                p.op("tensor", lambda e: e.matmul(pgb[:, :], Sel[:, 0 * 8 + h, :], gT[:, bs], start=True, stop=True), reads=[Sel, gT], writes=[pgb])
                gs_ = gs[hh % 2]
                p.op("scalar", lambda e: e.copy(gs_[0:64, :], pgb[0:64, :]), reads=[pgb], writes=[gs_])
                p.op("vector", lambda e: e.tensor_tensor(ycmp[:, hh, bs], pso[0:64, :], gs_[0:64, :], ALU.mult),
                     reads=[pso, gs_], writes=[(ycmp, (hh, b))])
                if hh == 3:
                    p.op("scalar", lambda e: e.copy(impT[:, bs], psimp[0:32, :]), reads=[psimp], writes=[(impT, b)])

            cmpA(0)
            for i in range(len(units)):
                if i + 1 < len(units):
                    cmpA(i + 1)
                cmpB(i)
            p.fence(impT)
            for i in range(NT):
                ts_ = slice(i * 128, (i + 1) * 128)
                pt1 = nps()
                p.op("tensor", lambda e, pt1=pt1, ts_=ts_: e.transpose(pt1[:, 0:32], impT[:, ts_], identf[0:32, 0:32]), reads=[impT, identf], writes=[pt1])
                sc_ = sc[i % 2]
                m8_ = m8[i % 2]
                p.op("vector", lambda e, sc_=sc_, pt1=pt1, i=i: e.tensor_tensor(sc_[:, :], pt1[:, 0:32], vmul[:, i, :], ALU.mult), reads=[pt1, vmul], writes=[sc_])
                p.op("vector", lambda e, sc_=sc_, i=i: e.tensor_tensor(sc_[:, :], sc_[:, :], amask[:, i, :], ALU.add), reads=[sc_, amask], writes=[sc_])
                p.op("vector", lambda e, sc_=sc_, m8_=m8_: e.max(m8_[:, :], sc_[:, :]), reads=[sc_], writes=[m8_])
                p.op("vector", lambda e, sc_=sc_, m8_=m8_: e.tensor_scalar(sc_[:, :], sc_[:, :], m8_[:, 7:8], None, ALU.is_ge), reads=[sc_, m8_], writes=[sc_])
                p.op("vector", lambda e, sc_=sc_: e.tensor_scalar(sc_[:, :], sc_[:, :], -1.0, -NEGB, ALU.add, ALU.mult), reads=[sc_], writes=[sc_])
                pt2 = nps()
                p.op("tensor", lambda e, pt2=pt2, sc_=sc_: e.transpose(pt2[0:32, 0:128], sc_[:, :], identf[:, :]), reads=[sc_, identf], writes=[pt2])
                p.op("scalar", lambda e, pt2=pt2, ts_=ts_: e.copy(selbT[:, 0, ts_], pt2[0:32, 0:128]), reads=[pt2], writes=[(selbT, i)])
            p.fence(selbT)
            cnt = 0
            for hh in range(4):
                h = g * 4 + hh
                for b in range(4):
                    bs = slice(b * 512, (b + 1) * 512)
                    res = {}
                    for br in (1, 2):
                        pairs = []
                        if br == 1:
                            for j in range(0, 4 * b + 4):
                                extra = [(Emat[:, j, :], selbT[:, 0, bs], [Emat, selbT])]
                                if j >= 4 * b:
                                    extra.append((identb[:, :], masks[:, NSA_MASK0 + (j - 4 * b) + 4, :], [identb, masks]))
                                pairs.append((j, extra))
                            kT_, v_ = ksT, vs
                        else:
                            for j in range(max(0, 4 * b - 4), 4 * b + 4):
                                pairs.append((j, [(identb[:, :], masks[:, NSA_MASK0 + (j - 4 * b) + 4, :], [identb, masks])]))
                            kT_, v_ = kwT, vw
                        r_ = rec[br - 1]
                        ts2 = tsel[br - 1]

                        def fin(ps2, r_=r_, ts2=ts2, br=br, h=h, bs=bs):
                            pgb = nps()
                            p.op("tensor", lambda e: e.matmul(pgb[:, :], Sel[:, br * 8 + h, :], gT[:, bs], start=True, stop=True), reads=[Sel, gT], writes=[pgb])
                            p.op("vector", lambda e: e.reciprocal(r_[64:128, :], ps2[64:128, :]), reads=[ps2], writes=[r_])
                            p.op("vector", lambda e: e.tensor_tensor(r_[64:128, :], r_[64:128, :], pgb[64:128, :], ALU.mult), reads=[r_, pgb], writes=[r_])
                            p.op("vector", lambda e: e.tensor_tensor(ts2[:, :], ps2[0:64, :], r_[64:128, :], ALU.mult), reads=[ps2, r_], writes=[ts2])
                        attn_group(qT, (hh, b), hh, b, kT_, 0, v_, pairs, PT, None, fin)
                    y_ = yo[cnt % 2]
                    cnt += 1
                    p.op("gpsimd", lambda e, hh=hh, bs=bs: e.tensor_tensor(tsel[0][:, :], tsel[0][:, :], ycmp[:, hh, bs], ALU.add), reads=[tsel[0], (ycmp, (hh, b))], writes=[tsel[0]])
                    p.op("gpsimd", lambda e, y_=y_: e.tensor_tensor(y_[:, :], tsel[0][:, :], tsel[1][:, :], ALU.add), reads=[tsel[0], tsel[1]], writes=[y_])
                    p.dma(yT_d[1][h // 2, (h % 2) * 64:(h % 2) * 64 + 64, bs], y_[:, :], reads=[y_], writes=[(yT_d[1], (h, b))])
            p.fence(qT)
            p.fence(ycmp)
            p.release(mk_g)
        p.fence(yT_d[1])
        p.release(mk)


    def mixer_rwkv(l):
        mk = p.mark()
        ro = 0
        co = l * COL_PER_LAYER
        crw = p.carve([128, 642], F32, "crw")
        p.dma(crw[:, :], C["c_rw"][:, :], writes=[crw])
        bones = crw[:, 0:128]
        hsel = crw[:, 128:130]
        cmask = crw[:, 130:642]
        colv = lambda cidx: cols[:, co + cidx:co + cidx + 1]
        lora_in = p.carve([128, T], BF16, "rw_lora_in")
        sgT = p.carve([128, T], BF16, "rw_sgT")
        wup = p.carve([128, 512], BF16, "rw_wup")
        aup = p.carve([128, 512], BF16, "rw_aup")
        gup = p.carve([128, 512], BF16, "rw_gup")
        p.op("gpsimd", lambda e: e.memset(wup[:, :], 0.0), writes=[wup])
        p.op("gpsimd", lambda e: e.memset(aup[:, :], 0.0), writes=[aup])
        load_w(wup, lambda sv: [(wup[0:64, :], sv[0:64, 0, :])], W["rwkv_w_up"][l].unsqueeze(1), (1, 512), parts=64)
        sA = stg[stgi[0] % NSTG]
        stgi[0] += 1
        p.dma(sA[64:128, 0:512], W["rwkv_a_up"][l], writes=[sA])
        p.op("gpsimd", lambda e: e.tensor_copy(aup[64:128, :], sA[64:128, 0:512]), reads=[sA], writes=[aup])
        load_w(gup, lambda sv: [(gup[:, :], sv[:, 0, :])], W["rwkv_g_up"][l].unsqueeze(1), (1, 512))
        wch = [p.carve([128, 8, 128], BF16, "rw_wch%d" % i) for i in range(3)]
        ur = [[p.carve([128, 520], F32, "rw_ur%d_%d" % (q, i)) for i in range(2)] for q in range(3)]
        dtmp = p.carve([128, 512], F32, "rw_dtmp")

        def proj_lerp(chunk, tb, wbuf, urq, dst_ap, dst_buf, dst_key, act=None):
            un = urq[tb % 2]
            uo = urq[(tb + 1) % 2]
            if tb == 0:
                p.op("gpsimd", lambda e: e.memset(un[:, 0:1], 0.0), writes=[(un, "c")])
            else:
                p.op("gpsimd", lambda e: e.tensor_copy(un[:, 0:1], uo[:, 512:513]), reads=[uo], writes=[(un, "c")])
            pa = nps()
            proj_fm(pa[:, :], pa, wbuf, lambda c: wbuf[:, c, :], tb * 512, 512)
            p.op("scalar", lambda e: e.copy(un[:, 1:513], pa[:, :]), reads=[pa], writes=[(un, "d")])
            p.fence(un)
            p.op("vector", lambda e: e.tensor_tensor(dtmp[:, :], un[:, 0:512], un[:, 1:513], ALU.subtract), reads=[un], writes=[dtmp])
            p.op("vector", lambda e: e.scalar_tensor_tensor(out=dst_ap, in0=dtmp[:, :], scalar=colv(COL_MU + chunk), in1=un[:, 1:513], op0=ALU.mult, op1=ALU.add),
                 reads=[dtmp, un, cols], writes=[(dst_buf, dst_key)])

        lx = p.carve([128, 512], F32, "rw_lx")
        for q, chunk in enumerate((12, 13)):
            load_w(wch[q], lambda sv, q=q: [(wch[q][:, :, :], sv)], win_cols(l, OFF_RW + chunk * 128, 128), (8, 128))
            for tb in range(4):
                bs = slice(tb * 512, (tb + 1) * 512)
                proj_lerp(chunk, tb, wch[q], ur[q], lx[:, :], lx, None)
                if chunk == 12:
                    p.op("scalar", lambda e, bs=bs: e.activation(lora_in[0:64, bs], lx[0:64, :], AF.Tanh), reads=[lx], writes=[(lora_in, ("w", tb))])
                    p.op("vector", lambda e, bs=bs: e.tensor_copy(lora_in[64:128, bs], lx[64:128, :]), reads=[lx], writes=[(lora_in, ("a", tb))])
                else:
                    p.op("scalar", lambda e, bs=bs: e.activation(sgT[:, bs], lx[:, :], AF.Sigmoid), reads=[lx], writes=[(sgT, tb)])
        p.fence(lora_in)
        p.fence(sgT)
        def B_(name):
            return p.carve([128, 512], F32, "rw_" + name)
        rT, kT, vT = B_("rT"), B_("kT"), B_("vT")
        lw, av, kk, kmod, bb, cl, eg, egi, egm, tq, rkr = [B_(n) for n in ("lw", "av", "kk", "kmod", "bb", "cl", "eg", "egi", "egm", "tq", "rkr")]
        KR = p.carve([128, 4, 2, 128], F32, "rw_KR")
        bt, kt = B_("bt"), B_("kt")
        mA = {n: [B_(n + "A"), B_(n + "B")] for n in ("b", "k", "kap", "r")}
        NCI = 4
        M1s = [lw, av, kk, kmod]
        M2s = [bb, cl, egi, egm]
        M3s = [p.carve([128, 256], F32, "rw_M3_%d" % i) for i in range(NCI)]
        PPs = [[tq, tq], [kt, kt], [B_("PP_2")] * 2, [B_("PP_3")] * 2]
        Zbs = [p.carve([128, 2, 128], F32, "rw_Z%d" % i) for i in range(NCI)]
        BKtms = [p.carve([128, 4, 128], F32, "rw_BKtm%d" % i) for i in range(NCI)]
        Vtms = [p.carve([128, 128], F32, "rw_Vtm%d" % i) for i in range(NCI)]
        print("rwkv arena words used", p.aoff, "of", p.awords)
        Ast = p.carve([128, 64], F32, "rw_A")
        Yn = p.carve([128, 128], F32, "rw_Yn")
        Us = p.carve([128, 128], F32, "rw_U")
        t1 = p.carve([128, 64], F32, "rw_t1")
        osb = p.carve([128, 128], F32, "rw_osb")
        oc = p.carve([128, 128], F32, "rw_oc")
        sq = p.carve([128, 128], F32, "rw_sq")
        st4 = p.carve([128, 8], F32, "rw_st4")
        ssb = p.carve([128, 2], F32, "rw_ssb")
        ybf = p.carve([128, 128], BF16, "rw_ybf")
        ysb = [p.carve([128, 128], BF16, "rw_ysb%d" % i) for i in range(2)]
        gne = p.carve([128, 1], F32, "rw_gne")
        p.op("vector", lambda e: e.memset(gne[:, :], 64e-5), writes=[gne])
        msk4 = lambda: None
        v2 = lambda ap: ap.rearrange("p (h d) -> p h d", h=2)
        for hp in range(4):
            for q, chunk in enumerate((hp, 4 + hp, 8 + hp)):
                load_w(wch[q], lambda sv, q=q: [(wch[q][:, :, :], sv)], win_cols(l, OFF_RW + chunk * 128, 128), (8, 128))
            p.op("vector", lambda e: e.memset(Ast[:, :], 0.0), writes=[Ast])
            for tb in range(4):
                bs = slice(tb * 512, (tb + 1) * 512)
                for q, (chunk, dst) in enumerate(((hp, rT), (4 + hp, kT), (8 + hp, vT))):
                    proj_lerp(chunk, tb, wch[q], ur[q], dst[:, :], dst, None)
                pw = nps()
                p.op("tensor", lambda e, pw=pw, bs=bs: e.matmul(pw[:, :], wup[:, hp * 128:(hp + 1) * 128], lora_in[:, bs], start=True, stop=True), reads=[wup, lora_in], writes=[pw])
                p.op("scalar", lambda e, pw=pw: e.activation(lw[:, :], pw[:, :], AF.Sigmoid, bias=colv(COL_W0 + hp)), reads=[pw, cols], writes=[lw])
                p.op("vector", lambda e: e.tensor_scalar(lw[:, :], lw[:, :], -0.6065306597126334, None, ALU.mult), reads=[lw], writes=[lw])
                pa_ = nps()
                p.op("tensor", lambda e, pa_=pa_, bs=bs: e.matmul(pa_[:, :], aup[:, hp * 128:(hp + 1) * 128], lora_in[:, bs], start=True, stop=True), reads=[aup, lora_in], writes=[pa_])
                p.op("scalar", lambda e, pa_=pa_: e.activation(av[:, :], pa_[:, :], AF.Sigmoid, bias=colv(COL_A0 + hp)), reads=[pa_, cols], writes=[av])
                p.op("vector", lambda e: e.tensor_scalar(kk[:, :], kT[:, :], colv(COL_KK + hp), None, ALU.mult), reads=[kT, cols], writes=[kk])
                p.op("gpsimd", lambda e: e.tensor_tensor(tq[:, :], kk[:, :], kk[:, :], ALU.mult), reads=[kk], writes=[tq])
                pss = nps()
                p.op("tensor", lambda e, pss=pss: e.matmul(pss[:, :], bones, tq[:, :], start=True, stop=True), reads=[crw, tq], writes=[pss])
                p.op("scalar", lambda e, pss=pss: e.activation(tq[:, :], pss[:, :], AF.Sqrt), reads=[pss], writes=[tq])
                p.op("vector", lambda e: e.tensor_scalar(tq[:, :], tq[:, :], 1e-12, None, ALU.max), reads=[tq], writes=[tq])
                p.op("vector", lambda e: e.reciprocal(tq[:, :], tq[:, :]), reads=[tq], writes=[tq])
                p.op("vector", lambda e: e.tensor_tensor(kk[:, :], kk[:, :], tq[:, :], ALU.mult), reads=[kk, tq], writes=[kk])
                p.op("gpsimd", lambda e: e.tensor_scalar(tq[:, :], av[:, :], -1.0, colv(COL_KA + hp), ALU.add, ALU.mult), reads=[av, cols, tq], writes=[tq])
                p.op("vector", lambda e: e.scalar_tensor_tensor(out=kmod[:, :], in0=tq[:, :], scalar=1.0, in1=kT[:, :], op0=ALU.add, op1=ALU.mult), reads=[tq, kT], writes=[kmod])
                p.op("gpsimd", lambda e: e.tensor_tensor(bb[:, :], kk[:, :], av[:, :], ALU.mult), reads=[kk, av], writes=[bb])
                p.op("vector", lambda e: e.scalar_tensor_tensor(out=rkr[:, :], in0=rT[:, :], scalar=colv(COL_RK + hp), in1=kmod[:, :], op0=ALU.mult, op1=ALU.mult), reads=[rT, kmod, cols], writes=[rkr])
                p.op("vector", lambda e: e.tensor_tensor_scan(cl[:, :], cmask, lw[:, :], 0.0, ALU.mult, ALU.add), reads=[crw, lw], writes=[cl])
                p.op("scalar", lambda e: e.activation(eg[:, :], cl[:, :], AF.Exp), reads=[cl], writes=[eg])
                p.op("scalar", lambda e: e.activation(egi[:, :], cl[:, :], AF.Exp, scale=-1.0), reads=[cl], writes=[egi])
                p.op("gpsimd", lambda e: e.tensor_tensor(egm[:, :], cl[:, :], lw[:, :], ALU.subtract), reads=[cl, lw], writes=[egm])
                p.op("scalar", lambda e: e.activation(egm[:, :], egm[:, :], AF.Exp), reads=[egm], writes=[egm])
                v4 = lambda ap: ap.rearrange("p (c t) -> p c t", c=4)
                p.op("vector", lambda e: e.tensor_tensor(KR[:, :, 0, :], v4(kk[:, :]), v4(egm[:, :]), ALU.mult), reads=[kk, egm], writes=[(KR, 0)])
                p.op("vector", lambda e: e.tensor_tensor(KR[:, :, 1, :], v4(rT[:, :]), v4(eg[:, :]), ALU.mult), reads=[rT, eg], writes=[(KR, 1)])
                p.fence(KR)
                p.op("gpsimd", lambda e: e.tensor_tensor(bt[:, :], bb[:, :], egi[:, :], ALU.mult), reads=[bb, egi], writes=[bt])
                p.op("gpsimd", lambda e: e.tensor_tensor(kt[:, :], kmod[:, :], egi[:, :], ALU.mult), reads=[kmod, egi], writes=[kt])
                for X in range(2):
                    hcol = crw[:, 128 + X:129 + X]
                    p.op("vector", lambda e, X=X, hcol=hcol: e.tensor_scalar(mA["b"][X][:, :], bt[:, :], hcol, None, ALU.mult), reads=[bt, crw], writes=[mA["b"][X]])
                    p.op("scalar", lambda e, X=X, hcol=hcol: e.activation(mA["k"][X][:, :], kt[:, :], AF.Copy, scale=hcol), reads=[kt, crw], writes=[mA["k"][X]])
                    p.op("vector", lambda e, X=X, hcol=hcol: e.tensor_scalar(v4(mA["kap"][X][:, :]), KR[:, :, 0, :], hcol, None, ALU.mult), reads=[KR, crw], writes=[mA["kap"][X]])
                    p.op("scalar", lambda e, X=X, hcol=hcol: e.activation(v4(mA["r"][X][:, :]), KR[:, :, 1, :], AF.Copy, scale=hcol), reads=[KR, crw], writes=[mA["r"][X]])
                for cc in range(4):
                    c = tb * 4 + cc
                    cs = slice(cc * 128, (cc + 1) * 128)
                    M1, M2, M3, Zb, BKtm, Vtm = M1s[cc], M2s[cc], M3s[cc], Zbs[cc], BKtms[cc], Vtms[cc]
                    ts_ = slice(c * 128, (c + 1) * 128)
                    b1, b2, b3 = nps(), nps(), nps()
                    krv = KR[:, cc, :, :].rearrange("p a t -> p (a t)")
                    for X in range(2):
                        p.op("tensor", lambda e, X=X: e.matmul(b1[:, X * 256:(X + 1) * 256], mA["b"][X][:, cs], krv, start=True, stop=True), reads=[mA["b"][X], KR], writes=[b1])
                        p.op("tensor", lambda e, X=X: e.matmul(b2[:, X * 256:(X + 1) * 256], mA["k"][X][:, cs], krv, start=True, stop=True), reads=[mA["k"][X], KR], writes=[b2])
                        p.op("tensor", lambda e, X=X: e.matmul(b3[:, X * 128:(X + 1) * 128], mA["kap"][X][:, cs], bt[:, cs], start=True, stop=True), reads=[mA["kap"][X], bt], writes=[b3])
                    msi = lambda ap: ap.rearrange("p (x m t) -> p x m t", x=2, m=2)
                    for m_, cidx in ((0, 4), (1, 0)):
                        mk_ = cssd[:, cidx, :].unsqueeze(1).to_broadcast([128, 2, 128])
                        p.op("vector", lambda e, m_=m_, mk_=mk_: e.tensor_tensor(msi(M1[:, :])[:, :, m_, :], msi(b1[:, :])[:, :, m_, :], mk_, ALU.mult), reads=[b1, cssd], writes=[(M1, m_)])
                        p.op("vector", lambda e, m_=m_, mk_=mk_: e.tensor_tensor(msi(M2[:, :])[:, :, m_, :], msi(b2[:, :])[:, :, m_, :], mk_, ALU.mult), reads=[b2, cssd], writes=[(M2, m_)])
                    p.fence(M1)
                    p.fence(M2)
                    p.op("vector", lambda e: e.tensor_tensor(v2(M3[:, :]), v2(b3[:, 0:256]), cssd[:, 1, :].unsqueeze(1).to_broadcast([128, 2, 128]), ALU.mult), reads=[b3, cssd], writes=[M3])
                    lbt = lambda X: M1[:, X * 256:X * 256 + 128]
                    p.op("gpsimd", lambda e: e.tensor_tensor(Zb[:, :, :], identf[:, :].unsqueeze(1).to_broadcast([128, 2, 128]), msi(M1[:, :])[:, :, 0, :], ALU.subtract), reads=[identf, M1], writes=[Zb])
                    ptr = nps()
                    for ti, src in enumerate((mA["b"][0], mA["b"][1], mA["k"][0], mA["k"][1])):
                        p.op("tensor", lambda e, ti=ti, src=src: e.transpose(ptr[:, ti * 128:(ti + 1) * 128], src[:, cs], identf[:, :]), reads=[src, identf], writes=[ptr])
                    p.op("scalar", lambda e, ptr=ptr: e.copy(BKtm[:, :, :], ptr[:, :].rearrange("p (a t) -> p a t", a=4)), reads=[ptr], writes=[BKtm])
                    pv_ = nps()
                    p.op("tensor", lambda e, pv_=pv_: e.transpose(pv_[:, 0:128], vT[:, cs], identf[:, :]), reads=[vT, identf], writes=[pv_])
                    p.op("vector", lambda e, pv_=pv_: e.tensor_copy(Vtm[:, :], pv_[:, 0:128]), reads=[pv_], writes=[Vtm])
                Pn_ = [[M3s[cc][:, 0:128], M3s[cc][:, 128:256]] for cc in range(4)]
                Pt_ = [[M1s[cc][:, 0:128], M1s[cc][:, 256:384]] for cc in range(4)]
                Pb_ = [[M3s[cc], M1s[cc]] for cc in range(4)]
                for lev in range(1, 7):
                    for cc in range(4):
                        Zb = Zbs[cc]
                        Pn, Pt = Pn_[cc], Pt_[cc]
                        pbn, pbt = Pb_[cc]
                        pq = nps()
                        for X in range(2):
                            p.op("tensor", lambda e, X=X: e.matmul(pq[:, X * 256:X * 256 + 128], Pt[X], Pn[X], start=True, stop=True), reads=[pbn, pbt], writes=[pq])
                            p.op("tensor", lambda e, X=X: e.matmul(pq[:, X * 256 + 128:X * 256 + 256], Pn[X], Pt[X], start=True, stop=True), reads=[pbn, pbt], writes=[pq])
                        pp_ = PPs[cc][lev % 2]
                        if cc % 2 == 0:
                            p.op("scalar", lambda e: e.copy(pp_[:, :], pq[:, :]), reads=[pq], writes=[pp_])
                        else:
                            p.op("vector", lambda e: e.tensor_copy(pp_[:, :], pq[:, :]), reads=[pq], writes=[pp_])
                        Pn_[cc] = [pp_[:, 0:128], pp_[:, 256:384]]
                        Pt_[cc] = [pp_[:, 128:256], pp_[:, 384:512]]
                        Pb_[cc] = [pp_, pp_]
                        Pn = Pn_[cc]
                        pz = nps()
                        for X in range(2):
                            p.op("tensor", lambda e, X=X: e.matmul(pz[:, X * 128:(X + 1) * 128], Pn[X], Zb[:, X, :], start=True, stop=True), reads=[pp_, Zb], writes=[pz])
                        p.op("vector", lambda e: e.tensor_tensor(Zb[:, :, :], Zb[:, :, :], v2(pz[:, 0:256]), ALU.add), reads=[pz, Zb], writes=[Zb])
                def seq_part(cc):
                    c = tb * 4 + cc
                    cs = slice(cc * 128, (cc + 1) * 128)
                    ts_ = slice(c * 128, (c + 1) * 128)
                    M1, M2, M3, Zb, BKtm, Vtm = M1s[cc], M2s[cc], M3s[cc], Zbs[cc], BKtms[cc], Vtms[cc]
                    py = nps()
                    for X in range(2):
                        p.op("tensor", lambda e, X=X, py=py: e.matmul(py[:, X * 64:(X + 1) * 64], mA["kap"][X][:, cs], Ast[:, :], start=True, stop=False), reads=[mA["kap"][X], Ast], writes=[py])
                        p.op("tensor", lambda e, X=X, py=py: e.matmul(py[:, X * 64:(X + 1) * 64], M2[:, X * 256:X * 256 + 128], Vtm[:, X * 64:(X + 1) * 64], start=False, stop=True), reads=[M2, Vtm], writes=[py])
                    p.op("vector", lambda e, py=py: e.tensor_scalar(Yn[:, :], py[:, 0:128], -1.0, None, ALU.mult), reads=[py], writes=[Yn])
                    pu = nps()
                    for X in range(2):
                        p.op("tensor", lambda e, X=X, pu=pu: e.matmul(pu[:, X * 64:(X + 1) * 64], Zb[:, X, :], Yn[:, X * 64:(X + 1) * 64], start=True, stop=True), reads=[Zb, Yn], writes=[pu])
                    p.op("vector", lambda e, pu=pu: e.tensor_copy(Us[:, :], pu[:, 0:128]), reads=[pu], writes=[Us])
                    po = npl()
                    for X in range(2):
                        p.op("tensor", lambda e, X=X, po=po: e.matmul(po[:, X * 64:(X + 1) * 64], mA["r"][X][:, cs], Ast[:, :], start=True, stop=False), reads=[mA["r"][X], Ast], writes=[po])
                        p.op("tensor", lambda e, X=X, po=po: e.matmul(po[:, X * 64:(X + 1) * 64], M1[:, X * 256 + 128:X * 256 + 256], Us[:, X * 64:(X + 1) * 64], start=False, stop=False), reads=[M1, Us], writes=[po])
                        p.op("tensor", lambda e, X=X, po=po: e.matmul(po[:, X * 64:(X + 1) * 64], M2[:, X * 256 + 128:X * 256 + 256], Vtm[:, X * 64:(X + 1) * 64], start=False, stop=True), reads=[M2, Vtm], writes=[po])
                    pi_ = nps()
                    seq = [(0, Us, 0), (1, Us, 1), (2, Vtm, 0), (3, Vtm, 1)]
                    for si, (ti, rb, X) in enumerate(seq):
                        p.op("tensor", lambda e, si=si, ti=ti, rb=rb, X=X, pi_=pi_: e.matmul(pi_[:, 0:64], BKtm[:, ti, :], rb[:, X * 64:(X + 1) * 64], start=(si == 0), stop=(si == 3)), reads=[BKtm, rb], writes=[pi_])
                    gC = eg[:, cc * 128 + 127:cc * 128 + 128]
                    p.op("vector", lambda e, pi_=pi_, gC=gC: e.tensor_scalar(t1[:, :], pi_[:, 0:64], gC, None, ALU.mult), reads=[pi_, eg], writes=[t1])
                    p.op("vector", lambda e, gC=gC: e.scalar_tensor_tensor(out=Ast[:, :], in0=Ast[:, :], scalar=gC, in1=t1[:, :], op0=ALU.mult, op1=ALU.add), reads=[t1, eg, Ast], writes=[Ast])
                    return po

                def epi_part(cc, po):
                    c = tb * 4 + cc
                    cs = slice(cc * 128, (cc + 1) * 128)
                    ts_ = slice(c * 128, (c + 1) * 128)
                    M1, M2, M3, Zb, BKtm, Vtm = M1s[cc], M2s[cc], M3s[cc], Zbs[cc], BKtms[cc], Vtms[cc]
                    p.op("scalar", lambda e, po=po: e.copy(osb[:, :], po[:, 0:128]), reads=[po], writes=[osb])
                    p.op("vector", lambda e: e.tensor_reduce(out=st4[:, 0:2], in_=v2(osb[:, :]), axis=AX.X, op=ALU.add), reads=[osb], writes=[(st4, 0)])
                    p.op("vector", lambda e: e.tensor_scalar(st4[:, 2:4], st4[:, 0:2], -1.0 / 64, None, ALU.mult), reads=[(st4, 0)], writes=[(st4, 1)])
                    p.op("vector", lambda e: e.tensor_tensor(v2(oc[:, :]), v2(osb[:, :]), st4[:, 2:4].unsqueeze(2).to_broadcast([128, 2, 64]), ALU.add), reads=[osb, (st4, 1)], writes=[oc])
                    p.op("gpsimd", lambda e: e.tensor_tensor(sq[:, :], oc[:, :], oc[:, :], ALU.mult), reads=[oc], writes=[sq])
                    p.op("vector", lambda e: e.tensor_reduce(out=st4[:, 4:6], in_=v2(sq[:, :]), axis=AX.X, op=ALU.add), reads=[sq], writes=[(st4, 2)])
                    p.op("scalar", lambda e: e.activation(st4[:, 6:8], st4[:, 4:6], AF.Sqrt, bias=gne[:, 0:1], scale=1.0 / 64), reads=[(st4, 2), gne], writes=[(st4, 3)])
                    p.op("vector", lambda e: e.reciprocal(st4[:, 6:8], st4[:, 6:8]), reads=[(st4, 3)], writes=[(st4, 3)])
                    p.op("vector", lambda e: e.tensor_tensor(v2(oc[:, :]), v2(oc[:, :]), st4[:, 6:8].unsqueeze(2).to_broadcast([128, 2, 64]), ALU.mult), reads=[oc, (st4, 3)], writes=[oc])
                    p.op("gpsimd", lambda e: e.tensor_tensor(oc[:, :], oc[:, :], rows[:, ro + ROW_LNW + hp * 128:ro + ROW_LNW + (hp + 1) * 128], ALU.mult), reads=[oc, rows], writes=[oc])
                    p.op("gpsimd", lambda e: e.tensor_tensor(oc[:, :], oc[:, :], rows[:, ro + ROW_LNB + hp * 128:ro + ROW_LNB + (hp + 1) * 128], ALU.add), reads=[oc, rows], writes=[oc])
                    pb_ = nps()
                    p.op("tensor", lambda e, pb_=pb_: e.matmul(pb_[:, 0:2], rkr[:, cs], hsel, start=True, stop=True), reads=[rkr, crw], writes=[pb_])
                    p.op("vector", lambda e, pb_=pb_: e.tensor_copy(ssb[:, :], pb_[:, 0:2]), reads=[pb_], writes=[ssb])
                    p.op("vector", lambda e: e.tensor_tensor(v2(sq[:, :]), v2(Vtm[:, :]), ssb[:, :].unsqueeze(2).to_broadcast([128, 2, 64]), ALU.mult), reads=[Vtm, ssb, sq], writes=[sq])
                    p.op("gpsimd", lambda e: e.tensor_tensor(oc[:, :], oc[:, :], sq[:, :], ALU.add), reads=[oc, sq], writes=[oc])
                    pg_ = nps()
                    p.op("tensor", lambda e, pg_=pg_, ts_=ts_: e.matmul(pg_[:, 0:128], sgT[:, ts_], gup[:, hp * 128:(hp + 1) * 128], start=True, stop=True), reads=[sgT, gup], writes=[pg_])
                    p.op("vector", lambda e, pg_=pg_: e.tensor_tensor(ybf[:, :], oc[:, :], pg_[:, 0:128], ALU.mult), reads=[oc, pg_], writes=[ybf])
                    pt = npst()
                    p.op("tensor", lambda e, pt=pt: e.transpose(pt[:, 0:128], ybf[:, :], identb[:]), reads=[ybf, identb], writes=[pt])
                    ys_ = ysb[c % 2]
                    p.op("scalar", lambda e, pt=pt, ys_=ys_: e.copy(ys_[:, :], pt[:, 0:128]), reads=[pt], writes=[ys_])
                    p.dma(yT_d[2][hp, :, ts_], ys_[:, :], reads=[ys_], writes=[(yT_d[2], (hp, c))])
                pos_ = {}
                for step in range(5):
                    if step < 4:
                        pos_[step] = seq_part(step)
                    if step >= 1:
                        epi_part(step - 1, pos_[step - 1])
        if l == 0:
            for nm, bf in (("rT", rT), ("kT", kT), ("vT", vT), ("lw", lw), ("av", av), ("kk", kk), ("kmod", kmod), ("cl", cl), ("rkr", rkr)):
                dump("rw_" + nm, bf[:, :], [128, 512], F32, [bf])
            dump("rw_osb", osb[:, :], [128, 128], F32, [osb])
            dump("rw_oc", oc[:, :], [128, 128], F32, [oc])
            dump("rw_Us", Us[:, :], [128, 128], F32, [Us])
            dump("rw_A", Ast[:, :], [128, 64], F32, [Ast])
        p.fence(yT_d[2])
        p.release(mk)

    def mixer_ssd(l):
        mk = p.mark()
        ro = 0
        co = l * COL_PER_LAYER
        xbcT = p.carve([128, 8, T], BF16, "xbcT")
        Xtm = p.carve([128, NT, 512], BF16, "Xtm")
        Btm = p.carve([128, NT, 256], BF16, "Btm")
        dt_all = p.carve([128, NT, 8], F32, "dt_all")
        a_all = p.carve([128, NT, 8], F32, "a_all")
        acum = p.carve([128, NT, 8], F32, "acum")
        tot = p.carve([128, NT, 8], F32, "tot")
        ea = p.carve([128, NT, 8], F32, "ea")
        dts = p.carve([128, NT, 8], F32, "dts")
        cd = p.carve([128, NT, 8], F32, "cd")
        Arow = p.carve([128, 8], F32, "Arow")
        mk2 = p.mark()
        xr = [p.carve([128, T + 8], F32, "xr%d" % i) for i in range(2)]
        cacc = [p.carve([128, T], F32, "cacc%d" % i) for i in range(2)]
        wx = [p.carve([128, 8, 128], BF16, "wx%d" % i) for i in range(2)]
        for ch in range(8):
            xr_, ca_, wx_ = xr[ch % 2], cacc[ch % 2], wx[ch % 2]
            load_w(wx_, lambda sv, wx_=wx_: [(wx_[:, :, :], sv)], win_cols(l, OFF_XBC + ch * 128, 128), (8, 128))
            p.op("vector", lambda e, xr_=xr_: e.memset(xr_[:, 0:4], 0.0), writes=[(xr_, "z")])
            for b in range(4):
                pa = nps()
                proj_fm(pa[:, :], pa, wx_, lambda c, wx_=wx_: wx_[:, c, :], b * 512, 512)
                p.op("scalar", lambda e, xr_=xr_, pa=pa, b=b: e.copy(xr_[:, 4 + b * 512:4 + (b + 1) * 512], pa[:, :]), reads=[pa], writes=[(xr_, b)])
            p.fence(xr_)
            for k in range(4):
                wcol = cols[:, co + COL_CONVW + ch * 4 + k:co + COL_CONVW + ch * 4 + k + 1]
                if k == 0:
                    p.op("vector", lambda e, ca_=ca_, xr_=xr_, wcol=wcol: e.tensor_scalar(ca_[:, :], xr_[:, 1:1 + T], wcol, None, ALU.mult),
                         reads=[xr_, cols], writes=[ca_])
                else:
                    p.op("vector", lambda e, ca_=ca_, xr_=xr_, wcol=wcol, k=k: e.scalar_tensor_tensor(out=ca_[:, :], in0=xr_[:, 1 + k:1 + k + T], scalar=wcol, in1=ca_[:, :], op0=ALU.mult, op1=ALU.add),
                         reads=[xr_, cols, ca_], writes=[ca_])
            p.op("scalar", lambda e, ca_=ca_, ch=ch: e.activation(xbcT[:, ch, :], ca_[:, :], AF.Silu, bias=cols[:, co + COL_CONVB + ch:co + COL_CONVB + ch + 1]),
                 reads=[ca_, cols], writes=[(xbcT, ch)])
        p.fence(xbcT)
        p.release(mk2)
        import os as _os
        _stop = int(_os.environ.get("SSD_STOP", "99"))
        if _stop <= 1:
            p.release(mk)
            return
        for i in range(NT):
            pt = npst()
            for c in range(6):
                p.op("tensor", lambda e, pt=pt, c=c, i=i: e.transpose(pt[:, c * 128:(c + 1) * 128], xbcT[:, c, i * 128:(i + 1) * 128], identb[:]),
                     reads=[xbcT, identb], writes=[pt])
            p.op("vector", lambda e, pt=pt, i=i: e.tensor_copy(Xtm[:, i, :], pt[:, 0:512]), reads=[pt], writes=[(Xtm, i)])
            p.op("vector", lambda e, pt=pt, i=i: e.tensor_copy(Btm[:, i, :], pt[:, 512:768]), reads=[pt], writes=[(Btm, i)])
        p.fence(Xtm)
        p.fence(Btm)
        if _stop <= 2:
            p.release(mk)
            return
        wdt = p.carve([128, 8, 8], BF16, "wdt")
        load_w(wdt, lambda sv: [(wdt[:, :, :], sv)], win_cols(l, OFF_DT, 8), (8, 8))
        pd = nps()
        for i in range(NT):
            proj_tm(pd[:, i * 8:(i + 1) * 8], pd, wdt, lambda c: wdt[:, c, :], i)
        b3 = lambda r0: rows[:, ro + r0:ro + r0 + 8].unsqueeze(1).to_broadcast([128, NT, 8])
        p.op("vector", lambda e: e.tensor_tensor(dt_all[:, :, :], pd[:, 0:128].rearrange("p (i h) -> p i h", h=8), b3(ROW_DTB), ALU.add),
             reads=[pd, rows], writes=[dt_all])
        p.op("scalar", lambda e: e.activation(dt_all[:, :, :], dt_all[:, :, :], AF.Exp), reads=[dt_all], writes=[dt_all])
        p.op("scalar", lambda e: e.activation(dt_all[:, :, :], dt_all[:, :, :], AF.Ln, bias=cssd[:, 3, 0:1]), reads=[dt_all, cssd], writes=[dt_all])
        p.op("scalar", lambda e: e.activation(Arow[:, :], rows[:, ro + ROW_ALOG:ro + ROW_ALOG + 8], AF.Exp), reads=[rows], writes=[Arow])
        p.op("vector", lambda e: e.tensor_scalar(Arow[:, :], Arow[:, :], -1.0, None, ALU.mult), reads=[Arow], writes=[Arow])
        p.op("vector", lambda e: e.tensor_tensor(a_all[:, :, :], dt_all[:, :, :], Arow[:, :].unsqueeze(1).to_broadcast([128, NT, 8]), ALU.mult),
             reads=[dt_all, Arow], writes=[a_all])
        pc = nps()
        pt_ = nps()
        for i in range(NT):
            p.op("tensor", lambda e, i=i: e.matmul(pc[:, i * 8:(i + 1) * 8], cssd[:, 0, :], a_all[:, i, :], start=True, stop=True), reads=[cssd, a_all], writes=[pc])
            p.op("tensor", lambda e, i=i: e.matmul(pt_[:, i * 8:(i + 1) * 8], cssd[:, 3, :], a_all[:, i, :], start=True, stop=True), reads=[cssd, a_all], writes=[pt_])
        v3 = lambda ps_: ps_[:, 0:128].rearrange("p (i h) -> p i h", h=8)
        p.op("vector", lambda e: e.tensor_copy(acum[:, :, :], v3(pc)), reads=[pc], writes=[acum])
        p.op("vector", lambda e: e.tensor_copy(tot[:, :, :], v3(pt_)), reads=[pt_], writes=[tot])
        p.op("scalar", lambda e: e.activation(ea[:, :, :], acum[:, :, :], AF.Exp), reads=[acum], writes=[ea])
        p.op("scalar", lambda e: e.activation(cd[:, :, :], tot[:, :, :], AF.Exp), reads=[tot], writes=[cd])
        p.op("vector", lambda e: e.tensor_tensor(dts[:, :, :], tot[:, :, :], acum[:, :, :], ALU.subtract), reads=[tot, acum], writes=[dts])
        p.op("scalar", lambda e: e.activation(dts[:, :, :], dts[:, :, :], AF.Exp), reads=[dts], writes=[dts])
        p.op("vector", lambda e: e.tensor_tensor(dts[:, :, :], dts[:, :, :], dt_all[:, :, :], ALU.mult), reads=[dts, dt_all], writes=[dts])
        if _stop <= 3:
            p.release(mk)
            return
        if l == 0 and _stop == 98:
            dump("ssd_Xtm", Xtm[:, :, :], [128, NT, 512], BF16, [Xtm])
            dump("ssd_Btm", Btm[:, :, :], [128, NT, 256], BF16, [Btm])
            dump("ssd_dt", dt_all[:, :, :], [128, NT, 8], F32, [dt_all])
            dump("ssd_acum", acum[:, :, :], [128, NT, 8], F32, [acum])
            dump("ssd_tot", tot[:, :, :], [128, NT, 8], F32, [tot])
            dump("ssd_xbcT", xbcT[:, :, :], [128, 8, T], BF16, [xbcT])
        wz = p.carve([128, 8, 512], BF16, "wz")
        load_w_plain(wz, wz, win_cols(l, OFF_Z, 512), (8, 512))
        M1 = [p.carve([128, 128], F32, "M1_%d" % i) for i in range(4)]
        Eb = [p.carve([128, 512], F32, "Eb%d" % i) for i in range(2)]
        CBs = [p.carve([128, 128], F32, "CBs%d" % i) for i in range(2)]
        Wt = [p.carve([128, 512], BF16, "Wt%d" % i) for i in range(2)]
        Xdt = [p.carve([128, 512], BF16, "Xdt%d" % i) for i in range(2)]
        Xds = [p.carve([128, 512], BF16, "Xds%d" % i) for i in range(2)]
        prev = p.carve([128, 512], F32, "prev")
        prevb = [p.carve([128, 512], BF16, "prevb%d" % i) for i in range(2)]
        ptmp = p.carve([128, 512], F32, "ptmp")
        y1 = [p.carve([128, 512], F32, "y1_%d" % i) for i in range(2)]
        y2 = [p.carve([128, 512], F32, "y2_%d" % i) for i in range(2)]
        sz = [p.carve([128, 512], F32, "sz%d" % i) for i in range(2)]
        ss = [p.carve([128, 4], F32, "ss%d" % i) for i in range(2)]
        junk = p.carve([128, 256], F32, "sjunk")
        yn = [p.carve([128, 512], BF16, "yn%d" % i) for i in range(2)]
        yst = [p.carve([128, 4, 128], BF16, "yst%d" % i) for i in range(2)]
        p.op("vector", lambda e: e.memset(prev[:, :], 0.0), writes=[prev])
        p.op("vector", lambda e: e.memset(prevb[0][:, :], 0.0), writes=[prevb[0]])
        bc8 = lambda ap8: ap8.unsqueeze(2).to_broadcast([128, 8, 64])
        v8 = lambda ap: ap.rearrange("p (h d) -> p h d", d=64)
        Eb2 = [Eb, [p.carve([128, 512], F32, "Eb2_%d" % i) for i in range(2)]]
        CBs2 = [CBs, [p.carve([128, 128], F32, "CBs2_%d" % i) for i in range(2)]]
        Wt2 = [Wt, [p.carve([128, 512], BF16, "Wt2_%d" % i) for i in range(2)]]

        def stage1(c):
            k = c % 2
            tsl = slice(c * 128, (c + 1) * 128)
            p.op("vector", lambda e: e.tensor_tensor(v8(Xdt[k][:, :]), v8(Xtm[:, c, :]), bc8(dt_all[:, c, :]), ALU.mult),
                 reads=[Xtm, dt_all], writes=[Xdt[k]])
            p.op("gpsimd", lambda e: e.tensor_tensor(v8(Xds[k][:, :]), v8(Xtm[:, c, :]), bc8(dts[:, c, :]), ALU.mult),
                 reads=[Xtm, dts], writes=[Xds[k]])
            for g in range(2):
                pcb = nps()
                p.op("tensor", lambda e: e.matmul(pcb[:, 0:128], xbcT[:, 4 + g, tsl], xbcT[:, 6 + g, tsl], start=True, stop=True),
                     reads=[xbcT], writes=[pcb])
                cb_ = CBs2[k][g]
                p.op("scalar", lambda e: e.copy(cb_[:, :], pcb[:, 0:128]), reads=[pcb], writes=[cb_])
                pseg = nps()
                for hh in range(4):
                    h = g * 4 + hh
                    m1 = M1[hh]
                    p.op("vector", lambda e: e.tensor_scalar(m1[:, :], cssd[:, 1, :], a_all[:, c, h:h + 1], None, ALU.mult),
                         reads=[cssd, a_all], writes=[m1])
                    p.op("tensor", lambda e: e.matmul(pseg[:, hh * 128:(hh + 1) * 128], m1[:, :], cssd[:, 0, :], start=True, stop=False),
                         reads=[m1, cssd], writes=[pseg])
                    p.op("tensor", lambda e: e.matmul(pseg[:, hh * 128:(hh + 1) * 128], identf[:, :], cssd[:, 2, :], start=False, stop=True),
                         reads=[identf, cssd], writes=[pseg])
                eb = Eb2[k][g]
                p.op("scalar", lambda e: e.activation(eb[:, :], pseg[:, :], AF.Exp), reads=[pseg], writes=[eb])
                wt = Wt2[k][g]
                p.op("vector", lambda e: e.tensor_tensor(wt[:, :].rearrange("p (h l) -> p h l", h=4), eb[:, :].rearrange("p (h l) -> p h l", h=4),
                                                         cb_[:, :].unsqueeze(1).to_broadcast([128, 4, 128]), ALU.mult),
                     reads=[eb, cb_], writes=[wt])

        pending_tail = []
        stage1(0)
        for c in range(NT):
            k = c % 2
            pb_c = prevb[c % 2]
            pb_n = prevb[(c + 1) % 2]
            tsl = slice(c * 128, (c + 1) * 128)
            if c + 1 < NT:
                stage1(c + 1)
            pyd = psb[4]
            for g in range(2):
                wt = Wt2[k][g]
                for hh in range(4):
                    h = g * 4 + hh
                    p.op("tensor", lambda e: e.matmul(pyd[:, h * 64:(h + 1) * 64], wt[:, hh * 128:(hh + 1) * 128], Xdt[k][:, h * 64:(h + 1) * 64], start=True, stop=True),
                         reads=[wt, Xdt[k]], writes=[pyd])
            pst_ = nps()
            pyo = psb[5]
            for g in range(2):
                p.op("tensor", lambda e, g=g, c=c, k=k: e.matmul(pst_[:, g * 256:(g + 1) * 256], Btm[:, c, g * 128:(g + 1) * 128], Xds[k][:, g * 256:(g + 1) * 256], start=True, stop=True),
                     reads=[Btm, Xds[k]], writes=[pst_])
                p.op("tensor", lambda e, g=g, tsl=tsl, pb_c=pb_c: e.matmul(pyo[:, g * 256:(g + 1) * 256], xbcT[:, 6 + g, tsl], pb_c[:, g * 256:(g + 1) * 256], start=True, stop=True),
                     reads=[xbcT, pb_c], writes=[pyo])
            y1_ = y1[k]
            y2_ = y2[k]
            p.op("vector", lambda e, y1_=y1_, c=c: e.tensor_tensor(v8(y1_[:, :]), v8(pyo[:, :]), bc8(ea[:, c, :]), ALU.mult), reads=[pyo, ea], writes=[y1_])
            p.op("vector", lambda e, y1_=y1_: e.tensor_tensor(y1_[:, :], y1_[:, :], pyd[:, :], ALU.add), reads=[pyd, y1_], writes=[y1_])
            p.op("gpsimd", lambda e, y2_=y2_, c=c: e.tensor_tensor(v8(y2_[:, :]), v8(Xtm[:, c, :]), bc8(rows[:, ro + ROW_DSK:ro + ROW_DSK + 8]), ALU.mult),
                 reads=[Xtm, rows], writes=[y2_])
            p.op("gpsimd", lambda e, y2_=y2_, y1_=y1_: e.tensor_tensor(y2_[:, :], y2_[:, :], y1_[:, :], ALU.add), reads=[y1_, y2_], writes=[y2_])
            if _stop <= 4 + c:
                break
            if l == 0 and c in (0, 1) and _stop == 98:
                dump("ssd_y2_%d" % c, y2_[:, :], [128, 512], F32, [y2_])
                dump("ssd_y1_%d" % c, y1_[:, :], [128, 512], F32, [y1_])
            if c < NT - 1:
                p.op("vector", lambda e, c=c: e.tensor_tensor(v8(ptmp[:, :]), v8(prev[:, :]), bc8(cd[:, c, :]), ALU.mult), reads=[prev, cd], writes=[ptmp])
                p.op("vector", lambda e: e.tensor_tensor(prev[:, :], ptmp[:, :], pst_[:, :], ALU.add), reads=[ptmp, pst_], writes=[prev])
                p.op("scalar", lambda e, pb_n=pb_n: e.copy(pb_n[:, :], prev[:, :]), reads=[prev], writes=[pb_n])
            pz = nps()
            proj_tm(pz[:, :], pz, wz, lambda cc: wz[:, cc, :], c)
            while pending_tail:
                pending_tail.pop(0)()
            sz_ = sz[k]
            ss_ = ss[k]
            p.op("scalar", lambda e, sz_=sz_, pz=pz: e.activation(sz_[:, :], pz[:, :], AF.Silu), reads=[pz], writes=[sz_])
            p.op("vector", lambda e, sz_=sz_, y2_=y2_: e.tensor_tensor(sz_[:, :], sz_[:, :], y2_[:, :], ALU.mult), reads=[y2_, sz_], writes=[sz_])
            for g in range(2):
                p.op("scalar", lambda e, sz_=sz_, ss_=ss_, g=g: e.activation(junk[:, :], sz_[:, g * 256:(g + 1) * 256], AF.Square, accum_out=ss_[:, g:g + 1]),
                     reads=[sz_], writes=[junk, (ss_, g)])
            p.fence(ss_)
            p.op("scalar", lambda e, ss_=ss_: e.activation(ss_[:, 2:4], ss_[:, 0:2], AF.Sqrt, bias=epsc[:, 0:1], scale=1.0 / 256), reads=[ss_, epsc], writes=[ss_])
            p.op("vector", lambda e, ss_=ss_: e.reciprocal(ss_[:, 2:4], ss_[:, 2:4]), reads=[ss_], writes=[ss_])
            yn_ = yn[k]
            for g in range(2):
                p.op("vector", lambda e, yn_=yn_, sz_=sz_, ss_=ss_, g=g: e.scalar_tensor_tensor(
                    out=yn_[:, g * 256:(g + 1) * 256], in0=sz_[:, g * 256:(g + 1) * 256], scalar=ss_[:, 2 + g:3 + g],
                    in1=rows[:, ro + ROW_SSDN + g * 256:ro + ROW_SSDN + (g + 1) * 256], op0=ALU.mult, op1=ALU.mult),
                     reads=[sz_, ss_, rows], writes=[(yn_, g)])
            p.fence(yn_)
            def tail(c=c, k=k, yn_=yn_):
                pt = npst()
                for cc in range(4):
                    p.op("tensor", lambda e: e.transpose(pt[:, cc * 128:(cc + 1) * 128], yn_[:, cc * 128:(cc + 1) * 128], identb[:]),
                         reads=[yn_, identb], writes=[pt])
                ys_ = yst[k]
                p.op("scalar", lambda e: e.copy(ys_[:, :, :], pt[:, 0:512].rearrange("p (c t) -> p c t", c=4)), reads=[pt], writes=[ys_])
                p.dma(yT_d[0][:, :, c * 128:(c + 1) * 128].rearrange("c p t -> p c t"), ys_[:, :, :], reads=[ys_], writes=[(yT_d[0], c)])
            pending_tail.append(tail)
        for t_ in pending_tail:
            t_()
        p.fence(yT_d[0])
        p.release(mk)

    def phase_merge(l, active):
        mk = p.mark()
        mergedT = p.carve([128, 8, T], BF16, "mergedT")
        mk_w = p.mark()
        wg = [p.carve([128, 8, 512], BF16, "mwg%d" % i) for i in range(2)]
        wb = [p.carve([128, 4, 512], BF16, "mwb%d" % i) for i in range(2)]
        acc = [p.carve([128, 512], F32, "macc%d" % i) for i in range(2)]
        sg = [p.carve([128, 512], F32, "msg%d" % i) for i in range(2)]
        tm = [p.carve([128, 512], F32, "mtm%d" % i) for i in range(2)]
        brw = [W["w_br_ssd"], W["w_br_nsa"], W["w_br_rwkv"], W["w_br_swa"]]
        mk2 = p.mark()
        cnt = 0
        wcnt = 0
        for th in range(2):
            p.release(mk2)
            yT = {}
            for m in active:
                yT[m] = p.carve([128, 4, T // 2], BF16, "yTs%d" % m)
                for c in range(4):
                    p.dma(yT[m][:, c, :], yT_d[m][c, :, th * 1024:(th + 1) * 1024], reads=[yT_d[m]], writes=[(yT[m], c)])
                p.fence(yT[m])
            for dc in range(8):
                wg_ = wg[wcnt % 2]
                wb_ = wb[wcnt % 2]
                wcnt += 1
                for m in active:
                    load_w(wg_, lambda sv, m=m, wg_=wg_: [(wg_[:, :, m * 128:(m + 1) * 128], sv)],
                           win_cols(l, OFF_GATE + m * 1024 + dc * 128, 128), (8, 128), key=m)
                    load_w(wb_, lambda sv, m=m, wb_=wb_: [(wb_[:, :, m * 128:(m + 1) * 128], sv)],
                           brw[m][l, :, dc * 128:(dc + 1) * 128].rearrange("(c p) n -> p c n", p=128), (4, 128), key=m)
                for bb in range(2):
                    b = th * 2 + bb
                    acc_ = acc[cnt % 2]
                    cnt += 1
                    for mi, m in enumerate(active):
                        pg = nps()
                        proj_fm(pg[:, :], pg, wg_, lambda c, m=m, wg_=wg_: wg_[:, c, m * 128:(m + 1) * 128], b * 512, 512, wkey=m)
                        sg_ = sg[mi % 2]
                        p.op("scalar", lambda e, sg_=sg_, pg=pg: e.activation(sg_[:, :], pg[:, :], AF.Sigmoid), reads=[pg], writes=[sg_])
                        pb_ = nps()
                        for kc in range(4):
                            p.op("tensor", lambda e, pb_=pb_, kc=kc, m=m, wb_=wb_, bb=bb, yTm=yT[m]: e.matmul(
                                pb_[:, :], wb_[:, kc, m * 128:(m + 1) * 128], yTm[:, kc, bb * 512:(bb + 1) * 512], start=(kc == 0), stop=(kc == 3)),
                                 reads=[(wb_, m), yT[m]], writes=[pb_])
                        last = (mi == len(active) - 1)
                        if mi == 0:
                            dst = mergedT[:, dc, b * 512:(b + 1) * 512] if last else acc_[:, :]
                            p.op("vector", lambda e, dst=dst, pb_=pb_, sg_=sg_: e.tensor_tensor(dst, pb_[:, :], sg_[:, :], ALU.mult),
                                 reads=[pb_, sg_], writes=[(mergedT, (dc, b)) if last else acc_])
                        else:
                            tm_ = tm[mi % 2]
                            p.op("vector", lambda e, tm_=tm_, pb_=pb_, sg_=sg_: e.tensor_tensor(tm_[:, :], pb_[:, :], sg_[:, :], ALU.mult),
                                 reads=[pb_, sg_], writes=[tm_])
                            dst = mergedT[:, dc, b * 512:(b + 1) * 512] if last else acc_[:, :]
                            p.op("vector", lambda e, dst=dst, tm_=tm_, acc_=acc_: e.tensor_tensor(dst, tm_[:, :], acc_[:, :], ALU.add),
                                 reads=[tm_, acc_], writes=[(mergedT, (dc, b)) if last else acc_])
        p.fence(mergedT)
        p.release(mk_w)
        wo = p.carve([128, 8, D], BF16, "wo")
        load_w_plain(wo, wo, W["w_out"][l].rearrange("(c p) n -> p c n", p=128), (8, D))
        xt = [p.carve([128, D], F32, "xt%d" % i) for i in range(2)]
        src = x_in if l == 0 else xs
        for i in range(NT):
            x_ = xt[i % 2]
            p.dma(x_[:, :], src[i * 128:(i + 1) * 128, :], reads=[(src, i)], writes=[x_])
            for hf in range(2):
                po = nps()
                for kc in range(8):
                    p.op("tensor", lambda e, po=po, kc=kc, i=i, hf=hf: e.matmul(po[:, :], mergedT[:, kc, i * 128:(i + 1) * 128], wo[:, kc, hf * 512:(hf + 1) * 512],
                                                                              start=(kc == 0), stop=(kc == 7)),
                         reads=[mergedT, wo], writes=[po])
                p.op("vector", lambda e, x_=x_, po=po, hf=hf: e.tensor_tensor(x_[:, hf * 512:(hf + 1) * 512], x_[:, hf * 512:(hf + 1) * 512], po[:, :], ALU.add),
                     reads=[po, x_], writes=[x_])
            p.dma(xs[i * 128:(i + 1) * 128, :], x_[:, :], reads=[x_], writes=[(xs, i)])
            if debug and l == 0:
                p.dma(dbg["x_mix"][i * 128:(i + 1) * 128, :], x_[:, :], reads=[x_], writes=[(dbg["x_mix"], i)], is_output=True)
        p.release(mk)

    def phase_ffn_ple(l, last):
        mk = p.mark()
        xa = p.carve([128, NT, D], F32, "xacc")
        for i in range(NT):
            p.dma(xa[:, i, :], xs[i * 128:(i + 1) * 128, :], reads=[(xs, i)], writes=[(xa, i)])
        is_moe = (l % 2 == 1)
        j = l // 2
        rw = None
        if is_moe:
            rw = p.carve([128, NT, 8], F32, "rw")
        mk_r = p.mark()
        if is_moe:
            rt = p.carve([128, 8, 8], F32, "router")
            p.dma(rt[:, :, :], W["moe_router"][j].rearrange("(c p) n -> p c n", p=128), writes=[rt])
            hf32 = [p.carve([128, D], F32, "hf32_%d" % i) for i in range(2)]
            hTf = [p.carve([128, 8, 128], F32, "hTf%d" % i) for i in range(2)]
            m8 = [p.carve([128, 8], F32, "m8_%d" % i) for i in range(2)]
            lg = [p.carve([128, 8], F32, "lg_%d" % i) for i in range(2)]
            wv = [p.carve([128, 4], F32, "wv_%d" % i) for i in range(2)]
            e1 = [p.carve([128, 8], F32, "e1_%d" % i) for i in range(2)]
            grow = 0 + ROW_NORM_FFN

            def also(i, s_, xb, xap, xk):
                hf_ = hf32[i % 2]
                p.op("vector", lambda e: e.scalar_tensor_tensor(out=hf_[:], in0=xap, scalar=s_[:, 2:3], in1=rows[:, grow:grow + D], op0=ALU.mult, op1=ALU.mult),
                     reads=[(xb, xk), s_, rows], writes=[hf_])
                hT_ = hTf[i % 2]
                for half in range(2):
                    pp = nps()
                    for c in range(4):
                        cc = half * 4 + c
                        p.op("tensor", lambda e, pp=pp, c=c, cc=cc: e.transpose(pp[:, c * 128:(c + 1) * 128], hf_[:, cc * 128:(cc + 1) * 128], identf[:]),
                             reads=[hf_, identf], writes=[pp])
                    p.op("vector", lambda e, pp=pp, half=half: e.tensor_copy(hT_[:, half * 4:(half + 1) * 4, :], pp[:, :].rearrange("p (c t) -> p c t", c=4)),
                         reads=[pp], writes=[(hT_, half)])
                p.fence(hT_)
                pl = nps()
                for c in range(8):
                    p.op("tensor", lambda e, c=c, pl=pl: e.matmul(pl[:, 0:8], hT_[:, c, :], rt[:, c, :], start=(c == 0), stop=(c == 7)),
                         reads=[hT_, rt], writes=[pl])
                lg_ = lg[i % 2]
                m8_ = m8[i % 2]
                wv_ = wv[i % 2]
                e1_ = e1[i % 2]
                p.op("vector", lambda e: e.tensor_copy(lg_[:, :], pl[:, 0:8]), reads=[pl], writes=[lg_])
                p.op("vector", lambda e: e.max(m8_[:, :], lg_[:, :]), reads=[lg_], writes=[m8_])
                p.op("vector", lambda e: e.tensor_tensor(wv_[:, 0:1], m8_[:, 0:1], m8_[:, 1:2], ALU.subtract), reads=[m8_], writes=[wv_])
                p.op("scalar", lambda e: e.activation(wv_[:, 1:2], wv_[:, 0:1], AF.Sigmoid), reads=[wv_], writes=[wv_])
                p.op("scalar", lambda e: e.activation(wv_[:, 2:3], wv_[:, 0:1], AF.Sigmoid, scale=-1.0), reads=[wv_], writes=[wv_])
                p.op("vector", lambda e: e.tensor_scalar(e1_[:, :], lg_[:, :], m8_[:, 0:1], wv_[:, 1:2], ALU.is_equal, ALU.mult),
                     reads=[lg_, m8_, wv_], writes=[e1_])
                p.op("vector", lambda e: e.tensor_scalar(rw[:, i, :], lg_[:, :], m8_[:, 1:2], wv_[:, 2:3], ALU.is_equal, ALU.mult),
                     reads=[lg_, m8_, wv_], writes=[(rw, i)])
                p.op("vector", lambda e: e.tensor_tensor(rw[:, i, :], rw[:, i, :], e1_[:, :], ALU.add), reads=[e1_, (rw, i)], writes=[(rw, i)])
        else:
            also = None
        rmsnorm_to_hT(lambda i: (xa, xa[:, i, :], i), 0 + ROW_NORM_FFN, also)
        if is_moe:
            p.fence(rw)
        p.release(mk_r)
        mk2 = p.mark()
        FC = 256
        nfc = D_FF // FC
        wgb = [p.carve([128, 8, FC], BF16, "fwg%d" % i) for i in range(2)]
        wub = [p.carve([128, 8, FC], BF16, "fwu%d" % i) for i in range(2)]
        wdb = [p.carve([128, 2, D], BF16, "fwd%d" % i) for i in range(2)]
        hid = [p.carve([128, 2, T], BF16, "hid%d" % i) for i in range(2)]
        sl = [p.carve([128, 512], F32, "sl%d" % i) for i in range(2)]
        experts = list(range(8)) if is_moe else [None]
        funits = [(ex, fc) for ex in experts for fc in range(nfc)]

        def wsel(ex):
            if is_moe:
                return W["moe_w_gate"][j, ex], W["moe_w_up"][j, ex], W["moe_w_down"][j, ex]
            return W["ffn_w_gate"][j], W["ffn_w_up"][j], W["ffn_w_down"][j]

        def up_part(it):
            ex, fc = funits[it]
            Wg, Wu, Wd = wsel(ex)
            wg_, wu_, wd_, hid_ = wgb[it % 2], wub[it % 2], wdb[it % 2], hid[it % 2]
            load_w(wg_, lambda sv: [(wg_[:, :, :], sv)], Wg[:, fc * FC:(fc + 1) * FC].rearrange("(c p) n -> p c n", p=128), (8, FC))
            load_w(wu_, lambda sv: [(wu_[:, :, :], sv)], Wu[:, fc * FC:(fc + 1) * FC].rearrange("(c p) n -> p c n", p=128), (8, FC))
            load_w(wd_, lambda sv: [(wd_[:, :, :], sv)], Wd[fc * FC:(fc + 1) * FC, :].rearrange("(c p) n -> p c n", p=128), (2, D))
            cnt = 0
            for fs in range(2):
                for b in range(4):
                    pg = nps()
                    pu = nps()
                    proj_fm(pg[:, :], pg, wg_, lambda c, wg_=wg_, fs=fs: wg_[:, c, fs * 128:(fs + 1) * 128], b * 512, 512)
                    proj_fm(pu[:, :], pu, wu_, lambda c, wu_=wu_, fs=fs: wu_[:, c, fs * 128:(fs + 1) * 128], b * 512, 512)
                    sl_ = sl[cnt % 2]
                    cnt += 1
                    p.op("scalar", lambda e: e.activation(sl_[:, :], pg[:, :], AF.Silu), reads=[pg], writes=[sl_])
                    p.op("vector", lambda e: e.tensor_tensor(hid_[:, fs, b * 512:(b + 1) * 512], sl_[:, :], pu[:, :], ALU.mult),
                         reads=[sl_, pu], writes=[(hid_, (fs, b))])
            p.fence(hid_)

        def down_part(it):
            ex, fc = funits[it]
            wd_, hid_ = wdb[it % 2], hid[it % 2]
            for i in range(NT):
                for hf in range(2):
                    pd = nps()
                    for fs in range(2):
                        p.op("tensor", lambda e: e.matmul(pd[:, :], hid_[:, fs, i * 128:(i + 1) * 128], wd_[:, fs, hf * 512:(hf + 1) * 512], start=(fs == 0), stop=(fs == 1)),
                             reads=[hid_, wd_], writes=[pd])
                    xv = xa[:, i, hf * 512:(hf + 1) * 512]
                    if is_moe:
                        p.op("vector", lambda e: e.scalar_tensor_tensor(out=xv, in0=pd[:, :], scalar=rw[:, i, ex:ex + 1], in1=xv, op0=ALU.mult, op1=ALU.add),
                             reads=[pd, rw, (xa, i)], writes=[(xa, i)])
                    else:
                        p.op("vector", lambda e: e.tensor_tensor(xv, xv, pd[:, :], ALU.add), reads=[pd, (xa, i)], writes=[(xa, i)])

        up_part(0)
        for it in range(len(funits)):
            if it + 1 < len(funits):
                up_part(it + 1)
            down_part(it)
        p.release(mk2)
        if debug and l == 0:
            for i in range(NT):
                p.dma(dbg["x_ffn"][i * 128:(i + 1) * 128, :], xa[:, i, :], reads=[(xa, i)], writes=[(dbg["x_ffn"], i)], is_output=True)
        mk3 = p.mark()
        wpg = p.carve([128, 8, D], BF16, "wpg")
        wpp = p.carve([128, 2, D], BF16, "wpp")
        load_w_plain(wpg, wpg, W["ple_gate"][l].rearrange("(c p) n -> p c n", p=128), (8, D))
        load_w_plain(wpp, wpp, W["ple_proj"][l].rearrange("(c p) n -> p c n", p=128), (2, D))
        xb16 = [p.carve([128, D], BF16, "xb16_%d" % i) for i in range(2)]
        pf = [p.carve([128, 256], F32, "pf%d" % i) for i in range(2)]
        pb16 = [p.carve([128, 256], BF16, "pb16_%d" % i) for i in range(2)]
        pT = [p.carve([128, 2, 128], BF16, "pT%d" % i) for i in range(2)]
        sgp = [p.carve([128, 512], F32, "sgp%d" % i) for i in range(2)]
        tmp = [p.carve([128, 512], F32, "tmpp%d" % i) for i in range(2)]
        for i in range(NT):
            xb_ = xb16[i % 2]
            p.op("gpsimd", lambda e, xb_=xb_, i=i: e.tensor_copy(xb_[:, :], xa[:, i, :]), reads=[(xa, i)], writes=[xb_])
            pt = npst()
            for c in range(8):
                p.op("tensor", lambda e, pt=pt, c=c, xb_=xb_: e.transpose(pt[:, c * 128:(c + 1) * 128], xb_[:, c * 128:(c + 1) * 128], identb[:]),
                     reads=[xb_, identb], writes=[pt])
            p.op("scalar", lambda e, pt=pt, i=i: e.copy(hT[:, :, i * 128:(i + 1) * 128], pt[:, :].rearrange("p (c t) -> p c t", c=8)),
                 reads=[pt], writes=[(hT, i)])
            pf_, pb_, pT_ = pf[i % 2], pb16[i % 2], pT[i % 2]
            p.dma(pf_[:, :], p_in[l, i * 128:(i + 1) * 128, :], writes=[pf_])
            p.op("gpsimd", lambda e, pf_=pf_, pb_=pb_: e.tensor_copy(pb_[:, :], pf_[:, :]), reads=[pf_], writes=[pb_])
            pt2 = npst()
            for c in range(2):
                p.op("tensor", lambda e, pt2=pt2, c=c, pb_=pb_: e.transpose(pt2[:, c * 128:(c + 1) * 128], pb_[:, c * 128:(c + 1) * 128], identb[:]),
                     reads=[pb_, identb], writes=[pt2])
            p.op("vector", lambda e, pt2=pt2, pT_=pT_: e.tensor_copy(pT_[:, :, :], pt2[:, 0:256].rearrange("p (c t) -> p c t", c=2)), reads=[pt2], writes=[pT_])
            for hf in range(2):
                pg = nps()
                proj_tm(pg[:, :], pg, wpg, lambda c, hf=hf: wpg[:, c, hf * 512:(hf + 1) * 512], i)
                pq = nps()
                for c in range(2):
                    p.op("tensor", lambda e, pq=pq, c=c, pT_=pT_, hf=hf: e.matmul(pq[:, :], pT_[:, c, :], wpp[:, c, hf * 512:(hf + 1) * 512], start=(c == 0), stop=(c == 1)),
                         reads=[pT_, wpp], writes=[pq])
                sg_, tm_ = sgp[hf], tmp[hf]
                p.op("scalar", lambda e, sg_=sg_, pg=pg: e.activation(sg_[:, :], pg[:, :], AF.Sigmoid), reads=[pg], writes=[sg_])
                p.op("vector", lambda e, tm_=tm_, sg_=sg_, pq=pq: e.tensor_tensor(tm_[:, :], sg_[:, :], pq[:, :], ALU.mult), reads=[sg_, pq], writes=[tm_])
                xv = xa[:, i, hf * 512:(hf + 1) * 512]
                p.op("vector", lambda e, xv=xv, tm_=tm_: e.tensor_tensor(xv, xv, tm_[:, :], ALU.add), reads=[tm_, (xa, i)], writes=[(xa, i)])
        p.release(mk3)
        if not last:
            for i in range(NT):
                p.dma(xs[i * 128:(i + 1) * 128, :], xa[:, i, :], reads=[(xa, i)], writes=[(xs, i)])
                if debug and l == 0:
                    p.dma(dbg["x_l0"][i * 128:(i + 1) * 128, :], xa[:, i, :], reads=[(xa, i)], writes=[(dbg["x_l0"], i)], is_output=True)
        else:
            mk4 = p.mark()
            junk = p.carve([128, D], F32, "fjunk")
            ob = [p.carve([128, D], F32, "fo%d" % i) for i in range(2)]
            st = [p.carve([128, 4], F32, "fst%d" % i) for i in range(2)]
            for i in range(NT):
                s_, o_ = st[i % 2], ob[i % 2]
                p.op("scalar", lambda e, s_=s_, i=i: e.activation(junk[:], xa[:, i, :], AF.Square, accum_out=s_[:, 0:1]), reads=[(xa, i)], writes=[junk, s_])
                p.op("scalar", lambda e, s_=s_: e.activation(s_[:, 1:2], s_[:, 0:1], AF.Sqrt, bias=epsc[:, 0:1], scale=1.0 / D), reads=[s_, epsc], writes=[s_])
                p.op("vector", lambda e, s_=s_: e.reciprocal(s_[:, 2:3], s_[:, 1:2]), reads=[s_], writes=[s_])
                p.op("vector", lambda e, s_=s_, o_=o_, i=i: e.scalar_tensor_tensor(out=o_[:], in0=xa[:, i, :], scalar=s_[:, 2:3], in1=rowsf[:, 0:D], op0=ALU.mult, op1=ALU.mult),
                     reads=[(xa, i), s_, rowsf], writes=[o_])
                p.dma(out_d[i * 128:(i + 1) * 128, :], o_[:, :], reads=[o_], writes=[(out_d, i)], is_output=True)
            p.release(mk4)
        p.release(mk)

    MIX = {"swa": (3, mixer_swa), "ssd": (0, mixer_ssd), "nsa": (1, mixer_nsa), "rwkv": (2, mixer_rwkv)}
    for l in range(n_layers):
        src = x_in if l == 0 else xs
        p.dma(rows[:], rows_in[:, l * ROW_PER_LAYER:(l + 1) * ROW_PER_LAYER], writes=[rows])
        mk = p.mark()
        xt = [p.carve([128, D], F32, "xin%d" % i) for i in range(2)]

        def src_tile(i, src=src, xt=xt):
            x_ = xt[i % 2]
            p.dma(x_[:, :], src[i * 128:(i + 1) * 128, :], reads=[(src, i)], writes=[x_])
            return (x_, x_[:, :], None)
        rmsnorm_to_hT(src_tile, 0 + ROW_NORM_MIX)
        p.release(mk)
        active = []
        for name in mixers:
            m, fn = MIX[name]
            fn(l)
            active.append(m)
            if debug and l == 0:
                for c in range(4):
                    p.dma(dbg["yT%d" % m][c, :, :], yT_d[m][c, :, :], reads=[yT_d[m]], writes=[(dbg["yT%d" % m], c)], is_output=True)
        phase_merge(l, sorted(active))
        phase_ffn_ple(l, last=(l == n_layers - 1))
    return p.finish()


def make_in_maps(inp, n_cores=8):
    consts = build_consts()
    rows = build_rows(inp)
    cols = build_cols(inp)
    inp = dict(inp)
    inp["nsa_peT"] = np.ascontiguousarray(np.asarray(inp["nsa_cmp_pe"]).transpose(0, 1, 3, 2))
    shared = {k: np.ascontiguousarray(np.asarray(inp[k], dtype=np.float32)) for k in WEIGHT_SPECS}
    maps = []
    for b in range(n_cores):
        m = dict(shared)
        m.update(consts)
        m["rows"] = rows
        m["cols"] = cols
        m["x"] = np.ascontiguousarray(inp["x"][b])
        m["p"] = np.ascontiguousarray(inp["p"][:, b])
        m["pos"] = np.ascontiguousarray(np.broadcast_to(np.asarray(inp["positions"][b], dtype=np.int32)[None, :], (64, T)))
        maps.append(m)
    return maps


def kernel(**inputs):
    inp = {k: np.asarray(v) for k, v in inputs.items()}
    nc = build()
    maps = make_in_maps(inp)
    res = run_bass_kernel_spmd(nc, maps, core_ids=list(range(8)))
    return np.stack([r["out"] for r in res.results], axis=0).astype(np.float32)
```
